# Optimizing a Trainium2 kernel written in Bass

```python
import math
import jax, jax.numpy as jnp
from jax import lax
import numpy as np

D_MODEL = 1024
BATCH = 8
SEQ = 2048
DEPTH = 1
DEC_BATCH = 128
DEC_SEQ = 4
PAST_LEN = 16384
PAGE_SIZE = 128

D_CONV = D_MODEL // 2
CONV_WIDTH = 31
D_SSM = D_MODEL // 2
SSM_GROUP = 16
SSM_GROUPS = D_SSM // SSM_GROUP
SSM_STATE = 64
N_EXPERTS = 32
TOP_K = 4
D_FF = D_MODEL
SWIGLU_LIMIT = 7.0
SWIGLU_ALPHA = 1.702
DT_MIN = 1e-3
DT_MAX = 1e-1
LN_EPS = 1e-5
DN_ALPHA = (2 * DEPTH) ** 0.25
DN_BETA = (8 * DEPTH) ** -0.25
IN_COLS = 2 * D_CONV + D_SSM + 2 * D_MODEL

kernel_name = "gated_conv_s5_moe_decoder_step"


def layer_norm(x, g, b):
    xf = x.astype(jnp.float32)
    mu = jnp.mean(xf, -1, keepdims=True)
    var = jnp.mean(jnp.square(xf - mu), -1, keepdims=True)
    y = (xf - mu) * lax.rsqrt(var + LN_EPS) * g.astype(jnp.float32) + b.astype(jnp.float32)
    return y.astype(x.dtype)


def conformer_conv(z, buf, w_dw, b_dw, ln_g, ln_b, w_pw, b_pw):
    a, gt = jnp.split(z, 2, axis=-1)
    u = a * jax.nn.sigmoid(gt)
    full = jnp.concatenate([buf.astype(u.dtype), u], axis=1)
    v = lax.conv_general_dilated(
        full, w_dw[:, None, :].astype(u.dtype), window_strides=(1,), padding="VALID",
        dimension_numbers=("NWC", "WIO", "NWC"), feature_group_count=D_CONV) + b_dw
    v = jax.nn.silu(layer_norm(v, ln_g, ln_b))
    return v @ w_pw + b_pw, full[:, -(CONV_WIDTH - 1):]


def _scan_combine(e1, e2):
    a1, b1 = e1
    a2, b2 = e2
    return a2 * a1, a2 * b1 + b2


def s5_layer(u, h0_re, h0_im, lam_re, lam_im, log_dt, b_re, b_im, c_re, c_im, d_skip):
    f32 = jnp.float32
    bsz, L, _ = u.shape
    uf = u.astype(f32).reshape(bsz, L, SSM_GROUPS, SSM_GROUP)
    lam = lax.complex(lam_re.astype(f32), lam_im.astype(f32))
    dt = jnp.exp(log_dt.astype(f32))[:, None]
    lam_bar = jnp.exp(lam * dt)
    b_bar = ((lam_bar - 1.0) / lam)[..., None] * lax.complex(b_re.astype(f32), b_im.astype(f32))
    bu = jnp.einsum('blgh,gph->blgp', uf.astype(jnp.complex64), b_bar)
    h0 = lax.complex(h0_re.astype(f32), h0_im.astype(f32))
    bu = bu.at[:, 0].add(lam_bar * h0)
    a = jnp.broadcast_to(lam_bar, bu.shape)
    _, h = lax.associative_scan(_scan_combine, (a, bu), axis=1)
    c = lax.complex(c_re.astype(f32), c_im.astype(f32))
    y = jnp.real(jnp.einsum('blgp,ghp->blgh', h, c)) + d_skip.astype(f32) * uf
    y = y.reshape(bsz, L, D_SSM).astype(u.dtype)
    h_last = h[:, -1]
    return y, jnp.real(h_last).astype(h0_re.dtype), jnp.imag(h_last).astype(h0_im.dtype)


def mixer(h, conv_buf, h0_re, h0_im, p):
    z = h @ p['w_in'] + p['b_in']
    z_conv = z[..., :2 * D_CONV]
    z_ssm = z[..., 2 * D_CONV:2 * D_CONV + D_SSM]
    g_conv = jax.nn.sigmoid(z[..., 2 * D_CONV + D_SSM:2 * D_CONV + D_SSM + D_MODEL])
    g_ssm = jax.nn.sigmoid(z[..., 2 * D_CONV + D_SSM + D_MODEL:])
    conv_out, new_buf = conformer_conv(z_conv, conv_buf, p['w_dw'], p['b_dw'], p['conv_ln_g'],
                                       p['conv_ln_b'], p['w_pw'], p['b_pw'])
    y, new_re, new_im = s5_layer(z_ssm, h0_re, h0_im, p['lam_re'], p['lam_im'], p['log_dt'],
                                 p['b_re'], p['b_im'], p['c_re'], p['c_im'], p['d_skip'])
    yg = jax.nn.gelu(y)
    ssm_out = (yg @ p['w_sv'] + p['b_sv']) * jax.nn.sigmoid(yg @ p['w_sg'] + p['b_sg'])
    merged = g_conv * conv_out + g_ssm * ssm_out
    return merged @ p['w_out'] + p['b_out'], new_buf, new_re, new_im


def moe(h, w_r, b_r, w1, b1, w2, b2):
    shp = h.shape
    t = h.reshape(-1, D_MODEL)
    logits = (t @ w_r + b_r).astype(jnp.float32)
    top_val, top_idx = lax.top_k(logits, TOP_K)
    gates = jax.nn.softmax(top_val, axis=-1)
    comb = jnp.einsum('tk,tke->te', gates,
                      jax.nn.one_hot(top_idx, N_EXPERTS, dtype=jnp.float32)).astype(h.dtype)
    out = jnp.zeros_like(t)
    for e in range(N_EXPERTS):
        gu = t @ w1[e] + b1[e]
        g = jnp.minimum(gu[:, :D_FF], SWIGLU_LIMIT)
        up = jnp.clip(gu[:, D_FF:], -SWIGLU_LIMIT, SWIGLU_LIMIT)
        act = g * jax.nn.sigmoid(SWIGLU_ALPHA * g) * (up + 1.0)
        out = out + comb[:, e:e + 1] * (act @ w2[e] + b2[e])
    return out.reshape(shp)


def block(x, c, conv_buf, h0_re, h0_im, p):
    mod = (jax.nn.silu(c) @ p['w_ada'] + p['b_ada'])[:, None, :]
    sh1, sc1, g1, sh2, sc2, g2 = jnp.split(mod, 6, axis=-1)
    h = x * (1.0 + sc1) + sh1
    m, new_buf, new_re, new_im = mixer(h, conv_buf, h0_re, h0_im, p)
    x = layer_norm(DN_ALPHA * x + g1 * m, p['ln1_g'], p['ln1_b'])
    h = x * (1.0 + sc2) + sh2
    f = moe(h, p['w_router'], p['b_router'], p['w1'], p['b1'], p['w2'], p['b2'])
    x = layer_norm(DN_ALPHA * x + g2 * f, p['ln2_g'], p['ln2_b'])
    return x, new_buf, new_re, new_im


def setup_inputs(seed: int = 0) -> dict:
    key = jax.random.key(seed)
    ks = iter(jax.random.split(key, 64))
    f32 = jnp.float32

    def nrm(shape, scale):
        return scale * jax.random.normal(next(ks), shape, f32)

    L = DEPTH
    G, P, H = SSM_GROUPS, SSM_STATE, SSM_GROUP
    inputs = {
        'x_prompt': nrm((BATCH, SEQ, D_MODEL), 1.0),
        'x_sample': nrm((DEC_BATCH, DEC_SEQ, D_MODEL), 1.0),
        'state_conv': nrm((L, DEC_BATCH, CONV_WIDTH - 1, D_CONV), 1.0),
        'state_ssm_re': nrm((L, DEC_BATCH, G, P), 1.0),
        'state_ssm_im': nrm((L, DEC_BATCH, G, P), 1.0),
        'c_prompt': nrm((BATCH, D_MODEL), 1.0),
        'c_sample': nrm((DEC_BATCH, D_MODEL), 1.0),
        'w_ada': nrm((L, D_MODEL, 6 * D_MODEL), 0.5 * D_MODEL ** -0.5),
        'b_ada': nrm((L, 6 * D_MODEL), 0.02),
        'w_in': nrm((L, D_MODEL, IN_COLS), D_MODEL ** -0.5),
        'b_in': nrm((L, IN_COLS), 0.02),
        'w_dw': nrm((L, CONV_WIDTH, D_CONV), CONV_WIDTH ** -0.5),
        'b_dw': nrm((L, D_CONV), 0.02),
        'conv_ln_g': 1.0 + nrm((L, D_CONV), 0.02),
        'conv_ln_b': nrm((L, D_CONV), 0.02),
        'w_pw': nrm((L, D_CONV, D_MODEL), D_CONV ** -0.5),
        'b_pw': nrm((L, D_MODEL), 0.02),
        'lam_re': -0.5 + nrm((L, G, P), 0.01),
        'lam_im': jnp.broadcast_to(jnp.pi * jnp.arange(P, dtype=f32), (L, G, P)) + nrm((L, G, P), 0.01),
        'log_dt': jax.random.uniform(next(ks), (L, G), f32, math.log(DT_MIN), math.log(DT_MAX)),
        'b_re': nrm((L, G, P, H), (2.0 * H) ** -0.5),
        'b_im': nrm((L, G, P, H), (2.0 * H) ** -0.5),
        'c_re': nrm((L, G, H, P), (2.0 * P) ** -0.5),
        'c_im': nrm((L, G, H, P), (2.0 * P) ** -0.5),
        'd_skip': nrm((L, G, H), 1.0),
        'w_sv': nrm((L, D_SSM, D_MODEL), D_SSM ** -0.5),
        'b_sv': nrm((L, D_MODEL), 0.02),
        'w_sg': nrm((L, D_SSM, D_MODEL), D_SSM ** -0.5),
        'b_sg': nrm((L, D_MODEL), 0.02),
        'w_out': nrm((L, D_MODEL, D_MODEL), DN_BETA * D_MODEL ** -0.5),
        'b_out': nrm((L, D_MODEL), 0.02),
        'ln1_g': 1.0 + nrm((L, D_MODEL), 0.02),
        'ln1_b': nrm((L, D_MODEL), 0.02),
        'w_router': nrm((L, D_MODEL, N_EXPERTS), D_MODEL ** -0.5),
        'b_router': nrm((L, N_EXPERTS), 0.01),
        'w1': nrm((L, N_EXPERTS, D_MODEL, 2 * D_FF), D_MODEL ** -0.5),
        'b1': nrm((L, N_EXPERTS, 2 * D_FF), 0.02),
        'w2': nrm((L, N_EXPERTS, D_FF, D_MODEL), DN_BETA * D_FF ** -0.5),
        'b2': nrm((L, N_EXPERTS, D_MODEL), 0.02),
        'ln2_g': 1.0 + nrm((L, D_MODEL), 0.02),
        'ln2_b': nrm((L, D_MODEL), 0.02),
    }
    return inputs


def reference(x_prompt, x_sample, state_conv, state_ssm_re, state_ssm_im, c_prompt, c_sample,
              w_ada, b_ada, w_in, b_in, w_dw, b_dw, conv_ln_g, conv_ln_b, w_pw, b_pw,
              lam_re, lam_im, log_dt, b_re, b_im, c_re, c_im, d_skip,
              w_sv, b_sv, w_sg, b_sg, w_out, b_out, ln1_g, ln1_b,
              w_router, b_router, w1, b1, w2, b2, ln2_g, ln2_b):
    xp, xs = x_prompt, x_sample
    conv_p, re_p, im_p, conv_s, re_s, im_s = [], [], [], [], [], []
    for l in range(DEPTH):
        p = {
            'w_ada': w_ada[l], 'b_ada': b_ada[l], 'w_in': w_in[l], 'b_in': b_in[l],
            'w_dw': w_dw[l], 'b_dw': b_dw[l], 'conv_ln_g': conv_ln_g[l], 'conv_ln_b': conv_ln_b[l],
            'w_pw': w_pw[l], 'b_pw': b_pw[l], 'lam_re': lam_re[l], 'lam_im': lam_im[l],
            'log_dt': log_dt[l], 'b_re': b_re[l], 'b_im': b_im[l], 'c_re': c_re[l], 'c_im': c_im[l],
            'd_skip': d_skip[l], 'w_sv': w_sv[l], 'b_sv': b_sv[l], 'w_sg': w_sg[l], 'b_sg': b_sg[l],
            'w_out': w_out[l], 'b_out': b_out[l], 'ln1_g': ln1_g[l], 'ln1_b': ln1_b[l],
            'w_router': w_router[l], 'b_router': b_router[l], 'w1': w1[l], 'b1': b1[l],
            'w2': w2[l], 'b2': b2[l], 'ln2_g': ln2_g[l], 'ln2_b': ln2_b[l],
        }
        zbuf = jnp.zeros((xp.shape[0], CONV_WIDTH - 1, D_CONV), xp.dtype)
        zre = jnp.zeros((xp.shape[0], SSM_GROUPS, SSM_STATE), state_ssm_re.dtype)
        xp, nb, nr, ni = block(xp, c_prompt, zbuf, zre, zre, p)
        conv_p.append(nb.astype(state_conv.dtype)); re_p.append(nr); im_p.append(ni)
        xs, nb, nr, ni = block(xs, c_sample, state_conv[l], state_ssm_re[l], state_ssm_im[l], p)
        conv_s.append(nb.astype(state_conv.dtype)); re_s.append(nr); im_s.append(ni)
    return (xp, xs, jnp.stack(conv_p), jnp.stack(re_p), jnp.stack(im_p),
            jnp.stack(conv_s), jnp.stack(re_s), jnp.stack(im_s))
```

```python
import contextlib
import math
import numpy as np
import concourse.bass as bass
import concourse.mybir as mybir
from concourse.bass_utils import run_bass_kernel_spmd

F32 = mybir.dt.float32
BF16 = mybir.dt.bfloat16
U8 = mybir.dt.uint8
AF = mybir.ActivationFunctionType
ALU = mybir.AluOpType
DTSIZE = {F32: 4, BF16: 2, U8: 1, mybir.dt.int32: 4, mybir.dt.uint32: 4}

ENGS = ("pe", "act", "dve", "pool", "sp")
SEM_ROLL = 30000
NDMA_SEM = 20

D = 1024
NT = 8
L = 2048
NB = 256
NBLK = L // NB
CH = NB // 4
DS = 64
NSEQ = 17
NTOK = L + DS
DN_ALPHA = 2.0 ** 0.25
LN_EPS = 1e-5
N_EXP = 32


class Buf:
    __slots__ = ("ap", "key")

    def __init__(self, ap, key):
        self.ap = ap
        self.key = key

    def __getitem__(self, idx):
        return Buf(self.ap[idx], self.key)

    def re(self, pat, **kw):
        return Buf(self.ap.rearrange(pat, **kw), self.key)

    def bc(self, shape):
        return Buf(self.ap.to_broadcast(list(shape)), self.key)

    def un(self, d):
        return Buf(self.ap.unsqueeze(d), self.key)

    def cast(self, dt):
        return Buf(self.ap.bitcast(dt), self.key)

    def k(self, suffix):
        return Buf(self.ap, f"{self.key}.{suffix}")


def A(x):
    return x.ap if isinstance(x, Buf) else x


class Prog:
    def __init__(self, nc):
        self.nc = nc
        self.ops = {e: [] for e in ENGS}
        self.last_w = {}
        self.readers = {}
        self.ndma = {e: 0 for e in ENGS}
        self.out_dmas = []
        self.open_dmas = set()
        self.pending = {e: set() for e in ENGS}
        self.rr = 0
        self._cap = None

    def begin(self):
        self._cap = []
        return self._cap

    def end(self):
        self._cap = None

    def replay(self, streams):
        pos = [0] * len(streams)
        total = sum(len(x) for x in streams)
        for _ in range(total):
            best, bi = None, None
            for i, x in enumerate(streams):
                if pos[i] < len(x):
                    frac = pos[i] / len(x)
                    if best is None or frac < best:
                        best, bi = frac, i
            kind, args, kw = streams[bi][pos[bi]]
            pos[bi] += 1
            if kind == "op":
                self.op(*args, **kw)
            else:
                self.dma(*args, **kw)

    def _deps(self, reads, writes):
        deps = set()
        for k in reads:
            if k in self.last_w:
                deps.add(self.last_w[k])
        for k in writes:
            if k in self.last_w:
                deps.add(self.last_w[k])
            for r in self.readers.get(k, ()):
                deps.add(r)
        return deps

    def _commit(self, ident, reads, writes):
        for k in reads:
            self.readers.setdefault(k, []).append(ident)
        for k in writes:
            self.last_w[k] = ident
            self.readers[k] = []

    def _keys(self, outs, ins):
        reads = [b.key for b in ins if isinstance(b, Buf)]
        writes = [b.key for b in outs if isinstance(b, Buf)]
        writes += [k for k in reads if k.startswith("ps")]
        return reads, writes

    def op(self, eng, fn, outs=(), ins=()):
        if self._cap is not None:
            self._cap.append(("op", (eng, fn, list(outs), list(ins)), {}))
            return {}
        reads, writes = self._keys(outs, ins)
        idx = len(self.ops[eng])
        deps = self._deps(reads, writes)
        deps |= self.pending[eng]
        self.pending[eng] = set()
        for d in deps:
            self.open_dmas.discard(d)
        rec = dict(kind="op", fn=fn, deps=deps, signal=False, eng=eng, idx=idx)
        self.ops[eng].append(rec)
        self._commit((eng, idx), reads, writes)
        return rec

    def dma(self, q, out, in_, is_out=False, extra_reads=(), **kw):
        if self._cap is not None:
            self._cap.append(("dma", (q, out, in_), dict(is_out=is_out, extra_reads=list(extra_reads), **kw)))
            return {}
        reads, writes = self._keys([out], [in_] + list(extra_reads))
        deps = self._deps(reads, writes)
        deps |= self.pending[q]
        self.pending[q] = set()
        for d in deps:
            self.open_dmas.discard(d)
        n = self.ndma[q]
        self.ndma[q] += 1
        did = ("dma", q, n)
        if n >= NDMA_SEM:
            deps.add(("dma", q, n - NDMA_SEM))
        o, i = A(out), A(in_)
        rec = dict(kind="dma", fn=(lambda h: h.dma_start(out=o, in_=i, **kw)), deps=deps, q=q, n=n,
                   eng=q, idx=len(self.ops[q]))
        self.ops[q].append(rec)
        self._commit(did, reads, writes)
        self.open_dmas.add(did)
        if is_out:
            self.out_dmas.append(did)
        return rec

    def barrier(self):
        deps = set(self.open_dmas)
        for e in ENGS:
            n = len(self.ops[e])
            for i in range(n - 1, -1, -1):
                if self.ops[e][i]["kind"] == "op":
                    deps.add((e, i))
                    break
        for e in ENGS:
            self.pending[e] |= deps
        self.open_dmas = set()

    def act(self, out, in_, func, bias=None, scale=None):
        kw = {}
        ins = [in_]
        if bias is not None:
            kw["bias"] = A(bias)
            ins.append(bias)
        if scale is not None:
            kw["scale"] = A(scale)
            ins.append(scale)
        o, i = A(out), A(in_)
        return self.op("act", lambda h: h.activation(out=o, in_=i, func=func, **kw), [out], ins)

    def tt(self, eng, out, in0, in1, op):
        o, a, b = A(out), A(in0), A(in1)
        return self.op(eng, lambda h: h.tensor_tensor(out=o, in0=a, in1=b, op=op), [out], [in0, in1])

    def ts(self, eng, out, in0, s1, s2=None, op0=ALU.mult, op1=None):
        o, a = A(out), A(in0)
        x1, x2 = A(s1), A(s2)
        if op1 is None:
            fn = lambda h: h.tensor_scalar(out=o, in0=a, scalar1=x1, scalar2=None, op0=op0)
        else:
            fn = lambda h: h.tensor_scalar(out=o, in0=a, scalar1=x1, scalar2=x2, op0=op0, op1=op1)
        return self.op(eng, fn, [out], [in0, s1, s2])

    def stt(self, out, in0, scalar, in1, op0, op1):
        o, a, s, b = A(out), A(in0), A(scalar), A(in1)
        return self.op("dve", lambda h: h.scalar_tensor_tensor(out=o, in0=a, scalar=s, in1=b, op0=op0, op1=op1),
                       [out], [in0, scalar, in1])

    def copy(self, eng, out, in_):
        o, i = A(out), A(in_)
        if eng == "act":
            return self.op("act", lambda h: h.copy(out=o, in_=i), [out], [in_])
        return self.op(eng, lambda h: h.tensor_copy(out=o, in_=i), [out], [in_])

    def memset(self, eng, out, val):
        o = A(out)
        return self.op(eng, lambda h: h.memset(o, val), [out], [])

    def mm(self, out, pairs, extra_ins=()):
        o = A(out)
        prs = [(A(l), A(r)) for l, r in pairs]
        n = len(prs)

        def fn(h):
            ins = None
            for i, (l, r) in enumerate(prs):
                ins = h.matmul(o, lhsT=l, rhs=r, start=(i == 0), stop=(i == n - 1))
            return ins
        ins_b = [x for pr in pairs for x in pr] + list(extra_ins)
        return self.op("pe", fn, [out], ins_b)

    def tr(self, out, in_, ident):
        o, i, idn = A(out), A(in_), A(ident)
        return self.op("pe", lambda h: h.transpose(out=o, in_=i, identity=idn), [out], [in_, ident])

    def scan(self, out, d0, d1, init):
        o, a, b, c = A(out), A(d0), A(d1), A(init)
        return self.op("dve", lambda h: h.tensor_tensor_scan(out=o, data0=a, data1=b, initial=c,
                                                             op0=ALU.mult, op1=ALU.add), [out], [d0, d1, init])

    def emit(self, stack):
        nc = self.nc
        for e in ENGS:
            for rec in self.ops[e]:
                for d in rec["deps"]:
                    if d[0] == "dma":
                        continue
                    de, di = d
                    if de == e and rec["kind"] == "op" and e == "pe":
                        continue
                    self.ops[de][di]["signal"] = True
        self.sems = {}
        for e in ENGS:
            cnt = 0
            for rec in self.ops[e]:
                if rec["kind"] == "op" and rec["signal"]:
                    rec["semi"] = cnt // SEM_ROLL
                    rec["semv"] = cnt % SEM_ROLL + 1
                    cnt += 1
            ns = max(1, (cnt + SEM_ROLL - 1) // SEM_ROLL)
            self.sems[e] = [stack.enter_context(nc.semaphore(f"s_{e}_{i}")) for i in range(ns)]
        self.dsems = {}
        for q in ENGS:
            if self.ndma[q]:
                self.dsems[q] = [stack.enter_context(nc.semaphore(f"d_{q}_{i}"))
                                 for i in range(min(NDMA_SEM, self.ndma[q]))]
        block = stack.enter_context(nc.Block())
        prog = self

        def run_engine(e, h):
            waited = {x: -1 for x in ENGS}
            waited_dma = set()
            for rec in prog.ops[e]:
                need = {}
                for d in rec["deps"]:
                    if d[0] == "dma":
                        if d in waited_dma:
                            continue
                        waited_dma.add(d)
                        _, q, n = d
                        h.wait_ge(prog.dsems[q][n % NDMA_SEM], 16 * (n // NDMA_SEM + 1))
                        continue
                    de, di = d
                    if de == e and rec["kind"] == "op" and e == "pe":
                        continue
                    if di <= waited[de]:
                        continue
                    need[de] = max(need.get(de, -1), di)
                for de, di in need.items():
                    src = prog.ops[de][di]
                    h.wait_ge(prog.sems[de][src["semi"]], src["semv"])
                    waited[de] = di
                ins = rec["fn"](h)
                if rec["kind"] == "dma":
                    ins.then_inc(prog.dsems[rec["q"]][rec["n"] % NDMA_SEM], 16)
                elif rec["signal"]:
                    ins.then_inc(prog.sems[e][rec["semi"]], 1)
            if e == "sp":
                for d in prog.out_dmas:
                    if d in waited_dma:
                        continue
                    _, q, n = d
                    h.wait_ge(prog.dsems[q][n % NDMA_SEM], 16 * (n // NDMA_SEM + 1))

        @block.tensor
        def _(h):
            run_engine("pe", h)

        @block.scalar
        def _(h):
            run_engine("act", h)

        @block.vector
        def _(h):
            run_engine("dve", h)

        @block.gpsimd
        def _(h):
            run_engine("pool", h)

        @block.sync
        def _(h):
            run_engine("sp", h)


class Arena:
    def __init__(self, nc, stack, nbytes):
        self.t = stack.enter_context(nc.sbuf_tensor("arena", [128, nbytes], U8))
        self.n = nbytes
        self.limit = nbytes
        self.off = 0
        self.stk = []
        self.peak = 0
        self.cnt = 0

    def alloc(self, key, shape, dt, parts=128):
        n = int(np.prod(shape)) * DTSIZE[dt]
        n_al = (n + 63) // 64 * 64
        assert self.off + n_al <= self.limit, (key, self.off, n_al, self.limit)
        ap = self.t[0:parts, self.off:self.off + n].bitcast(dt)
        if len(shape) > 1:
            names = [f"d{i}" for i in range(len(shape))]
            pat = "p (" + " ".join(names) + ") -> p " + " ".join(names)
            ap = ap.rearrange(pat, **{nm: s for nm, s in zip(names[1:], shape[1:])})
        self.off += n_al
        self.peak = max(self.peak, self.off)
        self.cnt += 1
        return Buf(ap, f"{key}#{self.cnt}")

    def alloc_top(self, key, shape, dt, parts=128):
        n = int(np.prod(shape)) * DTSIZE[dt]
        n_al = (n + 63) // 64 * 64
        self.limit -= n_al
        save = self.off
        self.off = self.limit
        lim = self.limit
        self.limit = self.n
        b = self.alloc(key, shape, dt, parts)
        self.limit = lim
        self.off = save
        return b

    def push(self):
        self.stk.append(self.off)

    def pop(self):
        self.off = self.stk.pop()


IN_SPECS = [
    ("xp", [L, D]), ("xs", [DS, D]), ("cc", [NSEQ, D]), ("sconv", [16, 30, 512]),
    ("sre", [16, 2048]), ("sim", [16, 2048]),
    ("w_ada", [D, 6 * D]), ("b_ada", [6 * D]), ("w_in", [D, 3584]), ("b_in", [3584]),
    ("w_dw", [31, 512]), ("b_dw", [512]), ("conv_ln_g", [512]), ("conv_ln_b", [512]),
    ("w_pw", [512, D]), ("b_pw", [D]), ("lam_re", [32, 64]), ("lam_im", [32, 64]), ("log_dt", [32]),
    ("b_re", [32, 64, 16]), ("b_im", [32, 64, 16]), ("c_re", [32, 16, 64]), ("c_im", [32, 16, 64]),
    ("d_skip", [32, 16]), ("w_sv", [512, D]), ("b_sv", [D]), ("w_sg", [512, D]), ("b_sg", [D]),
    ("w_out", [D, D]), ("b_out", [D]), ("ln1_g", [D]), ("ln1_b", [D]), ("w_router", [D, 32]),
    ("b_router", [32]), ("w1", [32, D, 2 * D]), ("b1l", [32 * 128, 16]), ("w2", [32, D, D]), ("b2", [32, D]),
    ("ln2_g", [D]), ("ln2_b", [D]),
    ("k_ident", [128, 128]), ("k_selu", [128, 16 * 128]), ("k_sely", [128, 16 * 128]),
    ("k_selp", [NSEQ, 128]), ("k_sels", [NSEQ, DS]),
    ("k_ls", [128, 128]), ("k_iota32", [128, 32]), ("k_tokid", [128, 17]), ("k_svals", [1, 48]),
    ("k_iotak", [128, 8]), ("k_iotae", [32, 1]), ("k_esel", [128, 17 * 17]), ("k_selt", [17, 17 * 128]),
]
OUT_SPECS = [
    ("yp", [L, D]), ("ys", [DS, D]), ("ncp", [30, 512]), ("nrp", [16, 128]), ("nip", [16, 128]),
    ("ncs", [16, 30, 512]), ("nrs", [16, 2048]), ("nis", [16, 2048]),
]
C_BIN, C_BDW, C_CG, C_CB, C_BPW, C_BSV, C_BSG, C_BADA, C_L1G, C_L1B = 0, 28, 32, 36, 40, 48, 56, 64, 112, 120
M_SH1, M_SC1, M_G1, M_SH2, M_SC2, M_G2 = 0, 8, 16, 24, 32, 40


def build_program(dbg=(), stop_after=None):
    nc = bass.Bass("TRN2", target_bir_lowering=False)
    P = Prog(nc)
    I = {n: nc.dram_tensor(n, list(s), F32, kind="ExternalInput").ap() for n, s in IN_SPECS}
    O = {n: nc.dram_tensor(n, list(s), F32, kind="ExternalOutput").ap() for n, s in OUT_SPECS}
    st = contextlib.ExitStack()
    st.__enter__()
    AR = Arena(nc, st, 204 * 1024)
    PS = [Buf(st.enter_context(nc.psum_tensor(f"psb{i}", [128, 512], F32))[:], f"ps{i}") for i in range(8)]
    psi = [0]
    bank_sets = {"all": list(range(8)), "conv": [0, 1, 2], "s5a": [3, 4], "s5b": [5, 6, 7]}
    bank_ctr = {k: 0 for k in bank_sets}
    cur_set = ["all"]

    def bank():
        st_ = cur_set[0]
        lst = bank_sets[st_]
        b = PS[lst[bank_ctr[st_] % len(lst)]]
        bank_ctr[st_] += 1
        return b

    def dbg_out(name, buf, shape, dt=F32):
        if name in dbg:
            t = nc.dram_tensor("dbg_" + name, list(shape), dt, kind="ExternalOutput").ap()
            P.dma("sp", t, buf, is_out=True)

    def finish():
        P.emit(st)
        st.close()
        return nc

    identf = AR.alloc("identf", [128], F32)
    identb = AR.alloc("identb", [128], BF16)
    onesf = AR.alloc("onesf", [128], F32)
    colsT = AR.alloc("colsT", [128], F32)
    modT = AR.alloc("modT", [48, NSEQ], F32)
    A_T = AR.alloc("A_T", [NT, NSEQ], F32)
    Bv_T = AR.alloc("Bv_T", [NT, NSEQ], F32)
    b1T = AR.alloc("b1T", [16, N_EXP], F32)
    merged = AR.alloc_top("merged", [NT, NTOK], BF16)
    epsc = AR.alloc("epsc", [1], F32)

    P.dma("sp", identf, I["k_ident"])
    P.copy("dve", identb, identf)
    P.memset("pool", onesf, 1.0)
    P.memset("pool", epsc, LN_EPS)

    AR.push()
    selu = AR.alloc("selu", [16, 128], BF16)
    sely = AR.alloc("sely", [16, 128], BF16)
    P.dma("pool", selu.re("p a b -> p (a b)"), I["k_selu"])
    P.dma("pool", sely.re("p a b -> p (a b)"), I["k_sely"])
    ring = [AR.alloc(f"ring{i}", [NT, 512], BF16) for i in range(3)]
    rix = [0]

    def ring_load(src_ap, view4=False):
        slot = ring[rix[0] % len(ring)]
        rix[0] += 1
        if view4:
            dst = slot.re("p a b -> p (a b)").re("p (k c) -> p k c", k=4)
        else:
            dst = slot
        P.dma("pool", dst, src_ap)
        return dst

    wdwT = AR.alloc("wdwT", [4, 31], F32)
    Kmat = AR.alloc("Kmat", [16, 128], BF16)
    CexpA = AR.alloc("CexpA", [16, 4, 2, 16], BF16)
    CexpB = AR.alloc("CexpB", [16, 4, 2, 16], BF16)
    BexpRe = AR.alloc("BexpRe", [16, 128], BF16)
    BexpIm = AR.alloc("BexpIm", [16, 128], BF16)
    Tre = AR.alloc("Tre", [16, CH], F32)
    Tim = AR.alloc("Tim", [16, CH], F32)
    dcol = AR.alloc("dcol", [16], F32)
    carry = AR.alloc("carry", [16, 2], F32)
    h0sb = AR.alloc("h0sb", [16, 2, 16], F32)
    rot0 = AR.alloc("rot0", [16, 2, 16], F32)
    Hsh_s = AR.alloc("Hsh_s", [16, 2, 16], BF16)
    hend_s = AR.alloc("hend_s", [16, 2, 16], F32)
    full_s = AR.alloc("full_s", [4, 16, 34], F32)
    P.memset("pool", carry, 0.0)

    AR.push()
    stg = AR.alloc("stg", [128], F32)
    stg_parts = []
    for nm, r0, J in (("b_in", C_BIN, 28), ("b_dw", C_BDW, 4), ("conv_ln_g", C_CG, 4), ("conv_ln_b", C_CB, 4),
                      ("b_pw", C_BPW, 8), ("b_sv", C_BSV, 8), ("b_sg", C_BSG, 8), ("b_ada", C_BADA, 48),
                      ("ln1_g", C_L1G, 8), ("ln1_b", C_L1B, 8)):
        sub = stg[r0:r0 + J, :].k(nm)
        P.dma("sp", sub, I[nm].rearrange("(j p) -> j p", p=128))
        stg_parts.append(sub)
    pb = bank()
    P.op("pe", (lambda h, o=A(pb[:, 0:128]), i=A(stg), idn=A(identf): h.transpose(out=o, in_=i, identity=idn)),
         [pb], [identf] + stg_parts)
    P.copy("dve", colsT, pb[:, 0:128])

    cc_sb = AR.alloc("cc_sb", [D], F32, parts=NSEQ)
    P.dma("sp", cc_sb, I["cc"])
    csl = AR.alloc("csl", [D], F32, parts=NSEQ)
    P.act(csl, cc_sb, AF.Silu)
    siluT = AR.alloc("siluT", [NT, NSEQ], BF16)
    pb = bank()
    for k in range(NT):
        P.tr(pb[:, k * NSEQ:(k + 1) * NSEQ], csl[:, k * 128:(k + 1) * 128], identf[0:NSEQ, 0:NSEQ])
    P.copy("dve", siluT.re("p a b -> p (a b)"), pb[:, 0:NT * NSEQ])

    for c in range(12):
        slot = ring_load(I["w_ada"][:, c * 512:(c + 1) * 512].rearrange("(kt p) c -> p kt c", p=128))
        pb = bank()
        for jj in range(4):
            P.mm(pb[:, jj * NSEQ:(jj + 1) * NSEQ],
                 [(slot[:, k, jj * 128:(jj + 1) * 128], siluT[:, k, :]) for k in range(NT)])
        P.tt("dve", modT[:, 4 * c:4 * c + 4, :], pb[:, 0:4 * NSEQ].re("p (a b) -> p a b", a=4),
             colsT[:, C_BADA + 4 * c:C_BADA + 4 * c + 4].un(2).bc([128, 4, NSEQ]), ALU.add)
    P.ts("dve", modT[:, M_SC1:M_SC1 + 8, :], modT[:, M_SC1:M_SC1 + 8, :], 1.0, None, op0=ALU.add)
    P.ts("dve", modT[:, M_SC2:M_SC2 + 8, :], modT[:, M_SC2:M_SC2 + 8, :], 1.0, None, op0=ALU.add)
    P.tt("dve", A_T, modT[:, M_SC2:M_SC2 + 8, :], colsT[:, C_L1G:C_L1G + 8].un(2).bc([128, NT, NSEQ]), ALU.mult)
    P.tt("dve", Bv_T, modT[:, M_SC2:M_SC2 + 8, :], colsT[:, C_L1B:C_L1B + 8].un(2).bc([128, NT, NSEQ]), ALU.mult)
    P.tt("dve", Bv_T, Bv_T, modT[:, M_SH2:M_SH2 + 8, :], ALU.add)
    dbg_out("modT", modT, [128, 48, NSEQ])

    stgw = AR.alloc("stgw", [512], F32, parts=31)
    P.dma("sp", stgw, I["w_dw"])
    pb = bank()
    for ct in range(4):
        P.tr(pb[:, ct * 31:(ct + 1) * 31], stgw[:, ct * 128:(ct + 1) * 128], identf[0:31, 0:31])
    P.copy("dve", wdwT.re("p a b -> p (a b)"), pb[:, 0:124])

    stgc = AR.alloc("stgc", [4, 512], F32, parts=30)
    for sq in range(4):
        P.dma("sp", stgc, I["sconv"][4 * sq:4 * sq + 4].rearrange("s r c -> r s c"))
        for ct in range(4):
            pb = bank()
            for s4 in range(4):
                P.tr(pb[:, s4 * 30:(s4 + 1) * 30], stgc[:, s4, ct * 128:(ct + 1) * 128], identf[0:30, 0:30])
            P.copy("dve", full_s[:, ct, 4 * sq:4 * sq + 4, 0:30], pb[:, 0:120].re("p (s r) -> p s r", s=4))
    P.dma("sp", O["ncs"][:, 0:26, :], I["sconv"][:, 4:30, :], is_out=True)

    stg2 = AR.alloc("stg2", [128], F32, parts=64)
    P.dma("sp", stg2[0:16, :].k("a"), I["lam_re"].rearrange("(q g) p -> q (g p)", g=2))
    P.dma("sp", stg2[16:32, :].k("b"), I["lam_im"].rearrange("(q g) p -> q (g p)", g=2))
    ldt_raw = AR.alloc("ldt_raw", [2], F32, parts=48)
    P.dma("sp", ldt_raw[32:48, :], I["log_dt"].rearrange("(q g) -> q g", g=2))
    P.copy("dve", stg2[32:48, :].k("c").re("q (g p) -> q g p", g=2), ldt_raw[32:48, :].un(2).bc([16, 2, 64]))
    P.dma("sp", stg2[48:64, :].k("d").re("q (s m) -> q s m", s=4),
          I["d_skip"].rearrange("(q g) h -> q (g h)", g=2).unsqueeze(1).to_broadcast([16, 4, 32]))
    parT = AR.alloc("parT", [64], F32)
    pb = bank()
    P.op("pe", (lambda h, o=A(pb[:, 0:64]), i=A(stg2), idn=A(identf[0:64, 0:64]): h.transpose(out=o, in_=i, identity=idn)),
         [pb], [identf, stg2.k("a"), stg2.k("b"), stg2.k("c"), stg2.k("d")])
    P.copy("dve", parT, pb[:, 0:64])
    lamre, lamim, ldt, dexp = parT[:, 0:16], parT[:, 16:32], parT[:, 32:48], parT[:, 48:64]

    Bre = AR.alloc("Bre", [16, 16], F32)
    Bim = AR.alloc("Bim", [16, 16], F32)
    P.dma("sp", Bre, I["b_re"].rearrange("(q g) p h -> (g p) q h", g=2))
    P.dma("sp", Bim, I["b_im"].rearrange("(q g) p h -> (g p) q h", g=2))
    Craw = AR.alloc("Craw", [32, 64], F32, parts=16)
    CTre = AR.alloc("CTre", [16, 16], F32)
    CTim = AR.alloc("CTim", [16, 16], F32)
    for (dst, nm) in ((CTre, "c_re"), (CTim, "c_im")):
        P.dma("sp", Craw, I[nm].rearrange("g h p -> h g p"))
        pb = bank()
        for q in range(16):
            P.tr(pb[:, q * 16:(q + 1) * 16], Craw[:, 2 * q:2 * q + 2, :].re("h g p -> h (g p)"), identf[0:16, 0:16])
        P.copy("dve", dst.re("p a b -> p (a b)"), pb[:, 0:256])

    def sm(name, n=16):
        return AR.alloc(name, [n], F32)

    dt_ = sm("dt")
    P.act(dt_, ldt, AF.Exp)
    mag = sm("mag")
    lrd = sm("lrd")
    P.tt("dve", lrd, lamre, dt_, ALU.mult)
    P.act(mag, lrd, AF.Exp)
    ang = sm("ang")
    P.tt("dve", ang, lamim, dt_, ALU.mult)
    halfpi = sm("halfpi", 1)
    P.memset("pool", halfpi, math.pi / 2)
    s_ = sm("s_")
    c_ = sm("c_")
    P.act(s_, ang, AF.Sin, scale=1.0 / 8)
    P.act(c_, ang, AF.Sin, bias=halfpi[:, 0:1], scale=-1.0 / 8)
    t1 = sm("t1")
    t2 = sm("t2")
    for _ in range(3):
        P.tt("dve", t1, c_, c_, ALU.mult)
        P.tt("dve", t2, s_, s_, ALU.mult)
        P.tt("dve", s_, s_, c_, ALU.mult)
        P.ts("dve", s_, s_, 2.0, None, op0=ALU.mult)
        P.tt("dve", c_, t1, t2, ALU.subtract)
    PWre = AR.alloc("PWre", [5, 16], F32)
    PWim = AR.alloc("PWim", [5, 16], F32)
    P.tt("dve", PWre[:, 1, :], mag, c_, ALU.mult)
    P.tt("dve", PWim[:, 1, :], mag, s_, ALU.mult)

    cmT = [AR.alloc(f"cmT{i}", [16 * 32], F32) for i in range(4)]

    def cmul(eng, ore, oim, are, aim, bre, bim, shape_tmp):
        n = int(np.prod(shape_tmp))

        def view(b):
            v = b[:, 0:n]
            if len(shape_tmp) == 2:
                v = v.re("p (a b) -> p a b", a=shape_tmp[0])
            return v
        ta, tb, tc_, td = [view(b) for b in cmT]
        P.tt(eng, ta, are, bre, ALU.mult)
        P.tt(eng, tb, aim, bim, ALU.mult)
        P.tt(eng, tc_, are, bim, ALU.mult)
        P.tt(eng, td, aim, bre, ALU.mult)
        P.tt(eng, ore, ta, tb, ALU.subtract)
        P.tt(eng, oim, tc_, td, ALU.add)

    for kk in (2, 3, 4):
        cmul("dve", PWre[:, kk, :], PWim[:, kk, :], PWre[:, kk - 1, :], PWim[:, kk - 1, :],
             PWre[:, 1, :], PWim[:, 1, :], [16])
    nr = sm("nr")
    P.ts("dve", nr, PWre[:, 1, :], -1.0, None, op0=ALU.add)
    den = sm("den")
    P.tt("dve", t1, lamre, lamre, ALU.mult)
    P.tt("dve", t2, lamim, lamim, ALU.mult)
    P.tt("dve", den, t1, t2, ALU.add)
    rden = sm("rden")
    P.op("dve", (lambda h, o=A(rden), i=A(den): h.reciprocal(out=o, in_=i)), [rden], [den])
    cre = sm("cre")
    cim = sm("cim")
    t3 = sm("t3")
    t4 = sm("t4")
    P.tt("dve", t1, nr, lamre, ALU.mult)
    P.tt("dve", t2, PWim[:, 1, :], lamim, ALU.mult)
    P.tt("dve", t3, t1, t2, ALU.add)
    P.tt("dve", cre, t3, rden, ALU.mult)
    P.tt("dve", t1, PWim[:, 1, :], lamre, ALU.mult)
    P.tt("dve", t2, nr, lamim, ALU.mult)
    P.tt("dve", t4, t1, t2, ALU.subtract)
    P.tt("dve", cim, t4, rden, ALU.mult)
    bbre = AR.alloc("bbre", [16, 16], F32)
    bbim = AR.alloc("bbim", [16, 16], F32)
    cmul("dve", bbre, bbim, cre.un(2).bc([128, 16, 16]), cim.un(2).bc([128, 16, 16]), Bre, Bim, [16, 16])
    CT0re = AR.alloc("CT0re", [16, 2, 16], F32)
    CT0imn = AR.alloc("CT0imn", [16, 2, 16], F32)
    P.memset("pool", CT0re, 0.0)
    P.memset("pool", CT0imn, 0.0)
    P.copy("pool", CT0re[0:64, :, 0, :], CTre[0:64])
    P.copy("pool", CT0re[64:128, :, 1, :], CTre[64:128])
    P.ts("pool", CT0imn[0:64, :, 0, :], CTim[0:64], -1.0, None, op0=ALU.mult)
    P.ts("pool", CT0imn[64:128, :, 1, :], CTim[64:128], -1.0, None, op0=ALU.mult)
    Are = AR.alloc("Are", [8, 7, 2, 16], F32)
    Aim = AR.alloc("Aim", [8, 7, 2, 16], F32)
    xr = AR.alloc("xr", [16, 16], F32)
    xi = AR.alloc("xi", [16, 16], F32)
    for qh in range(2):
        qo = 8 * qh
        P.memset("pool", Are, 0.0)
        P.memset("pool", Aim, 0.0)
        for a in range(4):
            kk = 3 - a
            if kk == 0:
                sr_, si_ = bbre, bbim
            else:
                cmul("dve", xr, xi, PWre[:, kk, :].un(2).bc([128, 16, 16]), PWim[:, kk, :].un(2).bc([128, 16, 16]),
                     bbre, bbim, [16, 16])
                sr_, si_ = xr, xi
            for (dstA, src) in ((Are, sr_), (Aim, si_)):
                P.copy("pool", dstA[0:64, :, a, 0, :], src[0:64, qo:qo + 8, :])
                P.copy("pool", dstA[64:128, :, a, 1, :], src[64:128, qo:qo + 8, :])
        for (dst, srcA) in ((BexpRe, Are), (BexpIm, Aim)):
            for q4 in range(2):
                pb = bank()
                for lp in range(4):
                    ql = 4 * q4 + lp
                    P.tr(pb[:, lp * 128:(lp + 1) * 128], srcA[:, ql, 0:4, :, :].re("p a g h -> p (a g h)"), identf)
                P.copy("act", dst[:, qo + 4 * q4:qo + 4 * q4 + 4, :].re("p a b -> p (a b)"), pb[:, 0:512])
        for q4 in range(2):
            pb = bank()
            for lp in range(4):
                ql = 4 * q4 + lp
                q = qo + ql
                for tau in range(4):
                    a0 = 3 - tau
                    P.mm(pb[:, lp * 128 + tau * 32: lp * 128 + tau * 32 + 32],
                         [(Are[:, ql, a0:a0 + 4, :, :].re("p a g h -> p (a g h)"), CT0re[:, q, :, :].re("p g h -> p (g h)")),
                          (Aim[:, ql, a0:a0 + 4, :, :].re("p a g h -> p (a g h)"), CT0imn[:, q, :, :].re("p g h -> p (g h)"))])
            for lp in range(4):
                q = qo + 4 * q4 + lp
                P.stt(Kmat[:, q, :], identf, dexp[:, q:q + 1], pb[:, lp * 128:(lp + 1) * 128], ALU.mult, ALU.add)
    P.memset("pool", CexpA, 0.0)
    P.memset("pool", CexpB, 0.0)
    for tau in range(4):
        cmul("dve", xr, xi, PWre[:, tau + 1, :].un(2).bc([128, 16, 16]), PWim[:, tau + 1, :].un(2).bc([128, 16, 16]),
             CTre, CTim, [16, 16])
        P.copy("pool", CexpA[0:64, :, tau, 0, :], xr[0:64])
        P.copy("pool", CexpA[64:128, :, tau, 1, :], xr[64:128])
        P.ts("pool", CexpB[0:64, :, tau, 0, :], xi[0:64], -1.0, None, op0=ALU.mult)
        P.ts("pool", CexpB[64:128, :, tau, 1, :], xi[64:128], -1.0, None, op0=ALU.mult)
    rd = sm("rd")
    P.tt("dve", t1, PWre[:, 4, :], PWre[:, 4, :], ALU.mult)
    P.tt("dve", t2, PWim[:, 4, :], PWim[:, 4, :], ALU.mult)
    P.tt("dve", t3, t1, t2, ALU.add)
    P.act(dcol, t3, AF.Sqrt)
    P.op("dve", (lambda h, o=A(rd), i=A(dcol): h.reciprocal(out=o, in_=i)), [rd], [dcol])
    P.tt("dve", Tre[:, :, 0], PWre[:, 4, :], rd, ALU.mult)
    P.tt("dve", t1, PWim[:, 4, :], rd, ALU.mult)
    P.ts("dve", Tim[:, :, 0], t1, -1.0, None, op0=ALU.mult)
    m = 1
    while m < CH:
        ar = Tre[:, :, m - 1:m].bc([128, 16, m])
        ai = Tim[:, :, m - 1:m].bc([128, 16, m])
        cmul("dve", Tre[:, :, m:2 * m], Tim[:, :, m:2 * m], Tre[:, :, 0:m], Tim[:, :, 0:m], ar, ai, [16, m])
        m *= 2
    sraw = AR.alloc("sraw", [2048], F32, parts=16)
    for ri, nm in ((0, "sre"), (1, "sim")):
        P.dma("sp", sraw, I[nm])
        pb = bank()
        for q in range(16):
            P.tr(pb[:, q * 16:(q + 1) * 16], sraw[:, q * 128:(q + 1) * 128], identf[0:16, 0:16])
        P.copy("dve", h0sb[:, :, ri, :], pb[:, 0:256].re("p (q s) -> p q s", q=16))
    P.copy("dve", Hsh_s, h0sb)
    cmul("dve", rot0[:, :, 0, :], rot0[:, :, 1, :], PWre[:, 4, :].un(2).bc([128, 16, 16]), PWim[:, 4, :].un(2).bc([128, 16, 16]),
         h0sb[:, :, 0, :], h0sb[:, :, 1, :], [16, 16])
    dbg_out("Kmat", Kmat, [128, 16, 128], BF16)
    dbg_out("Tre", Tre, [128, 16, CH])
    dbg_out("PWre", PWre, [128, 5, 16])
    dbg_out("PWim", PWim, [128, 5, 16])
    AR.pop()
    P.barrier()
    if stop_after == "setup":
        return finish()

    xt = AR.alloc("xt", [D], F32)
    hT = AR.alloc("hT", [NT, NB], BF16)
    a_sb = AR.alloc("a_sb", [4, NB], F32)
    sgt = AR.alloc("sgt", [4, NB], F32)
    fullp = AR.alloc("fullp", [4, 30 + NB], F32)
    hist = AR.alloc("hist", [4, 30], F32)
    zsd = AR.alloc("zsd", [4, 4, CH], BF16)
    v_sb = AR.alloc("v_sb", [4, NB], F32)
    sq_sb = Buf(a_sb.ap, a_sb.key)
    mean_sb = AR.alloc("mean_sb", [NB], F32)
    var_sb = AR.alloc("var_sb", [NB], F32)
    rstd_sb = AR.alloc("rstd_sb", [NB], F32)
    vact = AR.alloc("vact", [4, NB], BF16)
    cvo = AR.alloc("cvo", [NT, NB], BF16)
    sso = AR.alloc("sso", [NT, NB], BF16)
    sgg2 = [AR.alloc(f"sgg{i}", [NB], BF16) for i in range(2)]
    S5B = []
    for i in range(2):
        S5B.append(dict(
            U=AR.alloc(f"U{i}", [4, CH], BF16), hl=AR.alloc(f"hl{i}", [4, 2, CH], F32),
            gb=AR.alloc(f"gb{i}", [4, 2, CH], F32), gg=AR.alloc(f"gg{i}", [4, 2, CH], F32),
            hfull=AR.alloc(f"hfull{i}", [4, 2, CH], F32), rt=[AR.alloc(f"rt{i}_{j}", [4, CH], F32) for j in range(4)],
            Hsh=AR.alloc(f"Hsh{i}", [4, 2, CH], BF16), yg=AR.alloc(f"yg{i}", [4, CH], BF16)))
    ygT = AR.alloc("ygT", [4, NB], BF16)
    sgc = [AR.alloc(f"sgc{i}", [NB], BF16) for i in range(2)]
    sgs = [AR.alloc(f"sgs{i}", [NB], BF16) for i in range(2)]
    m1 = [AR.alloc(f"m1{i}", [NB], F32) for i in range(2)]
    m2 = [AR.alloc(f"m2{i}", [NB], F32) for i in range(2)]
    htmp = AR.alloc("htmp", [NT, DS], F32)
    P.memset("pool", hist, 0.0)

    def win_chunk(c):
        return I["w_in"][:, c * 512:(c + 1) * 512].rearrange("(kt p) c -> p kt c", p=128)

    def w512(name):
        return I[name].rearrange("(kt p) c -> p kt c", p=128)

    for blk in range(NBLK + 1):
        samp = blk == NBLK
        N = DS if samp else NB
        C = N // 4
        tok0 = L if samp else blk * NB
        full = None if samp else fullp
        if not samp:
            for tt_ in range(NB // 128):
                P.dma("sp", xt, I["xp"][tok0 + tt_ * 128: tok0 + (tt_ + 1) * 128, :])
                for half in range(2):
                    pb = bank()
                    for d4 in range(4):
                        dt = half * 4 + d4
                        P.tr(pb[:, d4 * 128:(d4 + 1) * 128], xt[:, dt * 128:(dt + 1) * 128], identf)
                    for d4 in range(4):
                        dt = half * 4 + d4
                        P.act(hT[:, dt, tt_ * 128:(tt_ + 1) * 128], pb[:, d4 * 128:(d4 + 1) * 128], AF.Identity,
                              bias=modT[:, M_SH1 + dt, 0:1], scale=modT[:, M_SC1 + dt, 0:1])
        else:
            P.dma("sp", xt[0:DS, :], I["xs"])
            pb = bank()
            for dt in range(NT):
                P.tr(pb[:, dt * DS:(dt + 1) * DS], xt[0:DS, dt * 128:(dt + 1) * 128], identf[0:DS, 0:DS])
            P.tt("dve", htmp.re("p d (s t) -> p d s t", t=4), pb[:, 0:NT * DS].re("p (d s t) -> p d s t", d=NT, t=4),
                 modT[:, M_SC1:M_SC1 + 8, 1:NSEQ].un(3).bc([128, NT, 16, 4]), ALU.mult)
            P.tt("dve", hT[:, :, 0:DS].re("p d (s t) -> p d s t", t=4), htmp.re("p d (s t) -> p d s t", t=4),
                 modT[:, M_SH1:M_SH1 + 8, 1:NSEQ].un(3).bc([128, NT, 16, 4]), ALU.add)
        if blk == 0:
            dbg_out("hT0", hT, [128, NT, NB], BF16)
        for c in range(3):
            slot = ring_load(win_chunk(c))
            for half in range(2):
                pb = bank()
                for c2 in range(2):
                    ct = half * 2 + c2
                    P.mm(pb[:, c2 * NB:c2 * NB + N], [(slot[:, k, ct * 128:(ct + 1) * 128], hT[:, k, 0:N]) for k in range(NT)])
                for c2 in range(2):
                    ct = half * 2 + c2
                    src = pb[:, c2 * NB:c2 * NB + N]
                    bcol = colsT[:, C_BIN + c * 4 + ct:C_BIN + c * 4 + ct + 1]
                    if c == 0:
                        P.act(a_sb[:, ct, 0:N], src, AF.Identity, bias=bcol)
                    elif c == 1:
                        P.act(sgt[:, ct, 0:N], src, AF.Sigmoid, bias=bcol)
                    else:
                        P.act(zsd[:, ct, :, 0:C], src.re("p (c s) -> p s c", s=4), AF.Identity, bias=bcol)
        st_conv = P.begin()
        cur_set[0] = "conv"
        if not samp:
            P.copy("act", full[:, :, 0:30], hist)
            P.tt("dve", full[:, :, 30:30 + NB], a_sb, sgt, ALU.mult)
            if blk == NBLK - 1:
                pb = bank()
                for ct in range(4):
                    P.tr(pb[0:30, ct * 128:(ct + 1) * 128], full[:, ct, NB:NB + 30], identf)
                nco = AR.alloc("nco", [512], F32, parts=30)
                P.copy("dve", nco, pb[0:30, :])
                P.dma("sp", O["ncp"], nco, is_out=True)
        else:
            P.tt("dve", full_s[:, :, :, 30:34], a_sb[:, :, 0:DS].re("p c (s t) -> p c s t", t=4),
                 sgt[:, :, 0:DS].re("p c (s t) -> p c s t", t=4), ALU.mult)
            pb = bank()
            for ct in range(4):
                ucont = AR.alloc("ucont", [16, 4], F32) if ct == 0 else ucont
                P.copy("dve", ucont, full_s[:, ct, :, 30:34])
                P.tr(pb[0:DS, ct * 128:(ct + 1) * 128], ucont.re("p s t -> p (s t)"), identf)
            ncs_sb = AR.alloc("ncs_sb", [512], F32, parts=DS)
            P.copy("dve", ncs_sb, pb[0:DS, :])
            for s in range(16):
                P.dma("sp", O["ncs"][s, 26:30, :], ncs_sb[4 * s:4 * s + 4, :], is_out=True)
        for k in range(31):
            for ct in range(4):
                if samp:
                    src = full_s[:, ct, :, k:k + 4]
                    dst = v_sb[:, ct, 0:DS].re("p (s t) -> p s t", t=4)
                else:
                    src = full[:, ct, k:k + NB]
                    dst = v_sb[:, ct, :]
                wcol = wdwT[:, ct, k:k + 1]
                if k == 0:
                    P.ts("dve", dst, src, wcol, colsT[:, C_BDW + ct:C_BDW + ct + 1], op0=ALU.mult, op1=ALU.add)
                else:
                    P.stt(dst, src, wcol, dst, ALU.mult, ALU.add)
        if not samp:
            P.copy("act", hist, full[:, :, NB:NB + 30])
        P.act(sq_sb[:, :, 0:N], v_sb[:, :, 0:N], AF.Square)
        pb = bank()
        P.mm(pb[:, 0:N], [(onesf, v_sb[:, ct, 0:N]) for ct in range(4)])
        P.mm(pb[:, NB:NB + N], [(onesf, sq_sb[:, ct, 0:N]) for ct in range(4)])
        P.ts("dve", mean_sb[:, 0:N], pb[:, 0:N], 1.0 / 512, None, op0=ALU.mult)
        P.tt("dve", var_sb[:, 0:N], mean_sb[:, 0:N], mean_sb[:, 0:N], ALU.mult)
        P.stt(var_sb[:, 0:N], pb[:, NB:NB + N], 1.0 / 512, var_sb[:, 0:N], ALU.mult, ALU.subtract)
        P.act(rstd_sb[:, 0:N], var_sb[:, 0:N], AF.Sqrt, bias=epsc[:, 0:1])
        P.op("dve", (lambda h, o=A(rstd_sb[:, 0:N]), i=A(rstd_sb[:, 0:N]): h.reciprocal(out=o, in_=i)), [rstd_sb], [rstd_sb])
        P.tt("dve", v_sb[:, :, 0:N], v_sb[:, :, 0:N], mean_sb[:, 0:N].un(1).bc([128, 4, N]), ALU.subtract)
        P.tt("dve", v_sb[:, :, 0:N], v_sb[:, :, 0:N], rstd_sb[:, 0:N].un(1).bc([128, 4, N]), ALU.mult)
        for ct in range(4):
            P.act(vact[:, ct, 0:N], v_sb[:, ct, 0:N], AF.Silu, bias=colsT[:, C_CB + ct:C_CB + ct + 1],
                  scale=colsT[:, C_CG + ct:C_CG + ct + 1])
        slot = ring_load(w512("w_pw"), view4=True)
        for j2 in range(4):
            pb = bank()
            for c2 in range(2):
                j = 2 * j2 + c2
                P.mm(pb[:, c2 * NB:c2 * NB + N], [(slot[:, k, j * 128:(j + 1) * 128], vact[:, k, 0:N]) for k in range(4)])
            for c2 in range(2):
                j = 2 * j2 + c2
                P.act(cvo[:, j, 0:N], pb[:, c2 * NB:c2 * NB + N], AF.Identity, bias=colsT[:, C_BPW + j:C_BPW + j + 1])
        P.end()
        st_s5 = []
        for ct in range(4):
            st_s5.append(P.begin())
            cur_set[0] = "s5a" if ct % 2 == 0 else "s5b"
            SB = S5B[ct % 2]
            U, hl, gb, gg, hfull, rt, Hsh, yg = (SB["U"], SB["hl"], SB["gb"], SB["gg"], SB["hfull"], SB["rt"], SB["Hsh"], SB["yg"])
            cry = carry.k(ct)
            pb = bank()
            for lp in range(4):
                P.mm(pb[:, lp * CH:lp * CH + C], [(selu[:, lp * 4 + s, :], zsd[:, ct, s, 0:C]) for s in range(4)])
            P.copy("act", U[:, :, 0:C], pb[:, 0:4 * CH].re("p (a c) -> p a c", a=4)[:, :, 0:C])
            pb = bank()
            for lp in range(4):
                q = 4 * ct + lp
                P.mm(pb[:, (lp * 2) * CH:(lp * 2) * CH + C], [(BexpRe[:, q, :], U[:, lp, 0:C])])
                P.mm(pb[:, (lp * 2 + 1) * CH:(lp * 2 + 1) * CH + C], [(BexpIm[:, q, :], U[:, lp, 0:C])])
            P.copy("act", hl[:, :, :, 0:C], pb[:, 0:8 * CH].re("p (a r c) -> p a r c", a=4, r=2)[:, :, :, 0:C])
            qs = slice(4 * ct, 4 * ct + 4)
            if not samp:
                hre, him = hl[:, :, 0, :], hl[:, :, 1, :]
                tr_, ti_ = Tre[:, qs, :], Tim[:, qs, :]
                P.tt("dve", rt[0], hre, tr_, ALU.mult)
                P.tt("dve", rt[1], him, ti_, ALU.mult)
                P.tt("dve", rt[2], him, tr_, ALU.mult)
                P.tt("dve", rt[3], hre, ti_, ALU.mult)
                P.tt("dve", gb[:, :, 0, :], rt[0], rt[1], ALU.subtract)
                P.tt("dve", gb[:, :, 1, :], rt[2], rt[3], ALU.add)
                P.copy("act", Hsh[:, :, :, 0], cry[:, qs, :])
                for lp in range(4):
                    q = 4 * ct + lp
                    for ri in range(2):
                        P.scan(gg[:, lp, ri, :], dcol[:, q:q + 1].bc([128, CH]), gb[:, lp, ri, :], cry[:, q, ri:ri + 1])
                gre, gim = gg[:, :, 0, :], gg[:, :, 1, :]
                P.tt("dve", rt[0], gre, tr_, ALU.mult)
                P.tt("dve", rt[1], gim, ti_, ALU.mult)
                P.tt("dve", rt[2], gim, tr_, ALU.mult)
                P.tt("dve", rt[3], gre, ti_, ALU.mult)
                P.tt("dve", hfull[:, :, 0, :], rt[0], rt[1], ALU.add)
                P.tt("dve", hfull[:, :, 1, :], rt[2], rt[3], ALU.subtract)
                P.copy("act", Hsh[:, :, :, 1:CH], hfull[:, :, :, 0:CH - 1])
                P.copy("dve", cry[:, qs, :], hfull[:, :, :, CH - 1])
                hsrc = lambda lp, ri: Hsh[:, lp, ri, 0:C]
                if blk == 0 and ct in (0, 2):
                    dbg_out(f"hl{ct}", hl, [128, 4, 2, CH])
                    dbg_out(f"gb{ct}", gb, [128, 4, 2, CH])
                    dbg_out(f"gg{ct}", gg, [128, 4, 2, CH])
                    dbg_out(f"hfull{ct}", hfull, [128, 4, 2, CH])
                    dbg_out(f"dcol{ct}", dcol, [128, 16])
                    dbg_out(f"Tre{ct}", Tre, [128, 16, CH])
                    dbg_out(f"Tim{ct}", Tim, [128, 16, CH])
            else:
                P.tt("dve", hend_s[:, qs, :, :], rot0[:, qs, :, :], hl[:, :, :, 0:C], ALU.add)
                hsrc = lambda lp, ri: Hsh_s[:, 4 * ct + lp, ri, :]
            pb = bank()
            for lp in range(4):
                q = 4 * ct + lp
                P.mm(pb[:, lp * CH:lp * CH + C],
                     [(Kmat[:, q, :], U[:, lp, 0:C]),
                      (CexpA[:, q].re("p t g h -> p (t g h)"), hsrc(lp, 0)),
                      (CexpB[:, q].re("p t g h -> p (t g h)"), hsrc(lp, 1))])
            P.act(yg[:, :, 0:C], pb[:, 0:4 * CH].re("p (a c) -> p a c", a=4)[:, :, 0:C], AF.Gelu_apprx_tanh)
            pb = bank()
            for tau in range(4):
                P.mm(pb[:, tau * CH:tau * CH + C], [(sely[:, lp * 4 + tau, :], yg[:, lp, 0:C]) for lp in range(4)])
            P.copy("dve", ygT[:, ct, 0:N].re("p (c t) -> p t c", t=4).k(ct),
                   pb[:, 0:4 * CH].re("p (t c) -> p t c", t=4)[:, :, 0:C])
            P.end()
        def _merge(a, b_):
            out, ia, ib = [], 0, 0
            while ia < len(a) or ib < len(b_):
                if ib >= len(b_) or (ia < len(a) and ia * len(b_) <= ib * len(a)):
                    out.append(a[ia]); ia += 1
                else:
                    out.append(b_[ib]); ib += 1
            return out
        cur_set[0] = "all"
        slot_sv = ring_load(w512("w_sv"), view4=True)
        slot_sg = ring_load(w512("w_sg"), view4=True)
        P.replay([_merge(st_s5[0], st_s5[1]) + _merge(st_s5[2], st_s5[3]), st_conv])
        if blk == 0:
            dbg_out("ygT0", ygT, [128, 4, NB], BF16)
            dbg_out("cvo0", cvo, [128, NT, NB], BF16)
        for j in range(NT):
            pb = bank()
            ygk_ = [ygT.k(c4) for c4 in range(4)]
            P.mm(pb[:, 0:N], [(slot_sv[:, k, j * 128:(j + 1) * 128], ygT[:, k, 0:N]) for k in range(4)], extra_ins=ygk_)
            P.mm(pb[:, NB:NB + N], [(slot_sg[:, k, j * 128:(j + 1) * 128], ygT[:, k, 0:N]) for k in range(4)], extra_ins=ygk_)
            P.act(sgg2[j % 2][:, 0:N], pb[:, NB:NB + N], AF.Sigmoid, bias=colsT[:, C_BSG + j:C_BSG + j + 1])
            P.stt(sso[:, j, 0:N], pb[:, 0:N], colsT[:, C_BSV + j:C_BSV + j + 1], sgg2[j % 2][:, 0:N], ALU.add, ALU.mult)
        for half in range(2):
            slot_c = ring_load(win_chunk(3 + half))
            slot_s = ring_load(win_chunk(5 + half))
            for j4 in range(4):
                j = half * 4 + j4
                pb = bank()
                P.mm(pb[:, 0:N], [(slot_c[:, k, j4 * 128:(j4 + 1) * 128], hT[:, k, 0:N]) for k in range(NT)])
                P.mm(pb[:, NB:NB + N], [(slot_s[:, k, j4 * 128:(j4 + 1) * 128], hT[:, k, 0:N]) for k in range(NT)])
                b = j % 2
                P.act(sgc[b][:, 0:N], pb[:, 0:N], AF.Sigmoid, bias=colsT[:, C_BIN + 12 + j:C_BIN + 12 + j + 1])
                P.act(sgs[b][:, 0:N], pb[:, NB:NB + N], AF.Sigmoid, bias=colsT[:, C_BIN + 20 + j:C_BIN + 20 + j + 1])
                P.tt("dve", m1[b][:, 0:N], cvo[:, j, 0:N], sgc[b][:, 0:N], ALU.mult)
                P.tt("dve", m2[b][:, 0:N], sso[:, j, 0:N], sgs[b][:, 0:N], ALU.mult)
                P.tt("dve", merged[:, j, tok0:tok0 + N], m1[b][:, 0:N], m2[b][:, 0:N], ALU.add)
    pb = bank()
    for ri in range(2):
        P.op("pe", (lambda h, o=A(pb[0:16, ri * 128:(ri + 1) * 128]), i=A(carry[:, :, ri]), idn=A(identf): h.transpose(out=o, in_=i, identity=idn)),
             [pb], [identf, carry] + [carry.k(c4) for c4 in range(4)])
    fst = AR.alloc("fst", [256], F32, parts=16)
    P.copy("dve", fst, pb[0:16, 0:256])
    P.dma("sp", O["nrp"], fst[:, 0:128], is_out=True)
    P.dma("sp", O["nip"], fst[:, 128:256], is_out=True)
    for ri in range(2):
        for q4 in range(4):
            pb = bank()
            for lp in range(4):
                q = 4 * q4 + lp
                P.tr(pb[0:16, lp * 128:(lp + 1) * 128], hend_s[:, q, ri, :], identf)
            P.copy("dve", ring[ri].re("p a b -> p (a b)").cast(F32)[0:16, q4 * 512:(q4 + 1) * 512], pb[0:16, :])
    P.dma("sp", O["nrs"], ring[0].re("p a b -> p (a b)").cast(F32)[0:16, :], is_out=True)
    P.dma("sp", O["nis"], ring[1].re("p a b -> p (a b)").cast(F32)[0:16, :], is_out=True)
    dbg_out("merged", merged, [128, NT, NTOK], BF16)
    AR.pop()
    P.barrier()
    if stop_after == "A":
        return finish()

    I32 = mybir.dt.int32
    U32 = mybir.dt.uint32
    NSUB = 4
    G = 128 * NSUB
    NSLOT_T = (NTOK * 4) // G + 32
    NTILE = 17
    tiles = [(t * 128, 128) for t in range(16)] + [(L, DS)]
    h2d = Buf(nc.dram_tensor("h2d", [NTOK, D], BF16).ap(), "h2d")
    accd = Buf(nc.dram_tensor("accd", [NTOK, D], F32).ap(), "accd")
    Yd = Buf(nc.dram_tensor("Yd", [NSLOT_T * G, D], F32).ap(), "Yd")
    tokslot = Buf(nc.dram_tensor("tokslot", [NSLOT_T * G, 1], I32).ap(), "tokslot")

    def idma(out, in_, idx, gather, extra_reads=()):
        rec = P.dma("pool", out, in_, extra_reads=[idx] + list(extra_reads))
        o, i, ix = A(out), A(in_), A(idx)
        if gather:
            rec["fn"] = (lambda h: h.indirect_dma_start(out=o, out_offset=None, in_=i,
                                                        in_offset=bass.IndirectOffsetOnAxis(ap=ix, axis=0)))
        else:
            rec["fn"] = (lambda h: h.indirect_dma_start(out=o, out_offset=bass.IndirectOffsetOnAxis(ap=ix, axis=0),
                                                        in_=i, in_offset=None))
        return rec

    g2bc_p = AR.alloc("g2bc_p", [D], F32)
    g2bc_s = AR.alloc("g2bc_s", [D], F32)
    gates = AR.alloc("gates", [NTILE, 4], F32)
    slots_i = AR.alloc("slots_i", [NTILE, 4], I32)
    widx = AR.alloc("widx", [NSLOT_T, NT], I32)
    widx_g = AR.alloc("widx_g", [NSLOT_T, NT], I32)
    widx_u = AR.alloc("widx_u", [NSLOT_T, NT], I32)
    bidx = AR.alloc("bidx", [NSLOT_T], I32)
    iota32 = AR.alloc("iota32", [N_EXP], F32)
    tokid = AR.alloc("tokid", [NTILE], I32)
    P.dma("sp", iota32, I["k_iota32"])
    tokid_f = AR.alloc("tokid_f", [NTILE], F32)
    P.dma("sp", tokid_f, I["k_tokid"])
    P.copy("dve", tokid, tokid_f)

    def make_gbc(dsts, src3, m0):
        AR.push()
        modtm = AR.alloc("modtm", [D], F32, parts=NSEQ)
        selp = AR.alloc("selp", [128], F32, parts=NSEQ)
        sels = AR.alloc("sels", [DS], F32, parts=NSEQ)
        P.dma("sp", selp, I["k_selp"])
        P.dma("sp", sels, I["k_sels"])
        for h4 in range(2):
            pb = bank()
            for d4 in range(4):
                dt = h4 * 4 + d4
                P.tr(pb[0:NSEQ, d4 * 128:(d4 + 1) * 128], src3[:, m0 + dt, :], identf)
            P.copy("dve", modtm[:, h4 * 512:(h4 + 1) * 512], pb[0:NSEQ, :])
        for (dst, sel, R) in ((dsts[0], selp, 128), (dsts[1], sels, DS)):
            for hh in range(2):
                pb = bank()
                P.mm(pb[0:R, :], [(sel[:, 0:R], modtm[:, hh * 512:(hh + 1) * 512])])
                P.copy("act", dst[0:R, hh * 512:(hh + 1) * 512], pb[0:R, :])
        AR.pop()
        P.barrier()

    make_gbc((g2bc_p, g2bc_s), modT, M_G2)

    AR.push()
    oh4 = AR.alloc("oh4", [NTILE, 4, N_EXP], F32)
    pos_all = AR.alloc("pos_all", [NTILE, N_EXP], F32)
    g1bc_p = AR.alloc("g1bc_p", [D], F32)
    g1bc_s = AR.alloc("g1bc_s", [D], F32)
    Abc_p = AR.alloc("Abc_p", [D], F32)
    Abc_s = AR.alloc("Abc_s", [D], F32)
    Bvbc_p = AR.alloc("Bvbc_p", [D], F32)
    Bvbc_s = AR.alloc("Bvbc_s", [D], F32)
    make_gbc((g1bc_p, g1bc_s), modT, M_G1)
    make_gbc((Abc_p, Abc_s), A_T, 0)
    make_gbc((Bvbc_p, Bvbc_s), Bv_T, 0)
    l1g_bc = AR.alloc("l1g_bc", [D], F32)
    l1b_bc = AR.alloc("l1b_bc", [D], F32)
    for dst, nm in ((l1g_bc, "ln1_g"), (l1b_bc, "ln1_b")):
        P.dma("sp", dst, I[nm].partition_broadcast(128))
        P.ts("pool", dst, dst, float(DN_ALPHA), None, op0=ALU.mult)
    wr_bf = AR.alloc("wr_bf", [NT, N_EXP], BF16)
    br_row = AR.alloc("br_row", [N_EXP], F32, parts=1)
    b2_bf = AR.alloc("b2_bf", [D], BF16, parts=N_EXP)
    bout_row = AR.alloc("bout_row", [D], F32, parts=1)
    Ls = AR.alloc("Ls", [128], F32)
    base_row = AR.alloc("base_row", [N_EXP], F32, parts=1)
    P.dma("pool", wr_bf, I["w_router"].rearrange("(kt p) e -> p kt e", p=128))
    P.dma("sp", br_row, I["b_router"].rearrange("(o n) -> o n", o=1))
    P.dma("pool", b2_bf, I["b2"])
    P.dma("sp", bout_row, I["b_out"].rearrange("(o n) -> o n", o=1))
    P.dma("sp", Ls, I["k_ls"])
    P.memset("pool", base_row, 0.0)
    wout = AR.alloc("wout", [NT, D], BF16)
    P.dma("pool", wout, I["w_out"].rearrange("(kt p) c -> p kt c", p=128))
    xtb = [AR.alloc(f"xtb{i}", [D], F32) for i in range(2)]
    accb = [AR.alloc(f"accb{i}", [D], F32) for i in range(2)]
    h2b = [AR.alloc(f"h2b{i}", [D], BF16) for i in range(2)]
    BSETS = []
    for i in range(2):
        BSETS.append(dict(
            tsb=AR.alloc(f"tsb{i}", [D], F32), pre=AR.alloc(f"pre{i}", [D], F32), xn=AR.alloc(f"xn{i}", [D], F32),
            u1=None, h2f=None, h2Tt=AR.alloc(f"h2Tt{i}", [NT, 128], BF16),
            bst=AR.alloc(f"bst{i}", [2, 6], F32), mv=AR.alloc(f"mv{i}", [2], F32), rstd=AR.alloc(f"rstd{i}", [1], F32),
            nmr=AR.alloc(f"nmr{i}", [1], F32), lg=AR.alloc(f"lg{i}", [N_EXP], F32), mx8=AR.alloc(f"mx8{i}", [8], F32),
            ix8=AR.alloc(f"ix8{i}", [8], U32), ixf=AR.alloc(f"ixf{i}", [4], F32), nmx=AR.alloc(f"nmx{i}", [1], F32),
            ex4=AR.alloc(f"ex4{i}", [4], F32), ssum=AR.alloc(f"ssum{i}", [1], F32), cmb3=AR.alloc(f"cmb3{i}", [4, N_EXP], F32),
            comb=AR.alloc(f"comb{i}", [N_EXP], F32), Mk=AR.alloc(f"Mk{i}", [N_EXP], F32),
            combT=AR.alloc(f"combT{i}", [128], BF16, parts=N_EXP)))
    esel = AR.alloc("esel", [NTILE, NTILE], F32)
    selt = AR.alloc("selt", [NTILE, 128], F32, parts=NTILE)
    P.dma("sp", esel.re("p a b -> p (a b)"), I["k_esel"])
    P.dma("sp", selt.re("p a b -> p (a b)"), I["k_selt"])
    cnt_ps = PS[7]
    bank_sets["b0"] = [0, 1, 2]
    bank_sets["b1"] = [3, 4, 5, 6]
    bank_ctr["b0"] = 0
    bank_ctr["b1"] = 0
    b_streams = []
    for ti, (g0, R) in enumerate(tiles):
        BS = BSETS[ti % 2]
        (tsb, pre, xn, u1, h2f, h2Tt, bst, mv, rstd, nmr, lg, mx8, ix8, ixf, nmx, ex4, ssum, cmb3, comb, Mk, combT) = (
            BS["tsb"], BS["pre"], BS["xn"], BS["u1"], BS["h2f"], BS["h2Tt"], BS["bst"], BS["mv"], BS["rstd"], BS["nmr"],
            BS["lg"], BS["mx8"], BS["ix8"], BS["ixf"], BS["nmx"], BS["ex4"], BS["ssum"], BS["cmb3"], BS["comb"], BS["Mk"],
            BS["combT"])
        b_streams.append(P.begin())
        cur_set[0] = "b0" if ti % 2 == 0 else "b1"
        samp = g0 >= L
        xsrc = I["xs"] if samp else I["xp"][g0:g0 + R, :]
        xb = xtb[ti % 2]
        P.dma("sp", xb[0:R, :], xsrc)
        g1bc = g1bc_s if samp else g1bc_p
        g2bc = g2bc_s if samp else g2bc_p
        Abc = Abc_s if samp else Abc_p
        Bvbc = Bvbc_s if samp else Bvbc_p
        for hh in range(2):
            pb = bank()
            o = A(pb[0:R, :])
            prs = [(A(merged[:, k, g0:g0 + R]), A(wout[:, k, hh * 512:(hh + 1) * 512])) for k in range(NT)]
            l1, r1 = A(onesf[0:1, 0:R]), A(bout_row[0:1, hh * 512:(hh + 1) * 512])

            def fn(h, o=o, prs=prs, l1=l1, r1=r1):
                for i, (l, r) in enumerate(prs):
                    h.matmul(o, lhsT=l, rhs=r, start=(i == 0), stop=False)
                return h.matmul(o, lhsT=l1, rhs=r1, start=False, stop=True)
            P.op("pe", fn, [pb], [merged, wout, onesf, bout_row])
            P.tt("dve", tsb[0:R, hh * 512:(hh + 1) * 512], pb[0:R, :], g1bc[0:R, hh * 512:(hh + 1) * 512], ALU.mult)
        P.stt(pre[0:R, :], xb[0:R, :], float(DN_ALPHA), tsb[0:R, :], ALU.mult, ALU.add)
        for hh in range(2):
            P.op("dve", (lambda h, o=A(bst[0:R, hh, :]), i=A(pre[0:R, hh * 512:(hh + 1) * 512]): h.bn_stats(out=o, in_=i)),
                 [bst.k(hh)], [pre])
        P.op("dve", (lambda h, o=A(mv[0:R, :]), i=A(bst[0:R].re("p a b -> p (a b)")): h.bn_aggr(out=o, in_=i)),
             [mv], [bst.k(0), bst.k(1)])
        P.act(rstd[0:R, :], mv[0:R, 1:2], AF.Sqrt, bias=epsc[0:R, 0:1])
        P.op("dve", (lambda h, o=A(rstd[0:R, :]), i=A(rstd[0:R, :]): h.reciprocal(out=o, in_=i)), [rstd], [rstd])
        P.stt(nmr[0:R, :], mv[0:R, 0:1], -1.0, rstd[0:R, :], ALU.mult, ALU.mult)
        P.act(xn[0:R, :], pre[0:R, :], AF.Identity, bias=nmr[0:R, 0:1], scale=rstd[0:R, 0:1])
        ab = accb[ti % 2]
        P.tt("dve", ab[0:R, :], xn[0:R, :], l1g_bc[0:R, :], ALU.mult)
        P.tt("dve", ab[0:R, :], ab[0:R, :], l1b_bc[0:R, :], ALU.add)
        hb_ = h2b[ti % 2]
        P.tt("dve", pre[0:R, :], xn[0:R, :], Abc[0:R, :], ALU.mult)
        P.tt("dve", hb_[0:R, :], pre[0:R, :], Bvbc[0:R, :], ALU.add)
        P.dma("sp", h2d[g0:g0 + R, :].k(ti), hb_[0:R, :])
        pb = bank()
        pbb = pb.cast(BF16)
        for dt in range(NT):
            P.tr(pbb[:, dt * 128:dt * 128 + R], hb_[0:R, dt * 128:(dt + 1) * 128], identb[0:R, 0:R])
        P.copy("act", h2Tt[:, :, 0:R], pbb[:, 0:NT * 128].re("p (d t) -> p d t", d=NT)[:, :, 0:R])
        pb = bank()
        o = A(pb[0:R, 0:N_EXP])
        prs = [(A(h2Tt[:, k, 0:R]), A(wr_bf[:, k, :])) for k in range(NT)]
        l1, r1 = A(onesf[0:1, 0:R]), A(br_row[0:1, :])

        def fn(h, o=o, prs=prs, l1=l1, r1=r1):
            for i, (l, r) in enumerate(prs):
                h.matmul(o, lhsT=l, rhs=r, start=(i == 0), stop=False)
            return h.matmul(o, lhsT=l1, rhs=r1, start=False, stop=True)
        P.op("pe", fn, [pb], [h2Tt, wr_bf, onesf, br_row])
        P.copy("dve", lg[0:R, :], pb[0:R, 0:N_EXP])
        P.op("dve", (lambda h, o=A(mx8[0:R, :]), i=A(lg[0:R, :]): h.max(out=o, in_=i)), [mx8], [lg])
        P.op("dve", (lambda h, o=A(ix8[0:R, :]), m=A(mx8[0:R, :]), i=A(lg[0:R, :]): h.max_index(out=o, in_max=m, in_values=i)),
             [ix8], [mx8, lg])
        P.copy("dve", ixf[0:R, :], ix8[0:R, 0:4])
        P.ts("dve", nmx[0:R, :], mx8[0:R, 0:1], -1.0, None, op0=ALU.mult)
        P.act(ex4[0:R, :], mx8[0:R, 0:4], AF.Exp, bias=nmx[0:R, 0:1])
        P.op("dve", (lambda h, o=A(ssum[0:R, :]), i=A(ex4[0:R, :]): h.reduce_sum(out=o, in_=i, axis=mybir.AxisListType.X)),
             [ssum], [ex4])
        P.op("dve", (lambda h, o=A(ssum[0:R, :]), i=A(ssum[0:R, :]): h.reciprocal(out=o, in_=i)), [ssum], [ssum])
        P.ts("dve", gates[0:R, ti, :].k(ti), ex4[0:R, :], ssum[0:R, 0:1], None, op0=ALU.mult)
        P.tt("dve", oh4[0:R, ti, :, :].k(ti), iota32[0:R, :].un(1).bc([R, 4, N_EXP]),
             ixf[0:R, :].un(2).bc([R, 4, N_EXP]), ALU.is_equal)
        P.tt("dve", cmb3[0:R], oh4[0:R, ti, :, :].k(ti), gates[0:R, ti, :].k(ti).un(2).bc([R, 4, N_EXP]), ALU.mult)
        P.op("dve", (lambda h, o=A(comb[0:R, :]), i=A(cmb3[0:R].re("p k e -> p e k")): h.reduce_sum(out=o, in_=i, axis=mybir.AxisListType.X)),
             [comb], [cmb3])
        P.op("dve", (lambda h, o=A(Mk[0:R, :]), i=A(oh4[0:R, ti, :, :].re("p k e -> p e k")): h.reduce_sum(out=o, in_=i, axis=mybir.AxisListType.X)),
             [Mk], [oh4.k(ti)])
        pb = bank()
        P.mm(pb[0:R, 0:N_EXP], [(Ls[0:R, 0:R], Mk[0:R, :])])
        P.copy("act", pos_all[0:R, ti, :].k(ti), pb[0:R, 0:N_EXP])
        o_, l_, r_ = A(cnt_ps[0:NTILE, 0:N_EXP]), A(esel[0:R, ti, :]), A(Mk[0:R, :])
        P.op("pe", (lambda h, o_=o_, l_=l_, r_=r_, first=(ti == 0), last=(ti == NTILE - 1):
                    h.matmul(o_, lhsT=l_, rhs=r_, start=first, stop=last)), [cnt_ps], [esel, Mk])
        pb = bank()
        P.tr(pb[0:N_EXP, 0:R], comb[0:R, :], identf[0:R, 0:R])
        P.copy("act", combT[:, 0:R], pb[0:N_EXP, 0:R])
        for hh in range(2):
            pb = bank()
            P.mm(pb[0:R, :], [(combT[:, 0:R], b2_bf[:, hh * 512:(hh + 1) * 512])])
            P.tt("dve", tsb[0:R, hh * 512:(hh + 1) * 512], pb[0:R, :], g2bc[0:R, hh * 512:(hh + 1) * 512], ALU.mult)
        P.tt("dve", ab[0:R, :], ab[0:R, :], tsb[0:R, :], ALU.add)
        P.dma("sp", accd[g0:g0 + R, :].k(ti), ab[0:R, :])
        P.end()
        cur_set[0] = "all"
        if ti % 2 == 1 or ti == NTILE - 1:
            P.replay(b_streams)
            b_streams = []
    bank_sets["all"] = [0, 1, 2, 3, 4, 5, 6]
    cnt_all = AR.alloc("cnt_all", [N_EXP], F32, parts=NTILE)
    P.copy("dve", cnt_all, cnt_ps[0:NTILE, 0:N_EXP])
    pb = bank()
    P.mm(pb[0:NTILE, 0:N_EXP], [(Ls[0:NTILE, 0:NTILE], cnt_all)])
    P.mm(pb[0:1, 64:64 + N_EXP], [(onesf[0:NTILE, 0:1], cnt_all)])
    base_all = AR.alloc("base_all", [N_EXP], F32, parts=NTILE)
    P.copy("dve", base_all, pb[0:NTILE, 0:N_EXP])
    P.copy("dve", base_row, pb[0:1, 64:64 + N_EXP])
    for ti, (g0, R) in enumerate(tiles):
        pb = bank()
        P.mm(pb[0:R, 0:N_EXP], [(selt[:, ti, 0:R], base_all)])
        P.tt("dve", pos_all[0:R, ti, :].k(ti), pos_all[0:R, ti, :].k(ti), pb[0:R, 0:N_EXP], ALU.add)
    bank_sets["all"] = list(range(8))
    NE = N_EXP
    qrow = AR.alloc("qrow", [NE], F32, parts=1)
    tmpr = AR.alloc("tmpr", [NE], F32, parts=1)
    incl = AR.alloc("incl", [NE], F32, parts=1)
    strt = AR.alloc("strt", [NE], F32, parts=1)
    onesr = AR.alloc("onesr", [NE], F32, parts=1)
    P.memset("pool", onesr, 1.0)
    P.ts("dve", qrow, base_row, 0.0, None, op0=ALU.is_gt)
    for j in range(1, NTOK // G + 1):
        P.ts("dve", tmpr, base_row, float(G * j), None, op0=ALU.is_gt)
        P.tt("dve", qrow, qrow, tmpr, ALU.add)
    P.ts("dve", qrow, qrow, float(G), None, op0=ALU.mult)
    P.scan(incl, onesr, qrow, onesr[0:1, 0:1].k("z") if False else 0.0)
    P.tt("dve", strt, incl, qrow, ALU.subtract)
    svals = AR.alloc("svals", [NSLOT_T], F32, parts=1)
    P.dma("sp", svals, I["k_svals"])
    cmpb = AR.alloc("cmpb", [NSLOT_T, NE], F32, parts=1)
    P.tt("dve", cmpb, incl[0:1, :].un(1).bc([1, NSLOT_T, NE]), svals[0:1, :].un(2).bc([1, NSLOT_T, NE]), ALU.is_le)
    etile = AR.alloc("etile", [NSLOT_T], F32, parts=1)
    P.op("dve", (lambda h, o=A(etile), i=A(cmpb): h.reduce_sum(out=o, in_=i, axis=mybir.AxisListType.X)), [etile], [cmpb])
    P.ts("dve", etile, etile, float(NE - 1), None, op0=ALU.min)
    pb = bank()
    P.mm(pb[:, 0:NE], [(onesf[0:1, :], strt[0:1, :])])
    P.mm(pb[:, 64:64 + NSLOT_T], [(onesf[0:1, :], etile[0:1, :])])
    start_bc = AR.alloc("start_bc", [NE], F32)
    etile_bc = AR.alloc("etile_bc", [NSLOT_T], F32)
    P.copy("dve", start_bc, pb[:, 0:NE])
    P.copy("dve", etile_bc, pb[:, 64:64 + NSLOT_T])
    iotaK = AR.alloc("iotaK", [NT], F32)
    P.dma("sp", iotaK, I["k_iotak"])
    wf = AR.alloc("wf", [NSLOT_T, NT], F32)
    e1k = AR.alloc("e1k", [NSLOT_T], F32)
    P.ts("dve", e1k, etile_bc, 1024.0, None, op0=ALU.mult)
    P.tt("dve", wf, e1k.un(2).bc([128, NSLOT_T, NT]), iotaK.un(1).bc([128, NSLOT_T, NT]), ALU.add)
    P.copy("dve", widx, wf)
    wf2 = AR.alloc("wf2", [NSLOT_T, NT], F32)
    P.ts("dve", wf2, wf, 2.0, None, op0=ALU.mult)
    P.copy("dve", widx_g, wf2)
    P.ts("dve", wf2, wf2, 1.0, None, op0=ALU.add)
    P.copy("dve", widx_u, wf2)
    bf_ = AR.alloc("bf_", [NSLOT_T], F32)
    P.ts("dve", bf_, etile_bc, 128.0, iotaK[:, 0:1], op0=ALU.mult, op1=ALU.add)
    P.copy("dve", bidx, bf_)
    sfull = AR.alloc("sfull", [NTILE, NE], F32)
    slf = AR.alloc("slf", [NTILE, 4], F32)
    zero_i = AR.alloc("zero_i", [NSLOT_T * NSUB], I32)
    P.memset("pool", zero_i, 0)
    P.dma("sp", tokslot.re("(p j) o -> p (j o)", p=128).k("init"), zero_i)
    allk = [pos_all.k(ti) for ti in range(NTILE)] + [oh4.k(ti) for ti in range(NTILE)]
    P.memset("pool", pos_all[64:128, NTILE - 1, :].k(NTILE - 1), 0.0)
    P.memset("pool", oh4[64:128, NTILE - 1, :, :].k(NTILE - 1), 0.0)
    P.op("dve", (lambda h, o=A(sfull), a=A(pos_all), c=A(start_bc.un(1).bc([128, NTILE, NE])):
                 h.tensor_tensor(out=o, in0=a, in1=c, op=ALU.add)), [sfull], [start_bc] + allk)
    ohk = [oh4.k(ti) for ti in range(NTILE)]
    for k in range(4):
        P.op("dve", (lambda h, o=A(oh4[:, :, k, :]), a=A(oh4[:, :, k, :]), c=A(sfull):
                     h.tensor_tensor(out=o, in0=a, in1=c, op=ALU.mult)), [oh4] + ohk, [sfull] + allk)
    P.op("dve", (lambda h, o=A(slf.re("p t k -> p (t k)")), i=A(oh4.re("p t k e -> p (t k) e")):
                 h.reduce_sum(out=o, in_=i, axis=mybir.AxisListType.X)), [slf], [oh4] + ohk)
    P.op("dve", (lambda h, o=A(slots_i), i=A(slf): h.tensor_copy(out=o, in_=i)),
         [slots_i] + [slots_i.k(ti) for ti in range(NTILE)], [slf])
    sc_keys = []
    for ti, (g0, R) in enumerate(tiles):
        for k in range(4):
            kk = tokslot.k(f"s{ti}_{k}")
            idma(kk, tokid[0:R, ti:ti + 1], slots_i[0:R, ti, k:k + 1].k(ti), gather=False, extra_reads=[tokslot.k("init")])
            sc_keys.append(kk)
    dbg_out("slots", slots_i, [128, NTILE, 4], I32)
    dbg_out("gates", gates, [128, NTILE, 4])
    dbg_out("etile", etile, [1, NSLOT_T])
    AR.pop()
    P.barrier()
    if stop_after == "B":
        return finish()

    AR.limit = AR.n
    AR.push()
    NWR = 6
    wring = [AR.alloc(f"wring{i}", [NT, D], BF16) for i in range(NWR)]
    wix = [0]
    w1rows = I["w1"].rearrange("e k (h f) -> (e k h) f", h=2)
    w2rows = I["w2"].rearrange("e k f -> (e k) f")

    def wgather(rows_ap, s, wi):
        slot = wring[wix[0] % NWR]
        wix[0] += 1
        for kt in range(NT):
            idma(slot[:, kt, :].k(kt), rows_ap, wi[:, s, kt:kt + 1], gather=True)
        return slot

    XT = [AR.alloc(f"XT{i}", [NT, G], BF16) for i in range(2)]
    xg = [AR.alloc(f"xg{i}", [NSUB, D], BF16) for i in range(2)]
    actT = [AR.alloc(f"actT{i}", [NT, G], BF16) for i in range(2)]
    gc = [AR.alloc(f"gc{i}", [G], F32) for i in range(2)]
    sgm = [AR.alloc(f"sgm{i}", [G], F32) for i in range(2)]
    uc = [AR.alloc(f"uc{i}", [G], F32) for i in range(2)]
    tg = [AR.alloc(f"tg{i}", [G], F32) for i in range(2)]
    ysb = [AR.alloc(f"ysb{i}", [D], F32) for i in range(2)]
    tsl = [AR.alloc(f"tsl{i}", [NSUB], I32) for i in range(2)]
    b1c = [AR.alloc(f"b1c{i}", [16], F32) for i in range(2)]
    h2keys = [h2d.k(ti) for ti in range(NTILE)]
    ykeys = []
    loaded = {}

    def issue_load(s):
        b = s % 2
        P.dma("sp", tsl[b], tokslot[s * G:(s + 1) * G, :].re("(p sub) o -> p (sub o)", sub=NSUB), extra_reads=sc_keys)
        for sub in range(NSUB):
            idma(xg[b][:, sub, :].k(sub), h2d, tsl[b][:, sub:sub + 1], gather=True, extra_reads=h2keys)
        idma(b1c[b], I["b1l"], bidx[:, s:s + 1], gather=True)
        loaded[s] = (wgather(w1rows, s, widx_g), wgather(w1rows, s, widx_u), wgather(w2rows, s, widx))

    issue_load(0)
    for s in range(NSLOT_T):
        b = s % 2
        wg_, wu_, w2_ = loaded.pop(s)
        for dt in range(NT):
            pb = bank()
            pbb = pb.cast(BF16)
            for sub in range(NSUB):
                P.tr(pbb[:, sub * 128:(sub + 1) * 128], xg[b][:, sub, dt * 128:(dt + 1) * 128].k(sub), identb)
            P.copy("act" if dt % 2 else "dve", XT[b][:, dt, :], pbb[:, 0:G])
        if s + 1 < NSLOT_T:
            issue_load(s + 1)
        wgk = [wg_.k(kt) for kt in range(NT)]
        wuk = [wu_.k(kt) for kt in range(NT)]
        w2k = [w2_.k(kt) for kt in range(NT)]
        P.ts("dve", b1c[b][:, 8:16], b1c[b][:, 8:16], 1.0, None, op0=ALU.add)
        aT = actT[b]
        for jj in range(NT):
            bb = jj % 2
            pg = bank()
            P.mm(pg[:, 0:G], [(wg_[:, k, jj * 128:(jj + 1) * 128], XT[b][:, k, :]) for k in range(NT)], extra_ins=wgk)
            pu = bank()
            P.mm(pu[:, 0:G], [(wu_[:, k, jj * 128:(jj + 1) * 128], XT[b][:, k, :]) for k in range(NT)], extra_ins=wuk)
            P.ts("dve", gc[bb], pg[:, 0:G], b1c[b][:, jj:jj + 1], 7.0, op0=ALU.add, op1=ALU.min)
            P.act(sgm[bb], gc[bb], AF.Sigmoid, scale=1.702)
            P.ts("dve", uc[bb], pu[:, 0:G], b1c[b][:, 8 + jj:9 + jj], 8.0, op0=ALU.add, op1=ALU.min)
            P.tt("dve", tg[bb], gc[bb], sgm[bb], ALU.mult)
            P.stt(aT[:, jj, :], uc[bb], -6.0, tg[bb], ALU.max, ALU.mult)
        for sub in range(NSUB):
            yb_ = ysb[sub % 2]
            for hh in range(2):
                pb = bank()
                P.mm(pb[:, :], [(aT[:, k, sub * 128:(sub + 1) * 128], w2_[:, k, hh * 512:(hh + 1) * 512]) for k in range(NT)],
                     extra_ins=w2k)
                P.copy("act", yb_[:, hh * 512:(hh + 1) * 512], pb[:, :])
            yk_ = Yd.k(f"{s}_{sub}")
            P.dma("sp", Buf(A(Yd)[s * G:(s + 1) * G, :].rearrange("(p sub) d -> p sub d", sub=NSUB)[:, sub, :], yk_.key), yb_)
            ykeys.append(yk_)
    AR.pop()
    P.barrier()

    AR.push()
    l2g_bc = AR.alloc("l2g_bc", [D], F32)
    l2b_bc = AR.alloc("l2b_bc", [D], F32)
    P.dma("sp", l2g_bc, I["ln2_g"].partition_broadcast(128))
    P.dma("sp", l2b_bc, I["ln2_b"].partition_broadcast(128))
    bst = AR.alloc("bst2", [2, 6], F32)
    mv = AR.alloc("mv2", [2], F32)
    rstd = AR.alloc("rstd2", [1], F32)
    nmr = AR.alloc("nmr2", [1], F32)
    accs = [AR.alloc(f"accs{i}", [D], F32) for i in range(2)]
    ygk2 = [[AR.alloc(f"ygk{j}_{i}", [D], F32) for i in range(4)] for j in range(2)]
    tq = [AR.alloc(f"tq{i}", [D], F32) for i in range(2)]
    yb = [AR.alloc(f"yb{i}", [D], F32) for i in range(2)]
    y2 = [AR.alloc(f"y2{i}", [D], F32) for i in range(2)]
    for ti, (g0, R) in enumerate(tiles):
        g2bc = g2bc_s if g0 >= L else g2bc_p
        av = accs[ti % 2]
        ygk = ygk2[ti % 2]
        P.dma("sp", av[0:R, :], accd[g0:g0 + R, :].k(ti))
        for k in range(4):
            idma(ygk[k][0:R, :], Yd, slots_i[0:R, ti, k:k + 1].k(ti), gather=True, extra_reads=ykeys)
        t_ = tq[ti % 2]
        P.ts("dve", t_[0:R, :], ygk[0][0:R, :], gates[0:R, ti, 0:1].k(ti), None, op0=ALU.mult)
        for k in range(1, 4):
            P.stt(t_[0:R, :], ygk[k][0:R, :], gates[0:R, ti, k:k + 1].k(ti), t_[0:R, :], ALU.mult, ALU.add)
        P.tt("dve", t_[0:R, :], t_[0:R, :], g2bc[0:R, :], ALU.mult)
        P.tt("dve", av[0:R, :], av[0:R, :], t_[0:R, :], ALU.add)
        for hh in range(2):
            P.op("dve", (lambda h, o=A(bst[0:R, hh, :]), i=A(av[0:R, hh * 512:(hh + 1) * 512]): h.bn_stats(out=o, in_=i)),
                 [bst.k(hh)], [av])
        P.op("dve", (lambda h, o=A(mv[0:R, :]), i=A(bst[0:R].re("p a b -> p (a b)")): h.bn_aggr(out=o, in_=i)),
             [mv], [bst.k(0), bst.k(1)])
        P.act(rstd[0:R, :], mv[0:R, 1:2], AF.Sqrt, bias=epsc[0:R, 0:1])
        P.op("dve", (lambda h, o=A(rstd[0:R, :]), i=A(rstd[0:R, :]): h.reciprocal(out=o, in_=i)), [rstd], [rstd])
        P.stt(nmr[0:R, :], mv[0:R, 0:1], -1.0, rstd[0:R, :], ALU.mult, ALU.mult)
        y_ = yb[ti % 2]
        z_ = y2[ti % 2]
        P.act(y_[0:R, :], av[0:R, :], AF.Identity, bias=nmr[0:R, 0:1], scale=rstd[0:R, 0:1])
        P.tt("dve", z_[0:R, :], y_[0:R, :], l2g_bc[0:R, :], ALU.mult)
        P.tt("dve", z_[0:R, :], z_[0:R, :], l2b_bc[0:R, :], ALU.add)
        dst = O["ys"] if g0 >= L else O["yp"][g0:g0 + R, :]
        P.dma("sp", dst, z_[0:R, :], is_out=True)
    AR.pop()
    print("arena peak bytes", AR.peak, "ops", {e: len(P.ops[e]) for e in ENGS})
    return finish()


def _consts():
    ident = np.eye(128, dtype=np.float32)
    selu = np.zeros((128, 16, 128), np.float32)
    sely = np.zeros((128, 16, 128), np.float32)
    for lp in range(4):
        for s in range(4):
            for r in range(32):
                selu[lp * 32 + r, lp * 4 + s, s * 32 + r] = 1.0
                sely[s * 32 + r, lp * 4 + s, lp * 32 + r] = 1.0
    selp = np.zeros((NSEQ, 128), np.float32)
    selp[0, :] = 1.0
    sels = np.zeros((NSEQ, DS), np.float32)
    for s in range(16):
        sels[1 + s, 4 * s:4 * s + 4] = 1.0
    ls = np.triu(np.ones((128, 128), np.float32), 1)
    iota32 = np.tile(np.arange(32, dtype=np.float32)[None, :], (128, 1))
    tokid = np.zeros((128, 17), np.float32)
    for ti in range(16):
        tokid[:, ti] = ti * 128 + np.arange(128)
    tokid[:, 16] = 2048 + np.arange(128)
    svals = (512.0 * np.arange(48, dtype=np.float32))[None, :]
    iotak = (128.0 * np.arange(8, dtype=np.float32))[None, :] + np.arange(128, dtype=np.float32)[:, None]
    iotae = np.arange(32, dtype=np.float32)[:, None]
    esel = np.zeros((128, 17, 17), np.float32)
    selt = np.zeros((17, 17, 128), np.float32)
    for ti in range(17):
        esel[:, ti, ti] = 1.0
        selt[ti, ti, :] = 1.0
    return dict(k_ident=ident, k_selu=selu.reshape(128, -1), k_sely=sely.reshape(128, -1), k_selp=selp, k_sels=sels,
                k_ls=ls, k_iota32=iota32, k_tokid=tokid, k_svals=svals, k_iotak=iotak, k_iotae=iotae,
                k_esel=esel.reshape(128, -1), k_selt=selt.reshape(17, -1))


_NC_CACHE = {}


def make_in_maps(inputs):
    f = lambda a: np.ascontiguousarray(np.asarray(a, dtype=np.float32))
    consts = _consts()
    shared = {}
    for nm in ("w_ada", "b_ada", "w_in", "b_in", "w_dw", "b_dw", "conv_ln_g", "conv_ln_b", "w_pw", "b_pw",
               "lam_re", "lam_im", "log_dt", "b_re", "b_im", "c_re", "c_im", "d_skip", "w_sv", "b_sv", "w_sg", "b_sg",
               "w_out", "b_out", "ln1_g", "ln1_b", "w_router", "b_router", "w1", "w2", "b2", "ln2_g", "ln2_b"):
        shared[nm] = f(inputs[nm][0])
    shared["b1l"] = np.ascontiguousarray(f(inputs["b1"][0]).reshape(32, 16, 128).transpose(0, 2, 1).reshape(32 * 128, 16))
    shared.update(consts)
    xp, xs = f(inputs["x_prompt"]), f(inputs["x_sample"])
    cp, cs = f(inputs["c_prompt"]), f(inputs["c_sample"])
    sc, sr, si = f(inputs["state_conv"][0]), f(inputs["state_ssm_re"][0]), f(inputs["state_ssm_im"][0])
    maps = []
    for i in range(8):
        m = dict(shared)
        m["xp"] = xp[i]
        m["xs"] = np.ascontiguousarray(xs[16 * i:16 * i + 16].reshape(DS, D))
        m["cc"] = np.ascontiguousarray(np.concatenate([cp[i:i + 1], cs[16 * i:16 * i + 16]], axis=0))
        m["sconv"] = np.ascontiguousarray(sc[16 * i:16 * i + 16])
        m["sre"] = np.ascontiguousarray(sr[16 * i:16 * i + 16].reshape(16, 2048))
        m["sim"] = np.ascontiguousarray(si[16 * i:16 * i + 16].reshape(16, 2048))
        maps.append(m)
    return maps


def assemble(results):
    yp = np.stack([np.asarray(r["yp"], np.float32) for r in results], 0)
    ys = np.concatenate([np.asarray(r["ys"], np.float32).reshape(16, 4, D) for r in results], 0)
    ncp = np.stack([np.asarray(r["ncp"], np.float32) for r in results], 0)[None]
    nrp = np.stack([np.asarray(r["nrp"], np.float32).reshape(32, 64) for r in results], 0)[None]
    nip = np.stack([np.asarray(r["nip"], np.float32).reshape(32, 64) for r in results], 0)[None]
    ncs = np.concatenate([np.asarray(r["ncs"], np.float32) for r in results], 0)[None]
    nrs = np.concatenate([np.asarray(r["nrs"], np.float32).reshape(16, 32, 64) for r in results], 0)[None]
    nis = np.concatenate([np.asarray(r["nis"], np.float32).reshape(16, 32, 64) for r in results], 0)[None]
    return (yp, ys, ncp, nrp, nip, ncs, nrs, nis)


def kernel(**inputs):
    if "nc" not in _NC_CACHE:
        _NC_CACHE["nc"] = build_program()
    nc = _NC_CACHE["nc"]
    maps = make_in_maps(inputs)
    res = run_bass_kernel_spmd(nc, maps, core_ids=list(range(8)))
    return assemble(res.results)
```

```python
import contextlib
import math
import numpy as np
import concourse.bass as bass
import concourse.mybir as mybir
from concourse.bass_utils import run_bass_kernel_spmd

F32 = mybir.dt.float32
BF16 = mybir.dt.bfloat16
U8 = mybir.dt.uint8
AF = mybir.ActivationFunctionType
ALU = mybir.AluOpType
DTSIZE = {F32: 4, BF16: 2, U8: 1, mybir.dt.int32: 4, mybir.dt.uint32: 4}

ENGS = ("pe", "act", "dve", "pool", "sp")
SEM_ROLL = 30000
NDMA_SEM = 20

D = 1024
NT = 8
L = 2048
NB = 256
NBLK = L // NB
CH = NB // 4
DS = 64
NSEQ = 17
NTOK = L + DS
DN_ALPHA = 2.0 ** 0.25
LN_EPS = 1e-5
N_EXP = 32


class Buf:
    __slots__ = ("ap", "key")

    def __init__(self, ap, key):
        self.ap = ap
        self.key = key

    def __getitem__(self, idx):
        return Buf(self.ap[idx], self.key)

    def re(self, pat, **kw):
        return Buf(self.ap.rearrange(pat, **kw), self.key)

    def bc(self, shape):
        return Buf(self.ap.to_broadcast(list(shape)), self.key)

    def un(self, d):
        return Buf(self.ap.unsqueeze(d), self.key)

    def cast(self, dt):
        return Buf(self.ap.bitcast(dt), self.key)

    def k(self, suffix):
        return Buf(self.ap, f"{self.key}.{suffix}")


def A(x):
    return x.ap if isinstance(x, Buf) else x


class Prog:
    def __init__(self, nc):
        self.nc = nc
        self.ops = {e: [] for e in ENGS}
        self.last_w = {}
        self.readers = {}
        self.ndma = {e: 0 for e in ENGS}
        self.out_dmas = []
        self.open_dmas = set()
        self.pending = {e: set() for e in ENGS}
        self.rr = 0
        self._cap = None

    def begin(self):
        self._cap = []
        return self._cap

    def end(self):
        self._cap = None

    def replay(self, streams):
        pos = [0] * len(streams)
        total = sum(len(x) for x in streams)
        for _ in range(total):
            best, bi = None, None
            for i, x in enumerate(streams):
                if pos[i] < len(x):
                    frac = pos[i] / len(x)
                    if best is None or frac < best:
                        best, bi = frac, i
            kind, args, kw = streams[bi][pos[bi]]
            pos[bi] += 1
            if kind == "op":
                self.op(*args, **kw)
            else:
                self.dma(*args, **kw)

    def _deps(self, reads, writes):
        deps = set()
        for k in reads:
            if k in self.last_w:
                deps.add(self.last_w[k])
        for k in writes:
            if k in self.last_w:
                deps.add(self.last_w[k])
            for r in self.readers.get(k, ()):
                deps.add(r)
        return deps

    def _commit(self, ident, reads, writes):
        for k in reads:
            self.readers.setdefault(k, []).append(ident)
        for k in writes:
            self.last_w[k] = ident
            self.readers[k] = []

    def _keys(self, outs, ins):
        reads = [b.key for b in ins if isinstance(b, Buf)]
        writes = [b.key for b in outs if isinstance(b, Buf)]
        writes += [k for k in reads if k.startswith("ps")]
        return reads, writes

    def op(self, eng, fn, outs=(), ins=()):
        if self._cap is not None:
            self._cap.append(("op", (eng, fn, list(outs), list(ins)), {}))
            return {}
        reads, writes = self._keys(outs, ins)
        idx = len(self.ops[eng])
        deps = self._deps(reads, writes)
        deps |= self.pending[eng]
        self.pending[eng] = set()
        for d in deps:
            self.open_dmas.discard(d)
        rec = dict(kind="op", fn=fn, deps=deps, signal=False, eng=eng, idx=idx)
        self.ops[eng].append(rec)
        self._commit((eng, idx), reads, writes)
        return rec

    def dma(self, q, out, in_, is_out=False, extra_reads=(), **kw):
        if self._cap is not None:
            self._cap.append(("dma", (q, out, in_), dict(is_out=is_out, extra_reads=list(extra_reads), **kw)))
            return {}
        reads, writes = self._keys([out], [in_] + list(extra_reads))
        deps = self._deps(reads, writes)
        deps |= self.pending[q]
        self.pending[q] = set()
        for d in deps:
            self.open_dmas.discard(d)
        n = self.ndma[q]
        self.ndma[q] += 1
        did = ("dma", q, n)
        if n >= NDMA_SEM:
            deps.add(("dma", q, n - NDMA_SEM))
        o, i = A(out), A(in_)
        rec = dict(kind="dma", fn=(lambda h: h.dma_start(out=o, in_=i, **kw)), deps=deps, q=q, n=n,
                   eng=q, idx=len(self.ops[q]))
        self.ops[q].append(rec)
        self._commit(did, reads, writes)
        self.open_dmas.add(did)
        if is_out:
            self.out_dmas.append(did)
        return rec

    def barrier(self):
        deps = set(self.open_dmas)
        for e in ENGS:
            n = len(self.ops[e])
            for i in range(n - 1, -1, -1):
                if self.ops[e][i]["kind"] == "op":
                    deps.add((e, i))
                    break
        for e in ENGS:
            self.pending[e] |= deps
        self.open_dmas = set()

    def act(self, out, in_, func, bias=None, scale=None):
        kw = {}
        ins = [in_]
        if bias is not None:
            kw["bias"] = A(bias)
            ins.append(bias)
        if scale is not None:
            kw["scale"] = A(scale)
            ins.append(scale)
        o, i = A(out), A(in_)
        return self.op("act", lambda h: h.activation(out=o, in_=i, func=func, **kw), [out], ins)

    def tt(self, eng, out, in0, in1, op):
        o, a, b = A(out), A(in0), A(in1)
        return self.op(eng, lambda h: h.tensor_tensor(out=o, in0=a, in1=b, op=op), [out], [in0, in1])

    def ts(self, eng, out, in0, s1, s2=None, op0=ALU.mult, op1=None):
        o, a = A(out), A(in0)
        x1, x2 = A(s1), A(s2)
        if op1 is None:
            fn = lambda h: h.tensor_scalar(out=o, in0=a, scalar1=x1, scalar2=None, op0=op0)
        else:
            fn = lambda h: h.tensor_scalar(out=o, in0=a, scalar1=x1, scalar2=x2, op0=op0, op1=op1)
        return self.op(eng, fn, [out], [in0, s1, s2])

    def stt(self, out, in0, scalar, in1, op0, op1):
        o, a, s, b = A(out), A(in0), A(scalar), A(in1)
        return self.op("dve", lambda h: h.scalar_tensor_tensor(out=o, in0=a, scalar=s, in1=b, op0=op0, op1=op1),
                       [out], [in0, scalar, in1])

    def copy(self, eng, out, in_):
        o, i = A(out), A(in_)
        if eng == "act":
            return self.op("act", lambda h: h.copy(out=o, in_=i), [out], [in_])
        return self.op(eng, lambda h: h.tensor_copy(out=o, in_=i), [out], [in_])

    def memset(self, eng, out, val):
        o = A(out)
        return self.op(eng, lambda h: h.memset(o, val), [out], [])

    def mm(self, out, pairs, extra_ins=()):
        o = A(out)
        prs = [(A(l), A(r)) for l, r in pairs]
        n = len(prs)

        def fn(h):
            ins = None
            for i, (l, r) in enumerate(prs):
                ins = h.matmul(o, lhsT=l, rhs=r, start=(i == 0), stop=(i == n - 1))
            return ins
        ins_b = [x for pr in pairs for x in pr] + list(extra_ins)
        return self.op("pe", fn, [out], ins_b)

    def tr(self, out, in_, ident):
        o, i, idn = A(out), A(in_), A(ident)
        return self.op("pe", lambda h: h.transpose(out=o, in_=i, identity=idn), [out], [in_, ident])

    def scan(self, out, d0, d1, init):
        o, a, b, c = A(out), A(d0), A(d1), A(init)
        return self.op("dve", lambda h: h.tensor_tensor_scan(out=o, data0=a, data1=b, initial=c,
                                                             op0=ALU.mult, op1=ALU.add), [out], [d0, d1, init])

    def emit(self, stack):
        nc = self.nc
        for e in ENGS:
            for rec in self.ops[e]:
                for d in rec["deps"]:
                    if d[0] == "dma":
                        continue
                    de, di = d
                    if de == e and rec["kind"] == "op" and e == "pe":
                        continue
                    self.ops[de][di]["signal"] = True
        self.sems = {}
        for e in ENGS:
            cnt = 0
            for rec in self.ops[e]:
                if rec["kind"] == "op" and rec["signal"]:
                    rec["semi"] = cnt // SEM_ROLL
                    rec["semv"] = cnt % SEM_ROLL + 1
                    cnt += 1
            ns = max(1, (cnt + SEM_ROLL - 1) // SEM_ROLL)
            self.sems[e] = [stack.enter_context(nc.semaphore(f"s_{e}_{i}")) for i in range(ns)]
        self.dsems = {}
        for q in ENGS:
            if self.ndma[q]:
                self.dsems[q] = [stack.enter_context(nc.semaphore(f"d_{q}_{i}"))
                                 for i in range(min(NDMA_SEM, self.ndma[q]))]
        block = stack.enter_context(nc.Block())
        prog = self

        def run_engine(e, h):
            waited = {x: -1 for x in ENGS}
            waited_dma = set()
            for rec in prog.ops[e]:
                need = {}
                for d in rec["deps"]:
                    if d[0] == "dma":
                        if d in waited_dma:
                            continue
                        waited_dma.add(d)
                        _, q, n = d
                        h.wait_ge(prog.dsems[q][n % NDMA_SEM], 16 * (n // NDMA_SEM + 1))
                        continue
                    de, di = d
                    if de == e and rec["kind"] == "op" and e == "pe":
                        continue
                    if di <= waited[de]:
                        continue
                    need[de] = max(need.get(de, -1), di)
                for de, di in need.items():
                    src = prog.ops[de][di]
                    h.wait_ge(prog.sems[de][src["semi"]], src["semv"])
                    waited[de] = di
                ins = rec["fn"](h)
                if rec["kind"] == "dma":
                    ins.then_inc(prog.dsems[rec["q"]][rec["n"] % NDMA_SEM], 16)
                elif rec["signal"]:
                    ins.then_inc(prog.sems[e][rec["semi"]], 1)
            if e == "sp":
                for d in prog.out_dmas:
                    if d in waited_dma:
                        continue
                    _, q, n = d
                    h.wait_ge(prog.dsems[q][n % NDMA_SEM], 16 * (n // NDMA_SEM + 1))

        @block.tensor
        def _(h):
            run_engine("pe", h)

        @block.scalar
        def _(h):
            run_engine("act", h)

        @block.vector
        def _(h):
            run_engine("dve", h)

        @block.gpsimd
        def _(h):
            run_engine("pool", h)

        @block.sync
        def _(h):
            run_engine("sp", h)


class Arena:
    def __init__(self, nc, stack, nbytes):
        self.t = stack.enter_context(nc.sbuf_tensor("arena", [128, nbytes], U8))
        self.n = nbytes
        self.limit = nbytes
        self.off = 0
        self.stk = []
        self.peak = 0
        self.cnt = 0

    def alloc(self, key, shape, dt, parts=128):
        n = int(np.prod(shape)) * DTSIZE[dt]
        n_al = (n + 63) // 64 * 64
        assert self.off + n_al <= self.limit, (key, self.off, n_al, self.limit)
        ap = self.t[0:parts, self.off:self.off + n].bitcast(dt)
        if len(shape) > 1:
            names = [f"d{i}" for i in range(len(shape))]
            pat = "p (" + " ".join(names) + ") -> p " + " ".join(names)
            ap = ap.rearrange(pat, **{nm: s for nm, s in zip(names[1:], shape[1:])})
        self.off += n_al
        self.peak = max(self.peak, self.off)
        self.cnt += 1
        return Buf(ap, f"{key}#{self.cnt}")

    def alloc_top(self, key, shape, dt, parts=128):
        n = int(np.prod(shape)) * DTSIZE[dt]
        n_al = (n + 63) // 64 * 64
        self.limit -= n_al
        save = self.off
        self.off = self.limit
        lim = self.limit
        self.limit = self.n
        b = self.alloc(key, shape, dt, parts)
        self.limit = lim
        self.off = save
        return b

    def push(self):
        self.stk.append(self.off)

    def pop(self):
        self.off = self.stk.pop()


IN_SPECS = [
    ("xp", [L, D]), ("xs", [DS, D]), ("cc", [NSEQ, D]), ("sconv", [16, 30, 512]),
    ("sre", [16, 2048]), ("sim", [16, 2048]),
    ("w_ada", [D, 6 * D]), ("b_ada", [6 * D]), ("w_in", [D, 3584]), ("b_in", [3584]),
    ("w_dw", [31, 512]), ("b_dw", [512]), ("conv_ln_g", [512]), ("conv_ln_b", [512]),
    ("w_pw", [512, D]), ("b_pw", [D]), ("lam_re", [32, 64]), ("lam_im", [32, 64]), ("log_dt", [32]),
    ("b_re", [32, 64, 16]), ("b_im", [32, 64, 16]), ("c_re", [32, 16, 64]), ("c_im", [32, 16, 64]),
    ("d_skip", [32, 16]), ("w_sv", [512, D]), ("b_sv", [D]), ("w_sg", [512, D]), ("b_sg", [D]),
    ("w_out", [D, D]), ("b_out", [D]), ("ln1_g", [D]), ("ln1_b", [D]), ("w_router", [D, 32]),
    ("b_router", [32]), ("w1", [32, D, 2 * D]), ("b1l", [32 * 128, 16]), ("w2", [32, D, D]), ("b2", [32, D]),
    ("ln2_g", [D]), ("ln2_b", [D]),
    ("k_ident", [128, 128]), ("k_selu", [128, 16 * 128]), ("k_sely", [128, 16 * 128]),
    ("k_selp", [NSEQ, 128]), ("k_sels", [NSEQ, DS]),
    ("k_ls", [128, 128]), ("k_iota32", [128, 32]), ("k_tokid", [128, 17]), ("k_svals", [1, 48]),
    ("k_iotak", [128, 8]), ("k_iotae", [32, 1]), ("k_esel", [128, 17 * 17]), ("k_selt", [17, 17 * 128]),
]
OUT_SPECS = [
    ("yp", [L, D]), ("ys", [DS, D]), ("ncp", [30, 512]), ("nrp", [16, 128]), ("nip", [16, 128]),
    ("ncs", [16, 30, 512]), ("nrs", [16, 2048]), ("nis", [16, 2048]),
]
C_BIN, C_BDW, C_CG, C_CB, C_BPW, C_BSV, C_BSG, C_BADA, C_L1G, C_L1B = 0, 28, 32, 36, 40, 48, 56, 64, 112, 120
M_SH1, M_SC1, M_G1, M_SH2, M_SC2, M_G2 = 0, 8, 16, 24, 32, 40


def build_program(dbg=(), stop_after=None):
    nc = bass.Bass("TRN2", target_bir_lowering=False)
    P = Prog(nc)
    I = {n: nc.dram_tensor(n, list(s), F32, kind="ExternalInput").ap() for n, s in IN_SPECS}
    O = {n: nc.dram_tensor(n, list(s), F32, kind="ExternalOutput").ap() for n, s in OUT_SPECS}
    st = contextlib.ExitStack()
    st.__enter__()
    AR = Arena(nc, st, 204 * 1024)
    PS = [Buf(st.enter_context(nc.psum_tensor(f"psb{i}", [128, 512], F32))[:], f"ps{i}") for i in range(8)]
    psi = [0]
    bank_sets = {"all": list(range(8)), "conv": [0, 1, 2], "s5a": [3, 4], "s5b": [5, 6, 7]}
    bank_ctr = {k: 0 for k in bank_sets}
    cur_set = ["all"]

    def bank():
        st_ = cur_set[0]
        lst = bank_sets[st_]
        b = PS[lst[bank_ctr[st_] % len(lst)]]
        bank_ctr[st_] += 1
        return b

    def dbg_out(name, buf, shape, dt=F32):
        if name in dbg:
            t = nc.dram_tensor("dbg_" + name, list(shape), dt, kind="ExternalOutput").ap()
            P.dma("sp", t, buf, is_out=True)

    def finish():
        P.emit(st)
        st.close()
        return nc

    identf = AR.alloc("identf", [128], F32)
    identb = AR.alloc("identb", [128], BF16)
    onesf = AR.alloc("onesf", [128], F32)
    colsT = AR.alloc("colsT", [128], F32)
    modT = AR.alloc("modT", [48, NSEQ], F32)
    A_T = AR.alloc("A_T", [NT, NSEQ], F32)
    Bv_T = AR.alloc("Bv_T", [NT, NSEQ], F32)
    b1T = AR.alloc("b1T", [16, N_EXP], F32)
    merged = AR.alloc_top("merged", [NT, NTOK], BF16)
    epsc = AR.alloc("epsc", [1], F32)

    P.dma("sp", identf, I["k_ident"])
    P.copy("dve", identb, identf)
    P.memset("pool", onesf, 1.0)
    P.memset("pool", epsc, LN_EPS)

    AR.push()
    selu = AR.alloc("selu", [16, 128], BF16)
    sely = AR.alloc("sely", [16, 128], BF16)
    P.dma("pool", selu.re("p a b -> p (a b)"), I["k_selu"])
    P.dma("pool", sely.re("p a b -> p (a b)"), I["k_sely"])
    ring = [AR.alloc(f"ring{i}", [NT, 512], BF16) for i in range(3)]
    rix = [0]

    def ring_load(src_ap, view4=False):
        slot = ring[rix[0] % len(ring)]
        rix[0] += 1
        if view4:
            dst = slot.re("p a b -> p (a b)").re("p (k c) -> p k c", k=4)
        else:
            dst = slot
        P.dma("pool", dst, src_ap)
        return dst

    wdwT = AR.alloc("wdwT", [4, 31], F32)
    Kmat = AR.alloc("Kmat", [16, 128], BF16)
    CexpA = AR.alloc("CexpA", [16, 4, 2, 16], BF16)
    CexpB = AR.alloc("CexpB", [16, 4, 2, 16], BF16)
    BexpRe = AR.alloc("BexpRe", [16, 128], BF16)
    BexpIm = AR.alloc("BexpIm", [16, 128], BF16)
    Tre = AR.alloc("Tre", [16, CH], F32)
    Tim = AR.alloc("Tim", [16, CH], F32)
    dcol = AR.alloc("dcol", [16], F32)
    carry = AR.alloc("carry", [16, 2], F32)
    h0sb = AR.alloc("h0sb", [16, 2, 16], F32)
    rot0 = AR.alloc("rot0", [16, 2, 16], F32)
    Hsh_s = AR.alloc("Hsh_s", [16, 2, 16], BF16)
    hend_s = AR.alloc("hend_s", [16, 2, 16], F32)
    full_s = AR.alloc("full_s", [4, 16, 34], F32)
    P.memset("pool", carry, 0.0)

    AR.push()
    stg = AR.alloc("stg", [128], F32)
    stg_parts = []
    for nm, r0, J in (("b_in", C_BIN, 28), ("b_dw", C_BDW, 4), ("conv_ln_g", C_CG, 4), ("conv_ln_b", C_CB, 4),
                      ("b_pw", C_BPW, 8), ("b_sv", C_BSV, 8), ("b_sg", C_BSG, 8), ("b_ada", C_BADA, 48),
                      ("ln1_g", C_L1G, 8), ("ln1_b", C_L1B, 8)):
        sub = stg[r0:r0 + J, :].k(nm)
        P.dma("sp", sub, I[nm].rearrange("(j p) -> j p", p=128))
        stg_parts.append(sub)
    pb = bank()
    P.op("pe", (lambda h, o=A(pb[:, 0:128]), i=A(stg), idn=A(identf): h.transpose(out=o, in_=i, identity=idn)),
         [pb], [identf] + stg_parts)
    P.copy("dve", colsT, pb[:, 0:128])

    cc_sb = AR.alloc("cc_sb", [D], F32, parts=NSEQ)
    P.dma("sp", cc_sb, I["cc"])
    csl = AR.alloc("csl", [D], F32, parts=NSEQ)
    P.act(csl, cc_sb, AF.Silu)
    siluT = AR.alloc("siluT", [NT, NSEQ], BF16)
    pb = bank()
    for k in range(NT):
        P.tr(pb[:, k * NSEQ:(k + 1) * NSEQ], csl[:, k * 128:(k + 1) * 128], identf[0:NSEQ, 0:NSEQ])
    P.copy("dve", siluT.re("p a b -> p (a b)"), pb[:, 0:NT * NSEQ])

    for c in range(12):
        slot = ring_load(I["w_ada"][:, c * 512:(c + 1) * 512].rearrange("(kt p) c -> p kt c", p=128))
        pb = bank()
        for jj in range(4):
            P.mm(pb[:, jj * NSEQ:(jj + 1) * NSEQ],
                 [(slot[:, k, jj * 128:(jj + 1) * 128], siluT[:, k, :]) for k in range(NT)])
        P.tt("dve", modT[:, 4 * c:4 * c + 4, :], pb[:, 0:4 * NSEQ].re("p (a b) -> p a b", a=4),
             colsT[:, C_BADA + 4 * c:C_BADA + 4 * c + 4].un(2).bc([128, 4, NSEQ]), ALU.add)
    P.ts("dve", modT[:, M_SC1:M_SC1 + 8, :], modT[:, M_SC1:M_SC1 + 8, :], 1.0, None, op0=ALU.add)
    P.ts("dve", modT[:, M_SC2:M_SC2 + 8, :], modT[:, M_SC2:M_SC2 + 8, :], 1.0, None, op0=ALU.add)
    P.tt("dve", A_T, modT[:, M_SC2:M_SC2 + 8, :], colsT[:, C_L1G:C_L1G + 8].un(2).bc([128, NT, NSEQ]), ALU.mult)
    P.tt("dve", Bv_T, modT[:, M_SC2:M_SC2 + 8, :], colsT[:, C_L1B:C_L1B + 8].un(2).bc([128, NT, NSEQ]), ALU.mult)
    P.tt("dve", Bv_T, Bv_T, modT[:, M_SH2:M_SH2 + 8, :], ALU.add)
    dbg_out("modT", modT, [128, 48, NSEQ])

    stgw = AR.alloc("stgw", [512], F32, parts=31)
    P.dma("sp", stgw, I["w_dw"])
    pb = bank()
    for ct in range(4):
        P.tr(pb[:, ct * 31:(ct + 1) * 31], stgw[:, ct * 128:(ct + 1) * 128], identf[0:31, 0:31])
    P.copy("dve", wdwT.re("p a b -> p (a b)"), pb[:, 0:124])

    stgc = AR.alloc("stgc", [4, 512], F32, parts=30)
    for sq in range(4):
        P.dma("sp", stgc, I["sconv"][4 * sq:4 * sq + 4].rearrange("s r c -> r s c"))
        for ct in range(4):
            pb = bank()
            for s4 in range(4):
                P.tr(pb[:, s4 * 30:(s4 + 1) * 30], stgc[:, s4, ct * 128:(ct + 1) * 128], identf[0:30, 0:30])
            P.copy("dve", full_s[:, ct, 4 * sq:4 * sq + 4, 0:30], pb[:, 0:120].re("p (s r) -> p s r", s=4))
    P.dma("sp", O["ncs"][:, 0:26, :], I["sconv"][:, 4:30, :], is_out=True)

    stg2 = AR.alloc("stg2", [128], F32, parts=64)
    P.dma("sp", stg2[0:16, :].k("a"), I["lam_re"].rearrange("(q g) p -> q (g p)", g=2))
    P.dma("sp", stg2[16:32, :].k("b"), I["lam_im"].rearrange("(q g) p -> q (g p)", g=2))
    ldt_raw = AR.alloc("ldt_raw", [2], F32, parts=48)
    P.dma("sp", ldt_raw[32:48, :], I["log_dt"].rearrange("(q g) -> q g", g=2))
    P.copy("dve", stg2[32:48, :].k("c").re("q (g p) -> q g p", g=2), ldt_raw[32:48, :].un(2).bc([16, 2, 64]))
    P.dma("sp", stg2[48:64, :].k("d").re("q (s m) -> q s m", s=4),
          I["d_skip"].rearrange("(q g) h -> q (g h)", g=2).unsqueeze(1).to_broadcast([16, 4, 32]))
    parT = AR.alloc("parT", [64], F32)
    pb = bank()
    P.op("pe", (lambda h, o=A(pb[:, 0:64]), i=A(stg2), idn=A(identf[0:64, 0:64]): h.transpose(out=o, in_=i, identity=idn)),
         [pb], [identf, stg2.k("a"), stg2.k("b"), stg2.k("c"), stg2.k("d")])
    P.copy("dve", parT, pb[:, 0:64])
    lamre, lamim, ldt, dexp = parT[:, 0:16], parT[:, 16:32], parT[:, 32:48], parT[:, 48:64]

    Bre = AR.alloc("Bre", [16, 16], F32)
    Bim = AR.alloc("Bim", [16, 16], F32)
    P.dma("sp", Bre, I["b_re"].rearrange("(q g) p h -> (g p) q h", g=2))
    P.dma("sp", Bim, I["b_im"].rearrange("(q g) p h -> (g p) q h", g=2))
    Craw = AR.alloc("Craw", [32, 64], F32, parts=16)
    CTre = AR.alloc("CTre", [16, 16], F32)
    CTim = AR.alloc("CTim", [16, 16], F32)
    for (dst, nm) in ((CTre, "c_re"), (CTim, "c_im")):
        P.dma("sp", Craw, I[nm].rearrange("g h p -> h g p"))
        pb = bank()
        for q in range(16):
            P.tr(pb[:, q * 16:(q + 1) * 16], Craw[:, 2 * q:2 * q + 2, :].re("h g p -> h (g p)"), identf[0:16, 0:16])
        P.copy("dve", dst.re("p a b -> p (a b)"), pb[:, 0:256])

    def sm(name, n=16):
        return AR.alloc(name, [n], F32)

    dt_ = sm("dt")
    P.act(dt_, ldt, AF.Exp)
    mag = sm("mag")
    lrd = sm("lrd")
    P.tt("dve", lrd, lamre, dt_, ALU.mult)
    P.act(mag, lrd, AF.Exp)
    ang = sm("ang")
    P.tt("dve", ang, lamim, dt_, ALU.mult)
    halfpi = sm("halfpi", 1)
    P.memset("pool", halfpi, math.pi / 2)
    s_ = sm("s_")
    c_ = sm("c_")
    P.act(s_, ang, AF.Sin, scale=1.0 / 8)
    P.act(c_, ang, AF.Sin, bias=halfpi[:, 0:1], scale=-1.0 / 8)
    t1 = sm("t1")
    t2 = sm("t2")
    for _ in range(3):
        P.tt("dve", t1, c_, c_, ALU.mult)
        P.tt("dve", t2, s_, s_, ALU.mult)
        P.tt("dve", s_, s_, c_, ALU.mult)
        P.ts("dve", s_, s_, 2.0, None, op0=ALU.mult)
        P.tt("dve", c_, t1, t2, ALU.subtract)
    PWre = AR.alloc("PWre", [5, 16], F32)
    PWim = AR.alloc("PWim", [5, 16], F32)
    P.tt("dve", PWre[:, 1, :], mag, c_, ALU.mult)
    P.tt("dve", PWim[:, 1, :], mag, s_, ALU.mult)

    cmT = [AR.alloc(f"cmT{i}", [16 * 32], F32) for i in range(4)]

    def cmul(eng, ore, oim, are, aim, bre, bim, shape_tmp):
        n = int(np.prod(shape_tmp))

        def view(b):
            v = b[:, 0:n]
            if len(shape_tmp) == 2:
                v = v.re("p (a b) -> p a b", a=shape_tmp[0])
            return v
        ta, tb, tc_, td = [view(b) for b in cmT]
        P.tt(eng, ta, are, bre, ALU.mult)
        P.tt(eng, tb, aim, bim, ALU.mult)
        P.tt(eng, tc_, are, bim, ALU.mult)
        P.tt(eng, td, aim, bre, ALU.mult)
        P.tt(eng, ore, ta, tb, ALU.subtract)
        P.tt(eng, oim, tc_, td, ALU.add)

    for kk in (2, 3, 4):
        cmul("dve", PWre[:, kk, :], PWim[:, kk, :], PWre[:, kk - 1, :], PWim[:, kk - 1, :],
             PWre[:, 1, :], PWim[:, 1, :], [16])
    nr = sm("nr")
    P.ts("dve", nr, PWre[:, 1, :], -1.0, None, op0=ALU.add)
    den = sm("den")
    P.tt("dve", t1, lamre, lamre, ALU.mult)
    P.tt("dve", t2, lamim, lamim, ALU.mult)
    P.tt("dve", den, t1, t2, ALU.add)
    rden = sm("rden")
    P.op("dve", (lambda h, o=A(rden), i=A(den): h.reciprocal(out=o, in_=i)), [rden], [den])
    cre = sm("cre")
    cim = sm("cim")
    t3 = sm("t3")
    t4 = sm("t4")
    P.tt("dve", t1, nr, lamre, ALU.mult)
    P.tt("dve", t2, PWim[:, 1, :], lamim, ALU.mult)
    P.tt("dve", t3, t1, t2, ALU.add)
    P.tt("dve", cre, t3, rden, ALU.mult)
    P.tt("dve", t1, PWim[:, 1, :], lamre, ALU.mult)
    P.tt("dve", t2, nr, lamim, ALU.mult)
    P.tt("dve", t4, t1, t2, ALU.subtract)
    P.tt("dve", cim, t4, rden, ALU.mult)
    bbre = AR.alloc("bbre", [16, 16], F32)
    bbim = AR.alloc("bbim", [16, 16], F32)
    cmul("dve", bbre, bbim, cre.un(2).bc([128, 16, 16]), cim.un(2).bc([128, 16, 16]), Bre, Bim, [16, 16])
    CT0re = AR.alloc("CT0re", [16, 2, 16], F32)
    CT0imn = AR.alloc("CT0imn", [16, 2, 16], F32)
    P.memset("pool", CT0re, 0.0)
    P.memset("pool", CT0imn, 0.0)
    P.copy("pool", CT0re[0:64, :, 0, :], CTre[0:64])
    P.copy("pool", CT0re[64:128, :, 1, :], CTre[64:128])
    P.ts("pool", CT0imn[0:64, :, 0, :], CTim[0:64], -1.0, None, op0=ALU.mult)
    P.ts("pool", CT0imn[64:128, :, 1, :], CTim[64:128], -1.0, None, op0=ALU.mult)
    Are = AR.alloc("Are", [8, 7, 2, 16], F32)
    Aim = AR.alloc("Aim", [8, 7, 2, 16], F32)
    xr = AR.alloc("xr", [16, 16], F32)
    xi = AR.alloc("xi", [16, 16], F32)
    for qh in range(2):
        qo = 8 * qh
        P.memset("pool", Are, 0.0)
        P.memset("pool", Aim, 0.0)
        for a in range(4):
            kk = 3 - a
            if kk == 0:
                sr_, si_ = bbre, bbim
            else:
                cmul("dve", xr, xi, PWre[:, kk, :].un(2).bc([128, 16, 16]), PWim[:, kk, :].un(2).bc([128, 16, 16]),
                     bbre, bbim, [16, 16])
                sr_, si_ = xr, xi
            for (dstA, src) in ((Are, sr_), (Aim, si_)):
                P.copy("pool", dstA[0:64, :, a, 0, :], src[0:64, qo:qo + 8, :])
                P.copy("pool", dstA[64:128, :, a, 1, :], src[64:128, qo:qo + 8, :])
        for (dst, srcA) in ((BexpRe, Are), (BexpIm, Aim)):
            for q4 in range(2):
                pb = bank()
                for lp in range(4):
                    ql = 4 * q4 + lp
                    P.tr(pb[:, lp * 128:(lp + 1) * 128], srcA[:, ql, 0:4, :, :].re("p a g h -> p (a g h)"), identf)
                P.copy("act", dst[:, qo + 4 * q4:qo + 4 * q4 + 4, :].re("p a b -> p (a b)"), pb[:, 0:512])
        for q4 in range(2):
            pb = bank()
            for lp in range(4):
                ql = 4 * q4 + lp
                q = qo + ql
                for tau in range(4):
                    a0 = 3 - tau
                    P.mm(pb[:, lp * 128 + tau * 32: lp * 128 + tau * 32 + 32],
                         [(Are[:, ql, a0:a0 + 4, :, :].re("p a g h -> p (a g h)"), CT0re[:, q, :, :].re("p g h -> p (g h)")),
                          (Aim[:, ql, a0:a0 + 4, :, :].re("p a g h -> p (a g h)"), CT0imn[:, q, :, :].re("p g h -> p (g h)"))])
            for lp in range(4):
                q = qo + 4 * q4 + lp
                P.stt(Kmat[:, q, :], identf, dexp[:, q:q + 1], pb[:, lp * 128:(lp + 1) * 128], ALU.mult, ALU.add)
    P.memset("pool", CexpA, 0.0)
    P.memset("pool", CexpB, 0.0)
    for tau in range(4):
        cmul("dve", xr, xi, PWre[:, tau + 1, :].un(2).bc([128, 16, 16]), PWim[:, tau + 1, :].un(2).bc([128, 16, 16]),
             CTre, CTim, [16, 16])
        P.copy("pool", CexpA[0:64, :, tau, 0, :], xr[0:64])
        P.copy("pool", CexpA[64:128, :, tau, 1, :], xr[64:128])
        P.ts("pool", CexpB[0:64, :, tau, 0, :], xi[0:64], -1.0, None, op0=ALU.mult)
        P.ts("pool", CexpB[64:128, :, tau, 1, :], xi[64:128], -1.0, None, op0=ALU.mult)
    rd = sm("rd")
    P.tt("dve", t1, PWre[:, 4, :], PWre[:, 4, :], ALU.mult)
    P.tt("dve", t2, PWim[:, 4, :], PWim[:, 4, :], ALU.mult)
    P.tt("dve", t3, t1, t2, ALU.add)
    P.act(dcol, t3, AF.Sqrt)
    P.op("dve", (lambda h, o=A(rd), i=A(dcol): h.reciprocal(out=o, in_=i)), [rd], [dcol])
    P.tt("dve", Tre[:, :, 0], PWre[:, 4, :], rd, ALU.mult)
    P.tt("dve", t1, PWim[:, 4, :], rd, ALU.mult)
    P.ts("dve", Tim[:, :, 0], t1, -1.0, None, op0=ALU.mult)
    m = 1
    while m < CH:
        ar = Tre[:, :, m - 1:m].bc([128, 16, m])
        ai = Tim[:, :, m - 1:m].bc([128, 16, m])
        cmul("dve", Tre[:, :, m:2 * m], Tim[:, :, m:2 * m], Tre[:, :, 0:m], Tim[:, :, 0:m], ar, ai, [16, m])
        m *= 2
    sraw = AR.alloc("sraw", [2048], F32, parts=16)
    for ri, nm in ((0, "sre"), (1, "sim")):
        P.dma("sp", sraw, I[nm])
        pb = bank()
        for q in range(16):
            P.tr(pb[:, q * 16:(q + 1) * 16], sraw[:, q * 128:(q + 1) * 128], identf[0:16, 0:16])
        P.copy("dve", h0sb[:, :, ri, :], pb[:, 0:256].re("p (q s) -> p q s", q=16))
    P.copy("dve", Hsh_s, h0sb)
    cmul("dve", rot0[:, :, 0, :], rot0[:, :, 1, :], PWre[:, 4, :].un(2).bc([128, 16, 16]), PWim[:, 4, :].un(2).bc([128, 16, 16]),
         h0sb[:, :, 0, :], h0sb[:, :, 1, :], [16, 16])
    dbg_out("Kmat", Kmat, [128, 16, 128], BF16)
    dbg_out("Tre", Tre, [128, 16, CH])
    dbg_out("PWre", PWre, [128, 5, 16])
    dbg_out("PWim", PWim, [128, 5, 16])
    AR.pop()
    P.barrier()
    if stop_after == "setup":
        return finish()

    xt = AR.alloc("xt", [D], F32)
    hT = AR.alloc("hT", [NT, NB], BF16)
    a_sb = AR.alloc("a_sb", [4, NB], F32)
    sgt = AR.alloc("sgt", [4, NB], F32)
    fullp = AR.alloc("fullp", [4, 30 + NB], F32)
    hist = AR.alloc("hist", [4, 30], F32)
    ptap = [AR.alloc(f"ptap{i}", [NB], BF16) for i in range(4)]
    zsd = AR.alloc("zsd", [4, 4, CH], BF16)
    v_sb = AR.alloc("v_sb", [4, NB], F32)
    sq_sb = Buf(a_sb.ap, a_sb.key)
    mean_sb = AR.alloc("mean_sb", [NB], F32)
    var_sb = AR.alloc("var_sb", [NB], F32)
    rstd_sb = AR.alloc("rstd_sb", [NB], F32)
    vact = AR.alloc("vact", [4, NB], BF16)
    cvo = AR.alloc("cvo", [NT, NB], BF16)
    sso = AR.alloc("sso", [NT, NB], BF16)
    sgg2 = [AR.alloc(f"sgg{i}", [NB], BF16) for i in range(2)]
    S5B = []
    for i in range(2):
        S5B.append(dict(
            U=AR.alloc(f"U{i}", [4, CH], BF16), hl=AR.alloc(f"hl{i}", [4, 2, CH], F32),
            gb=AR.alloc(f"gb{i}", [4, 2, CH], F32), gg=AR.alloc(f"gg{i}", [4, 2, CH], F32),
            hfull=AR.alloc(f"hfull{i}", [4, 2, CH], F32), rt=[AR.alloc(f"rt{i}_{j}", [4, CH], F32) for j in range(4)],
            Hsh=AR.alloc(f"Hsh{i}", [4, 2, CH], BF16), yg=AR.alloc(f"yg{i}", [4, CH], BF16)))
    ygT = AR.alloc("ygT", [4, NB], BF16)
    sgc = [AR.alloc(f"sgc{i}", [NB], BF16) for i in range(2)]
    sgs = [AR.alloc(f"sgs{i}", [NB], BF16) for i in range(2)]
    m1 = [AR.alloc(f"m1{i}", [NB], F32) for i in range(2)]
    m2 = [AR.alloc(f"m2{i}", [NB], F32) for i in range(2)]
    htmp = AR.alloc("htmp", [NT, DS], F32)
    P.memset("pool", hist, 0.0)

    def win_chunk(c):
        return I["w_in"][:, c * 512:(c + 1) * 512].rearrange("(kt p) c -> p kt c", p=128)

    def w512(name):
        return I[name].rearrange("(kt p) c -> p kt c", p=128)

    for blk in range(NBLK + 1):
        samp = blk == NBLK
        N = DS if samp else NB
        C = N // 4
        tok0 = L if samp else blk * NB
        full = None if samp else fullp
        if not samp:
            for tt_ in range(NB // 128):
                P.dma("sp", xt, I["xp"][tok0 + tt_ * 128: tok0 + (tt_ + 1) * 128, :])
                for half in range(2):
                    pb = bank()
                    for d4 in range(4):
                        dt = half * 4 + d4
                        P.tr(pb[:, d4 * 128:(d4 + 1) * 128], xt[:, dt * 128:(dt + 1) * 128], identf)
                    for d4 in range(4):
                        dt = half * 4 + d4
                        P.act(hT[:, dt, tt_ * 128:(tt_ + 1) * 128], pb[:, d4 * 128:(d4 + 1) * 128], AF.Identity,
                              bias=modT[:, M_SH1 + dt, 0:1], scale=modT[:, M_SC1 + dt, 0:1])
        else:
            P.dma("sp", xt[0:DS, :], I["xs"])
            pb = bank()
            for dt in range(NT):
                P.tr(pb[:, dt * DS:(dt + 1) * DS], xt[0:DS, dt * 128:(dt + 1) * 128], identf[0:DS, 0:DS])
            P.tt("dve", htmp.re("p d (s t) -> p d s t", t=4), pb[:, 0:NT * DS].re("p (d s t) -> p d s t", d=NT, t=4),
                 modT[:, M_SC1:M_SC1 + 8, 1:NSEQ].un(3).bc([128, NT, 16, 4]), ALU.mult)
            P.tt("dve", hT[:, :, 0:DS].re("p d (s t) -> p d s t", t=4), htmp.re("p d (s t) -> p d s t", t=4),
                 modT[:, M_SH1:M_SH1 + 8, 1:NSEQ].un(3).bc([128, NT, 16, 4]), ALU.add)
        if blk == 0:
            dbg_out("hT0", hT, [128, NT, NB], BF16)
        for c in range(3):
            slot = ring_load(win_chunk(c))
            for half in range(2):
                pb = bank()
                for c2 in range(2):
                    ct = half * 2 + c2
                    P.mm(pb[:, c2 * NB:c2 * NB + N], [(slot[:, k, ct * 128:(ct + 1) * 128], hT[:, k, 0:N]) for k in range(NT)])
                for c2 in range(2):
                    ct = half * 2 + c2
                    src = pb[:, c2 * NB:c2 * NB + N]
                    bcol = colsT[:, C_BIN + c * 4 + ct:C_BIN + c * 4 + ct + 1]
                    if c == 0:
                        P.act(a_sb[:, ct, 0:N], src, AF.Identity, bias=bcol)
                    elif c == 1:
                        P.act(sgt[:, ct, 0:N], src, AF.Sigmoid, bias=bcol)
                    else:
                        P.act(zsd[:, ct, :, 0:C], src.re("p (c s) -> p s c", s=4), AF.Identity, bias=bcol)
        st_conv = P.begin()
        cur_set[0] = "conv"
        if not samp:
            P.copy("act", full[:, :, 0:30], hist)
            P.tt("dve", full[:, :, 30:30 + NB], a_sb, sgt, ALU.mult)
            if blk == NBLK - 1:
                pb = bank()
                for ct in range(4):
                    P.tr(pb[0:30, ct * 128:(ct + 1) * 128], full[:, ct, NB:NB + 30], identf)
                nco = AR.alloc("nco", [512], F32, parts=30)
                P.copy("dve", nco, pb[0:30, :])
                P.dma("sp", O["ncp"], nco, is_out=True)
        else:
            P.tt("dve", full_s[:, :, :, 30:34], a_sb[:, :, 0:DS].re("p c (s t) -> p c s t", t=4),
                 sgt[:, :, 0:DS].re("p c (s t) -> p c s t", t=4), ALU.mult)
            pb = bank()
            for ct in range(4):
                ucont = AR.alloc("ucont", [16, 4], F32) if ct == 0 else ucont
                P.copy("dve", ucont, full_s[:, ct, :, 30:34])
                P.tr(pb[0:DS, ct * 128:(ct + 1) * 128], ucont.re("p s t -> p (s t)"), identf)
            ncs_sb = AR.alloc("ncs_sb", [512], F32, parts=DS)
            P.copy("dve", ncs_sb, pb[0:DS, :])
            for s in range(16):
                P.dma("sp", O["ncs"][s, 26:30, :], ncs_sb[4 * s:4 * s + 4, :], is_out=True)
        KSPLIT = 17
        for k in range(KSPLIT):
            for ct in range(4):
                if samp:
                    src = full_s[:, ct, :, k:k + 4]
                    dst = v_sb[:, ct, 0:DS].re("p (s t) -> p s t", t=4)
                else:
                    src = full[:, ct, k:k + NB]
                    dst = v_sb[:, ct, :]
                wcol = wdwT[:, ct, k:k + 1]
                if k == 0:
                    P.ts("dve", dst, src, wcol, colsT[:, C_BDW + ct:C_BDW + ct + 1], op0=ALU.mult, op1=ALU.add)
                else:
                    P.stt(dst, src, wcol, dst, ALU.mult, ALU.add)
        for ct in range(4):
            pbc = bank()
            for k in range(KSPLIT, 31):
                pk = ptap[k % 4]
                if samp:
                    P.act(pk[:, 0:DS].re("p (s t) -> p s t", t=4), full_s[:, ct, :, k:k + 4], AF.Identity, scale=wdwT[:, ct, k:k + 1])
                else:
                    P.act(pk, full[:, ct, k:k + NB], AF.Identity, scale=wdwT[:, ct, k:k + 1])
                o_, l_, r_ = A(pbc[:, 0:N]), A(identb), A(pk[:, 0:N])
                P.op("pe", (lambda h, o_=o_, l_=l_, r_=r_, first=(k == KSPLIT), last=(k == 30):
                            h.matmul(o_, lhsT=l_, rhs=r_, start=first, stop=last)), [pbc], [identb, pk])
            P.tt("dve", v_sb[:, ct, 0:N], v_sb[:, ct, 0:N], pbc[:, 0:N], ALU.add)
        if not samp:
            P.copy("act", hist, full[:, :, NB:NB + 30])
        P.act(sq_sb[:, :, 0:N], v_sb[:, :, 0:N], AF.Square)
        pb = bank()
        P.mm(pb[:, 0:N], [(onesf, v_sb[:, ct, 0:N]) for ct in range(4)])
        P.mm(pb[:, NB:NB + N], [(onesf, sq_sb[:, ct, 0:N]) for ct in range(4)])
        P.ts("dve", mean_sb[:, 0:N], pb[:, 0:N], 1.0 / 512, None, op0=ALU.mult)
        P.tt("dve", var_sb[:, 0:N], mean_sb[:, 0:N], mean_sb[:, 0:N], ALU.mult)
        P.stt(var_sb[:, 0:N], pb[:, NB:NB + N], 1.0 / 512, var_sb[:, 0:N], ALU.mult, ALU.subtract)
        P.act(rstd_sb[:, 0:N], var_sb[:, 0:N], AF.Sqrt, bias=epsc[:, 0:1])
        P.op("dve", (lambda h, o=A(rstd_sb[:, 0:N]), i=A(rstd_sb[:, 0:N]): h.reciprocal(out=o, in_=i)), [rstd_sb], [rstd_sb])
        P.tt("dve", v_sb[:, :, 0:N], v_sb[:, :, 0:N], mean_sb[:, 0:N].un(1).bc([128, 4, N]), ALU.subtract)
        P.tt("dve", v_sb[:, :, 0:N], v_sb[:, :, 0:N], rstd_sb[:, 0:N].un(1).bc([128, 4, N]), ALU.mult)
        for ct in range(4):
            P.act(vact[:, ct, 0:N], v_sb[:, ct, 0:N], AF.Silu, bias=colsT[:, C_CB + ct:C_CB + ct + 1],
                  scale=colsT[:, C_CG + ct:C_CG + ct + 1])
        slot = ring_load(w512("w_pw"), view4=True)
        for j2 in range(4):
            pb = bank()
            for c2 in range(2):
                j = 2 * j2 + c2
                P.mm(pb[:, c2 * NB:c2 * NB + N], [(slot[:, k, j * 128:(j + 1) * 128], vact[:, k, 0:N]) for k in range(4)])
            for c2 in range(2):
                j = 2 * j2 + c2
                P.act(cvo[:, j, 0:N], pb[:, c2 * NB:c2 * NB + N], AF.Identity, bias=colsT[:, C_BPW + j:C_BPW + j + 1])
        P.end()
        st_s5 = []
        for ct in range(4):
            st_s5.append(P.begin())
            cur_set[0] = "s5a" if ct % 2 == 0 else "s5b"
            SB = S5B[ct % 2]
            U, hl, gb, gg, hfull, rt, Hsh, yg = (SB["U"], SB["hl"], SB["gb"], SB["gg"], SB["hfull"], SB["rt"], SB["Hsh"], SB["yg"])
            cry = carry.k(ct)
            pb = bank()
            for lp in range(4):
                P.mm(pb[:, lp * CH:lp * CH + C], [(selu[:, lp * 4 + s, :], zsd[:, ct, s, 0:C]) for s in range(4)])
            P.copy("act", U[:, :, 0:C], pb[:, 0:4 * CH].re("p (a c) -> p a c", a=4)[:, :, 0:C])
            pb = bank()
            for lp in range(4):
                q = 4 * ct + lp
                P.mm(pb[:, (lp * 2) * CH:(lp * 2) * CH + C], [(BexpRe[:, q, :], U[:, lp, 0:C])])
                P.mm(pb[:, (lp * 2 + 1) * CH:(lp * 2 + 1) * CH + C], [(BexpIm[:, q, :], U[:, lp, 0:C])])
            P.copy("act", hl[:, :, :, 0:C], pb[:, 0:8 * CH].re("p (a r c) -> p a r c", a=4, r=2)[:, :, :, 0:C])
            qs = slice(4 * ct, 4 * ct + 4)
            if not samp:
                hre, him = hl[:, :, 0, :], hl[:, :, 1, :]
                tr_, ti_ = Tre[:, qs, :], Tim[:, qs, :]
                P.tt("dve", rt[0], hre, tr_, ALU.mult)
                P.tt("dve", rt[1], him, ti_, ALU.mult)
                P.tt("dve", rt[2], him, tr_, ALU.mult)
                P.tt("dve", rt[3], hre, ti_, ALU.mult)
                P.tt("dve", gb[:, :, 0, :], rt[0], rt[1], ALU.subtract)
                P.tt("dve", gb[:, :, 1, :], rt[2], rt[3], ALU.add)
                P.copy("act", Hsh[:, :, :, 0], cry[:, qs, :])
                for lp in range(4):
                    q = 4 * ct + lp
                    for ri in range(2):
                        P.scan(gg[:, lp, ri, :], dcol[:, q:q + 1].bc([128, CH]), gb[:, lp, ri, :], cry[:, q, ri:ri + 1])
                gre, gim = gg[:, :, 0, :], gg[:, :, 1, :]
                P.tt("dve", rt[0], gre, tr_, ALU.mult)
                P.tt("dve", rt[1], gim, ti_, ALU.mult)
                P.tt("dve", rt[2], gim, tr_, ALU.mult)
                P.tt("dve", rt[3], gre, ti_, ALU.mult)
                P.tt("dve", hfull[:, :, 0, :], rt[0], rt[1], ALU.add)
                P.tt("dve", hfull[:, :, 1, :], rt[2], rt[3], ALU.subtract)
                P.copy("act", Hsh[:, :, :, 1:CH], hfull[:, :, :, 0:CH - 1])
                P.copy("dve", cry[:, qs, :], hfull[:, :, :, CH - 1])
                hsrc = lambda lp, ri: Hsh[:, lp, ri, 0:C]
                if blk == 0 and ct in (0, 2):
                    dbg_out(f"hl{ct}", hl, [128, 4, 2, CH])
                    dbg_out(f"gb{ct}", gb, [128, 4, 2, CH])
                    dbg_out(f"gg{ct}", gg, [128, 4, 2, CH])
                    dbg_out(f"hfull{ct}", hfull, [128, 4, 2, CH])
                    dbg_out(f"dcol{ct}", dcol, [128, 16])
                    dbg_out(f"Tre{ct}", Tre, [128, 16, CH])
                    dbg_out(f"Tim{ct}", Tim, [128, 16, CH])
            else:
                P.tt("dve", hend_s[:, qs, :, :], rot0[:, qs, :, :], hl[:, :, :, 0:C], ALU.add)
                hsrc = lambda lp, ri: Hsh_s[:, 4 * ct + lp, ri, :]
            pb = bank()
            for lp in range(4):
                q = 4 * ct + lp
                P.mm(pb[:, lp * CH:lp * CH + C],
                     [(Kmat[:, q, :], U[:, lp, 0:C]),
                      (CexpA[:, q].re("p t g h -> p (t g h)"), hsrc(lp, 0)),
                      (CexpB[:, q].re("p t g h -> p (t g h)"), hsrc(lp, 1))])
            P.act(yg[:, :, 0:C], pb[:, 0:4 * CH].re("p (a c) -> p a c", a=4)[:, :, 0:C], AF.Gelu_apprx_tanh)
            pb = bank()
            for tau in range(4):
                P.mm(pb[:, tau * CH:tau * CH + C], [(sely[:, lp * 4 + tau, :], yg[:, lp, 0:C]) for lp in range(4)])
            P.copy("dve", ygT[:, ct, 0:N].re("p (c t) -> p t c", t=4).k(ct),
                   pb[:, 0:4 * CH].re("p (t c) -> p t c", t=4)[:, :, 0:C])
            P.end()
        def _merge(a, b_):
            out, ia, ib = [], 0, 0
            while ia < len(a) or ib < len(b_):
                if ib >= len(b_) or (ia < len(a) and ia * len(b_) <= ib * len(a)):
                    out.append(a[ia]); ia += 1
                else:
                    out.append(b_[ib]); ib += 1
            return out
        cur_set[0] = "all"
        slot_sv = ring_load(w512("w_sv"), view4=True)
        slot_sg = ring_load(w512("w_sg"), view4=True)
        P.replay([_merge(st_s5[0], st_s5[1]) + _merge(st_s5[2], st_s5[3]), st_conv])
        if blk == 0:
            dbg_out("ygT0", ygT, [128, 4, NB], BF16)
            dbg_out("cvo0", cvo, [128, NT, NB], BF16)
        for j in range(NT):
            pb = bank()
            ygk_ = [ygT.k(c4) for c4 in range(4)]
            P.mm(pb[:, 0:N], [(slot_sv[:, k, j * 128:(j + 1) * 128], ygT[:, k, 0:N]) for k in range(4)], extra_ins=ygk_)
            P.mm(pb[:, NB:NB + N], [(slot_sg[:, k, j * 128:(j + 1) * 128], ygT[:, k, 0:N]) for k in range(4)], extra_ins=ygk_)
            P.act(sgg2[j % 2][:, 0:N], pb[:, NB:NB + N], AF.Sigmoid, bias=colsT[:, C_BSG + j:C_BSG + j + 1])
            P.stt(sso[:, j, 0:N], pb[:, 0:N], colsT[:, C_BSV + j:C_BSV + j + 1], sgg2[j % 2][:, 0:N], ALU.add, ALU.mult)
        for half in range(2):
            slot_c = ring_load(win_chunk(3 + half))
            slot_s = ring_load(win_chunk(5 + half))
            for j4 in range(4):
                j = half * 4 + j4
                pb = bank()
                P.mm(pb[:, 0:N], [(slot_c[:, k, j4 * 128:(j4 + 1) * 128], hT[:, k, 0:N]) for k in range(NT)])
                P.mm(pb[:, NB:NB + N], [(slot_s[:, k, j4 * 128:(j4 + 1) * 128], hT[:, k, 0:N]) for k in range(NT)])
                b = j % 2
                P.act(sgc[b][:, 0:N], pb[:, 0:N], AF.Sigmoid, bias=colsT[:, C_BIN + 12 + j:C_BIN + 12 + j + 1])
                P.act(sgs[b][:, 0:N], pb[:, NB:NB + N], AF.Sigmoid, bias=colsT[:, C_BIN + 20 + j:C_BIN + 20 + j + 1])
                P.tt("dve", m1[b][:, 0:N], cvo[:, j, 0:N], sgc[b][:, 0:N], ALU.mult)
                P.tt("dve", m2[b][:, 0:N], sso[:, j, 0:N], sgs[b][:, 0:N], ALU.mult)
                P.tt("dve", merged[:, j, tok0:tok0 + N], m1[b][:, 0:N], m2[b][:, 0:N], ALU.add)
    pb = bank()
    for ri in range(2):
        P.op("pe", (lambda h, o=A(pb[0:16, ri * 128:(ri + 1) * 128]), i=A(carry[:, :, ri]), idn=A(identf): h.transpose(out=o, in_=i, identity=idn)),
             [pb], [identf, carry] + [carry.k(c4) for c4 in range(4)])
    fst = AR.alloc("fst", [256], F32, parts=16)
    P.copy("dve", fst, pb[0:16, 0:256])
    P.dma("sp", O["nrp"], fst[:, 0:128], is_out=True)
    P.dma("sp", O["nip"], fst[:, 128:256], is_out=True)
    for ri in range(2):
        for q4 in range(4):
            pb = bank()
            for lp in range(4):
                q = 4 * q4 + lp
                P.tr(pb[0:16, lp * 128:(lp + 1) * 128], hend_s[:, q, ri, :], identf)
            P.copy("dve", ring[ri].re("p a b -> p (a b)").cast(F32)[0:16, q4 * 512:(q4 + 1) * 512], pb[0:16, :])
    P.dma("sp", O["nrs"], ring[0].re("p a b -> p (a b)").cast(F32)[0:16, :], is_out=True)
    P.dma("sp", O["nis"], ring[1].re("p a b -> p (a b)").cast(F32)[0:16, :], is_out=True)
    dbg_out("merged", merged, [128, NT, NTOK], BF16)
    AR.pop()
    P.barrier()
    if stop_after == "A":
        return finish()

    I32 = mybir.dt.int32
    U32 = mybir.dt.uint32
    NSUB = 4
    G = 128 * NSUB
    NSLOT_T = (NTOK * 4) // G + 32
    NTILE = 17
    tiles = [(t * 128, 128) for t in range(16)] + [(L, DS)]
    h2d = Buf(nc.dram_tensor("h2d", [NTOK, D], BF16).ap(), "h2d")
    accd = Buf(nc.dram_tensor("accd", [NTOK, D], F32).ap(), "accd")
    Yd = Buf(nc.dram_tensor("Yd", [NSLOT_T * G, D], F32).ap(), "Yd")
    tokslot = Buf(nc.dram_tensor("tokslot", [NSLOT_T * G, 1], I32).ap(), "tokslot")

    def idma(out, in_, idx, gather, extra_reads=()):
        rec = P.dma("pool", out, in_, extra_reads=[idx] + list(extra_reads))
        o, i, ix = A(out), A(in_), A(idx)
        if gather:
            rec["fn"] = (lambda h: h.indirect_dma_start(out=o, out_offset=None, in_=i,
                                                        in_offset=bass.IndirectOffsetOnAxis(ap=ix, axis=0)))
        else:
            rec["fn"] = (lambda h: h.indirect_dma_start(out=o, out_offset=bass.IndirectOffsetOnAxis(ap=ix, axis=0),
                                                        in_=i, in_offset=None))
        return rec

    g2bc_p = AR.alloc("g2bc_p", [D], F32)
    g2bc_s = AR.alloc("g2bc_s", [D], F32)
    gates = AR.alloc("gates", [NTILE, 4], F32)
    slots_i = AR.alloc("slots_i", [NTILE, 4], I32)
    widx = AR.alloc("widx", [NSLOT_T, NT], I32)
    widx_g = AR.alloc("widx_g", [NSLOT_T, NT], I32)
    widx_u = AR.alloc("widx_u", [NSLOT_T, NT], I32)
    bidx = AR.alloc("bidx", [NSLOT_T], I32)
    iota32 = AR.alloc("iota32", [N_EXP], F32)
    tokid = AR.alloc("tokid", [NTILE], I32)
    P.dma("sp", iota32, I["k_iota32"])
    tokid_f = AR.alloc("tokid_f", [NTILE], F32)
    P.dma("sp", tokid_f, I["k_tokid"])
    P.copy("dve", tokid, tokid_f)

    def make_gbc(dsts, src3, m0):
        AR.push()
        modtm = AR.alloc("modtm", [D], F32, parts=NSEQ)
        selp = AR.alloc("selp", [128], F32, parts=NSEQ)
        sels = AR.alloc("sels", [DS], F32, parts=NSEQ)
        P.dma("sp", selp, I["k_selp"])
        P.dma("sp", sels, I["k_sels"])
        for h4 in range(2):
            pb = bank()
            for d4 in range(4):
                dt = h4 * 4 + d4
                P.tr(pb[0:NSEQ, d4 * 128:(d4 + 1) * 128], src3[:, m0 + dt, :], identf)
            P.copy("dve", modtm[:, h4 * 512:(h4 + 1) * 512], pb[0:NSEQ, :])
        for (dst, sel, R) in ((dsts[0], selp, 128), (dsts[1], sels, DS)):
            for hh in range(2):
                pb = bank()
                P.mm(pb[0:R, :], [(sel[:, 0:R], modtm[:, hh * 512:(hh + 1) * 512])])
                P.copy("act", dst[0:R, hh * 512:(hh + 1) * 512], pb[0:R, :])
        AR.pop()
        P.barrier()

    make_gbc((g2bc_p, g2bc_s), modT, M_G2)

    AR.push()
    oh4 = AR.alloc("oh4", [NTILE, 4, N_EXP], F32)
    pos_all = AR.alloc("pos_all", [NTILE, N_EXP], F32)
    g1bc_p = AR.alloc("g1bc_p", [D], F32)
    g1bc_s = AR.alloc("g1bc_s", [D], F32)
    Abc_p = AR.alloc("Abc_p", [D], F32)
    Abc_s = AR.alloc("Abc_s", [D], F32)
    Bvbc_p = AR.alloc("Bvbc_p", [D], F32)
    Bvbc_s = AR.alloc("Bvbc_s", [D], F32)
    make_gbc((g1bc_p, g1bc_s), modT, M_G1)
    make_gbc((Abc_p, Abc_s), A_T, 0)
    make_gbc((Bvbc_p, Bvbc_s), Bv_T, 0)
    l1g_bc = AR.alloc("l1g_bc", [D], F32)
    l1b_bc = AR.alloc("l1b_bc", [D], F32)
    for dst, nm in ((l1g_bc, "ln1_g"), (l1b_bc, "ln1_b")):
        P.dma("sp", dst, I[nm].partition_broadcast(128))
        P.ts("pool", dst, dst, float(DN_ALPHA), None, op0=ALU.mult)
    wr_bf = AR.alloc("wr_bf", [NT, N_EXP], BF16)
    br_row = AR.alloc("br_row", [N_EXP], F32, parts=1)
    b2_bf = AR.alloc("b2_bf", [D], BF16, parts=N_EXP)
    bout_row = AR.alloc("bout_row", [D], F32, parts=1)
    Ls = AR.alloc("Ls", [128], F32)
    base_row = AR.alloc("base_row", [N_EXP], F32, parts=1)
    P.dma("pool", wr_bf, I["w_router"].rearrange("(kt p) e -> p kt e", p=128))
    P.dma("sp", br_row, I["b_router"].rearrange("(o n) -> o n", o=1))
    P.dma("pool", b2_bf, I["b2"])
    P.dma("sp", bout_row, I["b_out"].rearrange("(o n) -> o n", o=1))
    P.dma("sp", Ls, I["k_ls"])
    P.memset("pool", base_row, 0.0)
    wout = AR.alloc("wout", [NT, D], BF16)
    P.dma("pool", wout, I["w_out"].rearrange("(kt p) c -> p kt c", p=128))
    xtb = [AR.alloc(f"xtb{i}", [D], F32) for i in range(2)]
    accb = [AR.alloc(f"accb{i}", [D], F32) for i in range(2)]
    h2b = [AR.alloc(f"h2b{i}", [D], BF16) for i in range(2)]
    BSETS = []
    for i in range(2):
        BSETS.append(dict(
            tsb=AR.alloc(f"tsb{i}", [D], F32), pre=AR.alloc(f"pre{i}", [D], F32), xn=AR.alloc(f"xn{i}", [D], F32),
            u1=None, h2f=None, h2Tt=AR.alloc(f"h2Tt{i}", [NT, 128], BF16),
            bst=AR.alloc(f"bst{i}", [2, 6], F32), mv=AR.alloc(f"mv{i}", [2], F32), rstd=AR.alloc(f"rstd{i}", [1], F32),
            nmr=AR.alloc(f"nmr{i}", [1], F32), lg=AR.alloc(f"lg{i}", [N_EXP], F32), mx8=AR.alloc(f"mx8{i}", [8], F32),
            ix8=AR.alloc(f"ix8{i}", [8], U32), ixf=AR.alloc(f"ixf{i}", [4], F32), nmx=AR.alloc(f"nmx{i}", [1], F32),
            ex4=AR.alloc(f"ex4{i}", [4], F32), ssum=AR.alloc(f"ssum{i}", [1], F32), cmb3=AR.alloc(f"cmb3{i}", [4, N_EXP], F32),
            comb=AR.alloc(f"comb{i}", [N_EXP], F32), Mk=AR.alloc(f"Mk{i}", [N_EXP], F32),
            combT=AR.alloc(f"combT{i}", [128], BF16, parts=N_EXP)))
    esel = AR.alloc("esel", [NTILE, NTILE], F32)
    selt = AR.alloc("selt", [NTILE, 128], F32, parts=NTILE)
    P.dma("sp", esel.re("p a b -> p (a b)"), I["k_esel"])
    P.dma("sp", selt.re("p a b -> p (a b)"), I["k_selt"])
    cnt_ps = PS[7]
    bank_sets["b0"] = [0, 1, 2]
    bank_sets["b1"] = [3, 4, 5, 6]
    bank_ctr["b0"] = 0
    bank_ctr["b1"] = 0
    b_streams = []
    for ti, (g0, R) in enumerate(tiles):
        BS = BSETS[ti % 2]
        (tsb, pre, xn, u1, h2f, h2Tt, bst, mv, rstd, nmr, lg, mx8, ix8, ixf, nmx, ex4, ssum, cmb3, comb, Mk, combT) = (
            BS["tsb"], BS["pre"], BS["xn"], BS["u1"], BS["h2f"], BS["h2Tt"], BS["bst"], BS["mv"], BS["rstd"], BS["nmr"],
            BS["lg"], BS["mx8"], BS["ix8"], BS["ixf"], BS["nmx"], BS["ex4"], BS["ssum"], BS["cmb3"], BS["comb"], BS["Mk"],
            BS["combT"])
        b_streams.append(P.begin())
        cur_set[0] = "b0" if ti % 2 == 0 else "b1"
        samp = g0 >= L
        xsrc = I["xs"] if samp else I["xp"][g0:g0 + R, :]
        xb = xtb[ti % 2]
        P.dma("sp", xb[0:R, :], xsrc)
        g1bc = g1bc_s if samp else g1bc_p
        g2bc = g2bc_s if samp else g2bc_p
        Abc = Abc_s if samp else Abc_p
        Bvbc = Bvbc_s if samp else Bvbc_p
        for hh in range(2):
            pb = bank()
            o = A(pb[0:R, :])
            prs = [(A(merged[:, k, g0:g0 + R]), A(wout[:, k, hh * 512:(hh + 1) * 512])) for k in range(NT)]
            l1, r1 = A(onesf[0:1, 0:R]), A(bout_row[0:1, hh * 512:(hh + 1) * 512])

            def fn(h, o=o, prs=prs, l1=l1, r1=r1):
                for i, (l, r) in enumerate(prs):
                    h.matmul(o, lhsT=l, rhs=r, start=(i == 0), stop=False)
                return h.matmul(o, lhsT=l1, rhs=r1, start=False, stop=True)
            P.op("pe", fn, [pb], [merged, wout, onesf, bout_row])
            P.tt("dve", tsb[0:R, hh * 512:(hh + 1) * 512], pb[0:R, :], g1bc[0:R, hh * 512:(hh + 1) * 512], ALU.mult)
        P.stt(pre[0:R, :], xb[0:R, :], float(DN_ALPHA), tsb[0:R, :], ALU.mult, ALU.add)
        for hh in range(2):
            P.op("dve", (lambda h, o=A(bst[0:R, hh, :]), i=A(pre[0:R, hh * 512:(hh + 1) * 512]): h.bn_stats(out=o, in_=i)),
                 [bst.k(hh)], [pre])
        P.op("dve", (lambda h, o=A(mv[0:R, :]), i=A(bst[0:R].re("p a b -> p (a b)")): h.bn_aggr(out=o, in_=i)),
             [mv], [bst.k(0), bst.k(1)])
        P.act(rstd[0:R, :], mv[0:R, 1:2], AF.Sqrt, bias=epsc[0:R, 0:1])
        P.op("dve", (lambda h, o=A(rstd[0:R, :]), i=A(rstd[0:R, :]): h.reciprocal(out=o, in_=i)), [rstd], [rstd])
        P.stt(nmr[0:R, :], mv[0:R, 0:1], -1.0, rstd[0:R, :], ALU.mult, ALU.mult)
        P.act(xn[0:R, :], pre[0:R, :], AF.Identity, bias=nmr[0:R, 0:1], scale=rstd[0:R, 0:1])
        ab = accb[ti % 2]
        P.tt("dve", ab[0:R, :], xn[0:R, :], l1g_bc[0:R, :], ALU.mult)
        P.tt("dve", ab[0:R, :], ab[0:R, :], l1b_bc[0:R, :], ALU.add)
        hb_ = h2b[ti % 2]
        P.tt("dve", pre[0:R, :], xn[0:R, :], Abc[0:R, :], ALU.mult)
        P.tt("dve", hb_[0:R, :], pre[0:R, :], Bvbc[0:R, :], ALU.add)
        P.dma("sp", h2d[g0:g0 + R, :].k(ti), hb_[0:R, :])
        pb = bank()
        pbb = pb.cast(BF16)
        for dt in range(NT):
            P.tr(pbb[:, dt * 128:dt * 128 + R], hb_[0:R, dt * 128:(dt + 1) * 128], identb[0:R, 0:R])
        P.copy("act", h2Tt[:, :, 0:R], pbb[:, 0:NT * 128].re("p (d t) -> p d t", d=NT)[:, :, 0:R])
        pb = bank()
        o = A(pb[0:R, 0:N_EXP])
        prs = [(A(h2Tt[:, k, 0:R]), A(wr_bf[:, k, :])) for k in range(NT)]
        l1, r1 = A(onesf[0:1, 0:R]), A(br_row[0:1, :])

        def fn(h, o=o, prs=prs, l1=l1, r1=r1):
            for i, (l, r) in enumerate(prs):
                h.matmul(o, lhsT=l, rhs=r, start=(i == 0), stop=False)
            return h.matmul(o, lhsT=l1, rhs=r1, start=False, stop=True)
        P.op("pe", fn, [pb], [h2Tt, wr_bf, onesf, br_row])
        P.copy("dve", lg[0:R, :], pb[0:R, 0:N_EXP])
        P.op("dve", (lambda h, o=A(mx8[0:R, :]), i=A(lg[0:R, :]): h.max(out=o, in_=i)), [mx8], [lg])
        P.op("dve", (lambda h, o=A(ix8[0:R, :]), m=A(mx8[0:R, :]), i=A(lg[0:R, :]): h.max_index(out=o, in_max=m, in_values=i)),
             [ix8], [mx8, lg])
        P.copy("dve", ixf[0:R, :], ix8[0:R, 0:4])
        P.ts("dve", nmx[0:R, :], mx8[0:R, 0:1], -1.0, None, op0=ALU.mult)
        P.act(ex4[0:R, :], mx8[0:R, 0:4], AF.Exp, bias=nmx[0:R, 0:1])
        P.op("dve", (lambda h, o=A(ssum[0:R, :]), i=A(ex4[0:R, :]): h.reduce_sum(out=o, in_=i, axis=mybir.AxisListType.X)),
             [ssum], [ex4])
        P.op("dve", (lambda h, o=A(ssum[0:R, :]), i=A(ssum[0:R, :]): h.reciprocal(out=o, in_=i)), [ssum], [ssum])
        P.ts("dve", gates[0:R, ti, :].k(ti), ex4[0:R, :], ssum[0:R, 0:1], None, op0=ALU.mult)
        P.tt("dve", oh4[0:R, ti, :, :].k(ti), iota32[0:R, :].un(1).bc([R, 4, N_EXP]),
             ixf[0:R, :].un(2).bc([R, 4, N_EXP]), ALU.is_equal)
        P.tt("dve", cmb3[0:R], oh4[0:R, ti, :, :].k(ti), gates[0:R, ti, :].k(ti).un(2).bc([R, 4, N_EXP]), ALU.mult)
        P.op("dve", (lambda h, o=A(comb[0:R, :]), i=A(cmb3[0:R].re("p k e -> p e k")): h.reduce_sum(out=o, in_=i, axis=mybir.AxisListType.X)),
             [comb], [cmb3])
        P.op("dve", (lambda h, o=A(Mk[0:R, :]), i=A(oh4[0:R, ti, :, :].re("p k e -> p e k")): h.reduce_sum(out=o, in_=i, axis=mybir.AxisListType.X)),
             [Mk], [oh4.k(ti)])
        pb = bank()
        P.mm(pb[0:R, 0:N_EXP], [(Ls[0:R, 0:R], Mk[0:R, :])])
        P.copy("act", pos_all[0:R, ti, :].k(ti), pb[0:R, 0:N_EXP])
        o_, l_, r_ = A(cnt_ps[0:NTILE, 0:N_EXP]), A(esel[0:R, ti, :]), A(Mk[0:R, :])
        P.op("pe", (lambda h, o_=o_, l_=l_, r_=r_, first=(ti == 0), last=(ti == NTILE - 1):
                    h.matmul(o_, lhsT=l_, rhs=r_, start=first, stop=last)), [cnt_ps], [esel, Mk])
        pb = bank()
        P.tr(pb[0:N_EXP, 0:R], comb[0:R, :], identf[0:R, 0:R])
        P.copy("act", combT[:, 0:R], pb[0:N_EXP, 0:R])
        for hh in range(2):
            pb = bank()
            P.mm(pb[0:R, :], [(combT[:, 0:R], b2_bf[:, hh * 512:(hh + 1) * 512])])
            P.tt("dve", tsb[0:R, hh * 512:(hh + 1) * 512], pb[0:R, :], g2bc[0:R, hh * 512:(hh + 1) * 512], ALU.mult)
        P.tt("dve", ab[0:R, :], ab[0:R, :], tsb[0:R, :], ALU.add)
        P.dma("sp", accd[g0:g0 + R, :].k(ti), ab[0:R, :])
        P.end()
        cur_set[0] = "all"
        if ti % 2 == 1 or ti == NTILE - 1:
            P.replay(b_streams)
            b_streams = []
    bank_sets["all"] = [0, 1, 2, 3, 4, 5, 6]
    cnt_all = AR.alloc("cnt_all", [N_EXP], F32, parts=NTILE)
    P.copy("dve", cnt_all, cnt_ps[0:NTILE, 0:N_EXP])
    pb = bank()
    P.mm(pb[0:NTILE, 0:N_EXP], [(Ls[0:NTILE, 0:NTILE], cnt_all)])
    P.mm(pb[0:1, 64:64 + N_EXP], [(onesf[0:NTILE, 0:1], cnt_all)])
    base_all = AR.alloc("base_all", [N_EXP], F32, parts=NTILE)
    P.copy("dve", base_all, pb[0:NTILE, 0:N_EXP])
    P.copy("dve", base_row, pb[0:1, 64:64 + N_EXP])
    for ti, (g0, R) in enumerate(tiles):
        pb = bank()
        P.mm(pb[0:R, 0:N_EXP], [(selt[:, ti, 0:R], base_all)])
        P.tt("dve", pos_all[0:R, ti, :].k(ti), pos_all[0:R, ti, :].k(ti), pb[0:R, 0:N_EXP], ALU.add)
    bank_sets["all"] = list(range(8))
    NE = N_EXP
    qrow = AR.alloc("qrow", [NE], F32, parts=1)
    tmpr = AR.alloc("tmpr", [NE], F32, parts=1)
    incl = AR.alloc("incl", [NE], F32, parts=1)
    strt = AR.alloc("strt", [NE], F32, parts=1)
    onesr = AR.alloc("onesr", [NE], F32, parts=1)
    P.memset("pool", onesr, 1.0)
    P.ts("dve", qrow, base_row, 0.0, None, op0=ALU.is_gt)
    for j in range(1, NTOK // G + 1):
        P.ts("dve", tmpr, base_row, float(G * j), None, op0=ALU.is_gt)
        P.tt("dve", qrow, qrow, tmpr, ALU.add)
    P.ts("dve", qrow, qrow, float(G), None, op0=ALU.mult)
    P.scan(incl, onesr, qrow, onesr[0:1, 0:1].k("z") if False else 0.0)
    P.tt("dve", strt, incl, qrow, ALU.subtract)
    svals = AR.alloc("svals", [NSLOT_T], F32, parts=1)
    P.dma("sp", svals, I["k_svals"])
    cmpb = AR.alloc("cmpb", [NSLOT_T, NE], F32, parts=1)
    P.tt("dve", cmpb, incl[0:1, :].un(1).bc([1, NSLOT_T, NE]), svals[0:1, :].un(2).bc([1, NSLOT_T, NE]), ALU.is_le)
    etile = AR.alloc("etile", [NSLOT_T], F32, parts=1)
    P.op("dve", (lambda h, o=A(etile), i=A(cmpb): h.reduce_sum(out=o, in_=i, axis=mybir.AxisListType.X)), [etile], [cmpb])
    P.ts("dve", etile, etile, float(NE - 1), None, op0=ALU.min)
    pb = bank()
    P.mm(pb[:, 0:NE], [(onesf[0:1, :], strt[0:1, :])])
    P.mm(pb[:, 64:64 + NSLOT_T], [(onesf[0:1, :], etile[0:1, :])])
    start_bc = AR.alloc("start_bc", [NE], F32)
    etile_bc = AR.alloc("etile_bc", [NSLOT_T], F32)
    P.copy("dve", start_bc, pb[:, 0:NE])
    P.copy("dve", etile_bc, pb[:, 64:64 + NSLOT_T])
    iotaK = AR.alloc("iotaK", [NT], F32)
    P.dma("sp", iotaK, I["k_iotak"])
    wf = AR.alloc("wf", [NSLOT_T, NT], F32)
    e1k = AR.alloc("e1k", [NSLOT_T], F32)
    P.ts("dve", e1k, etile_bc, 1024.0, None, op0=ALU.mult)
    P.tt("dve", wf, e1k.un(2).bc([128, NSLOT_T, NT]), iotaK.un(1).bc([128, NSLOT_T, NT]), ALU.add)
    P.copy("dve", widx, wf)
    wf2 = AR.alloc("wf2", [NSLOT_T, NT], F32)
    P.ts("dve", wf2, wf, 2.0, None, op0=ALU.mult)
    P.copy("dve", widx_g, wf2)
    P.ts("dve", wf2, wf2, 1.0, None, op0=ALU.add)
    P.copy("dve", widx_u, wf2)
    bf_ = AR.alloc("bf_", [NSLOT_T], F32)
    P.ts("dve", bf_, etile_bc, 128.0, iotaK[:, 0:1], op0=ALU.mult, op1=ALU.add)
    P.copy("dve", bidx, bf_)
    sfull = AR.alloc("sfull", [NTILE, NE], F32)
    slf = AR.alloc("slf", [NTILE, 4], F32)
    zero_i = AR.alloc("zero_i", [NSLOT_T * NSUB], I32)
    P.memset("pool", zero_i, 0)
    P.dma("sp", tokslot.re("(p j) o -> p (j o)", p=128).k("init"), zero_i)
    allk = [pos_all.k(ti) for ti in range(NTILE)] + [oh4.k(ti) for ti in range(NTILE)]
    P.memset("pool", pos_all[64:128, NTILE - 1, :].k(NTILE - 1), 0.0)
    P.memset("pool", oh4[64:128, NTILE - 1, :, :].k(NTILE - 1), 0.0)
    P.op("dve", (lambda h, o=A(sfull), a=A(pos_all), c=A(start_bc.un(1).bc([128, NTILE, NE])):
                 h.tensor_tensor(out=o, in0=a, in1=c, op=ALU.add)), [sfull], [start_bc] + allk)
    ohk = [oh4.k(ti) for ti in range(NTILE)]
    for k in range(4):
        P.op("dve", (lambda h, o=A(oh4[:, :, k, :]), a=A(oh4[:, :, k, :]), c=A(sfull):
                     h.tensor_tensor(out=o, in0=a, in1=c, op=ALU.mult)), [oh4] + ohk, [sfull] + allk)
    P.op("dve", (lambda h, o=A(slf.re("p t k -> p (t k)")), i=A(oh4.re("p t k e -> p (t k) e")):
                 h.reduce_sum(out=o, in_=i, axis=mybir.AxisListType.X)), [slf], [oh4] + ohk)
    P.op("dve", (lambda h, o=A(slots_i), i=A(slf): h.tensor_copy(out=o, in_=i)),
         [slots_i] + [slots_i.k(ti) for ti in range(NTILE)], [slf])
    sc_keys = []
    for ti, (g0, R) in enumerate(tiles):
        for k in range(4):
            kk = tokslot.k(f"s{ti}_{k}")
            idma(kk, tokid[0:R, ti:ti + 1], slots_i[0:R, ti, k:k + 1].k(ti), gather=False, extra_reads=[tokslot.k("init")])
            sc_keys.append(kk)
    dbg_out("slots", slots_i, [128, NTILE, 4], I32)
    dbg_out("gates", gates, [128, NTILE, 4])
    dbg_out("etile", etile, [1, NSLOT_T])
    AR.pop()
    P.barrier()
    if stop_after == "B":
        return finish()

    AR.limit = AR.n
    AR.push()
    NWR = 6
    wring = [AR.alloc(f"wring{i}", [NT, D], BF16) for i in range(NWR)]
    wix = [0]
    w1rows = I["w1"].rearrange("e k (h f) -> (e k h) f", h=2)
    w2rows = I["w2"].rearrange("e k f -> (e k) f")

    def wgather(rows_ap, s, wi):
        slot = wring[wix[0] % NWR]
        wix[0] += 1
        for kt in range(NT):
            idma(slot[:, kt, :].k(kt), rows_ap, wi[:, s, kt:kt + 1], gather=True)
        return slot

    XT = [AR.alloc(f"XT{i}", [NT, G], BF16) for i in range(2)]
    xg = [AR.alloc(f"xg{i}", [NSUB, D], BF16) for i in range(2)]
    actT = [AR.alloc(f"actT{i}", [NT, G], BF16) for i in range(2)]
    gc = [AR.alloc(f"gc{i}", [G], F32) for i in range(2)]
    sgm = [AR.alloc(f"sgm{i}", [G], F32) for i in range(2)]
    uc = [AR.alloc(f"uc{i}", [G], F32) for i in range(2)]
    tg = [AR.alloc(f"tg{i}", [G], F32) for i in range(2)]
    ysb = [AR.alloc(f"ysb{i}", [D], F32) for i in range(2)]
    tsl = [AR.alloc(f"tsl{i}", [NSUB], I32) for i in range(2)]
    b1c = [AR.alloc(f"b1c{i}", [16], F32) for i in range(2)]
    h2keys = [h2d.k(ti) for ti in range(NTILE)]
    ykeys = []
    loaded = {}

    def issue_load(s):
        b = s % 2
        P.dma("sp", tsl[b], tokslot[s * G:(s + 1) * G, :].re("(p sub) o -> p (sub o)", sub=NSUB), extra_reads=sc_keys)
        for sub in range(NSUB):
            idma(xg[b][:, sub, :].k(sub), h2d, tsl[b][:, sub:sub + 1], gather=True, extra_reads=h2keys)
        idma(b1c[b], I["b1l"], bidx[:, s:s + 1], gather=True)
        loaded[s] = (wgather(w1rows, s, widx_g), wgather(w1rows, s, widx_u), wgather(w2rows, s, widx))

    issue_load(0)
    for s in range(NSLOT_T):
        b = s % 2
        wg_, wu_, w2_ = loaded.pop(s)
        for dt in range(NT):
            pb = bank()
            pbb = pb.cast(BF16)
            for sub in range(NSUB):
                P.tr(pbb[:, sub * 128:(sub + 1) * 128], xg[b][:, sub, dt * 128:(dt + 1) * 128].k(sub), identb)
            P.copy("act" if dt % 2 else "dve", XT[b][:, dt, :], pbb[:, 0:G])
        if s + 1 < NSLOT_T:
            issue_load(s + 1)
        wgk = [wg_.k(kt) for kt in range(NT)]
        wuk = [wu_.k(kt) for kt in range(NT)]
        w2k = [w2_.k(kt) for kt in range(NT)]
        P.ts("dve", b1c[b][:, 8:16], b1c[b][:, 8:16], 1.0, None, op0=ALU.add)
        aT = actT[b]
        for jj in range(NT):
            bb = jj % 2
            pg = bank()
            P.mm(pg[:, 0:G], [(wg_[:, k, jj * 128:(jj + 1) * 128], XT[b][:, k, :]) for k in range(NT)], extra_ins=wgk)
            pu = bank()
            P.mm(pu[:, 0:G], [(wu_[:, k, jj * 128:(jj + 1) * 128], XT[b][:, k, :]) for k in range(NT)], extra_ins=wuk)
            P.ts("dve", gc[bb], pg[:, 0:G], b1c[b][:, jj:jj + 1], 7.0, op0=ALU.add, op1=ALU.min)
            P.act(sgm[bb], gc[bb], AF.Sigmoid, scale=1.702)
            P.ts("dve", uc[bb], pu[:, 0:G], b1c[b][:, 8 + jj:9 + jj], 8.0, op0=ALU.add, op1=ALU.min)
            P.tt("dve", tg[bb], gc[bb], sgm[bb], ALU.mult)
            P.stt(aT[:, jj, :], uc[bb], -6.0, tg[bb], ALU.max, ALU.mult)
        for sub in range(NSUB):
            yb_ = ysb[sub % 2]
            for hh in range(2):
                pb = bank()
                P.mm(pb[:, :], [(aT[:, k, sub * 128:(sub + 1) * 128], w2_[:, k, hh * 512:(hh + 1) * 512]) for k in range(NT)],
                     extra_ins=w2k)
                P.copy("act", yb_[:, hh * 512:(hh + 1) * 512], pb[:, :])
            yk_ = Yd.k(f"{s}_{sub}")
            P.dma("sp", Buf(A(Yd)[s * G:(s + 1) * G, :].rearrange("(p sub) d -> p sub d", sub=NSUB)[:, sub, :], yk_.key), yb_)
            ykeys.append(yk_)
    AR.pop()
    P.barrier()

    AR.push()
    l2g_bc = AR.alloc("l2g_bc", [D], F32)
    l2b_bc = AR.alloc("l2b_bc", [D], F32)
    P.dma("sp", l2g_bc, I["ln2_g"].partition_broadcast(128))
    P.dma("sp", l2b_bc, I["ln2_b"].partition_broadcast(128))
    bst = AR.alloc("bst2", [2, 6], F32)
    mv = AR.alloc("mv2", [2], F32)
    rstd = AR.alloc("rstd2", [1], F32)
    nmr = AR.alloc("nmr2", [1], F32)
    accs = [AR.alloc(f"accs{i}", [D], F32) for i in range(2)]
    ygk2 = [[AR.alloc(f"ygk{j}_{i}", [D], F32) for i in range(4)] for j in range(2)]
    tq = [AR.alloc(f"tq{i}", [D], F32) for i in range(2)]
    yb = [AR.alloc(f"yb{i}", [D], F32) for i in range(2)]
    y2 = [AR.alloc(f"y2{i}", [D], F32) for i in range(2)]
    for ti, (g0, R) in enumerate(tiles):
        g2bc = g2bc_s if g0 >= L else g2bc_p
        av = accs[ti % 2]
        ygk = ygk2[ti % 2]
        P.dma("sp", av[0:R, :], accd[g0:g0 + R, :].k(ti))
        for k in range(4):
            idma(ygk[k][0:R, :], Yd, slots_i[0:R, ti, k:k + 1].k(ti), gather=True, extra_reads=ykeys)
        t_ = tq[ti % 2]
        P.ts("dve", t_[0:R, :], ygk[0][0:R, :], gates[0:R, ti, 0:1].k(ti), None, op0=ALU.mult)
        for k in range(1, 4):
            P.stt(t_[0:R, :], ygk[k][0:R, :], gates[0:R, ti, k:k + 1].k(ti), t_[0:R, :], ALU.mult, ALU.add)
        P.tt("dve", t_[0:R, :], t_[0:R, :], g2bc[0:R, :], ALU.mult)
        P.tt("dve", av[0:R, :], av[0:R, :], t_[0:R, :], ALU.add)
        for hh in range(2):
            P.op("dve", (lambda h, o=A(bst[0:R, hh, :]), i=A(av[0:R, hh * 512:(hh + 1) * 512]): h.bn_stats(out=o, in_=i)),
                 [bst.k(hh)], [av])
        P.op("dve", (lambda h, o=A(mv[0:R, :]), i=A(bst[0:R].re("p a b -> p (a b)")): h.bn_aggr(out=o, in_=i)),
             [mv], [bst.k(0), bst.k(1)])
        P.act(rstd[0:R, :], mv[0:R, 1:2], AF.Sqrt, bias=epsc[0:R, 0:1])
        P.op("dve", (lambda h, o=A(rstd[0:R, :]), i=A(rstd[0:R, :]): h.reciprocal(out=o, in_=i)), [rstd], [rstd])
        P.stt(nmr[0:R, :], mv[0:R, 0:1], -1.0, rstd[0:R, :], ALU.mult, ALU.mult)
        y_ = yb[ti % 2]
        z_ = y2[ti % 2]
        P.act(y_[0:R, :], av[0:R, :], AF.Identity, bias=nmr[0:R, 0:1], scale=rstd[0:R, 0:1])
        P.tt("dve", z_[0:R, :], y_[0:R, :], l2g_bc[0:R, :], ALU.mult)
        P.tt("dve", z_[0:R, :], z_[0:R, :], l2b_bc[0:R, :], ALU.add)
        dst = O["ys"] if g0 >= L else O["yp"][g0:g0 + R, :]
        P.dma("sp", dst, z_[0:R, :], is_out=True)
    AR.pop()
    print("arena peak bytes", AR.peak, "ops", {e: len(P.ops[e]) for e in ENGS})
    return finish()


def _consts():
    ident = np.eye(128, dtype=np.float32)
    selu = np.zeros((128, 16, 128), np.float32)
    sely = np.zeros((128, 16, 128), np.float32)
    for lp in range(4):
        for s in range(4):
            for r in range(32):
                selu[lp * 32 + r, lp * 4 + s, s * 32 + r] = 1.0
                sely[s * 32 + r, lp * 4 + s, lp * 32 + r] = 1.0
    selp = np.zeros((NSEQ, 128), np.float32)
    selp[0, :] = 1.0
    sels = np.zeros((NSEQ, DS), np.float32)
    for s in range(16):
        sels[1 + s, 4 * s:4 * s + 4] = 1.0
    ls = np.triu(np.ones((128, 128), np.float32), 1)
    iota32 = np.tile(np.arange(32, dtype=np.float32)[None, :], (128, 1))
    tokid = np.zeros((128, 17), np.float32)
    for ti in range(16):
        tokid[:, ti] = ti * 128 + np.arange(128)
    tokid[:, 16] = 2048 + np.arange(128)
    svals = (512.0 * np.arange(48, dtype=np.float32))[None, :]
    iotak = (128.0 * np.arange(8, dtype=np.float32))[None, :] + np.arange(128, dtype=np.float32)[:, None]
    iotae = np.arange(32, dtype=np.float32)[:, None]
    esel = np.zeros((128, 17, 17), np.float32)
    selt = np.zeros((17, 17, 128), np.float32)
    for ti in range(17):
        esel[:, ti, ti] = 1.0
        selt[ti, ti, :] = 1.0
    return dict(k_ident=ident, k_selu=selu.reshape(128, -1), k_sely=sely.reshape(128, -1), k_selp=selp, k_sels=sels,
                k_ls=ls, k_iota32=iota32, k_tokid=tokid, k_svals=svals, k_iotak=iotak, k_iotae=iotae,
                k_esel=esel.reshape(128, -1), k_selt=selt.reshape(17, -1))


_NC_CACHE = {}


def make_in_maps(inputs):
    f = lambda a: np.ascontiguousarray(np.asarray(a, dtype=np.float32))
    consts = _consts()
    shared = {}
    for nm in ("w_ada", "b_ada", "w_in", "b_in", "w_dw", "b_dw", "conv_ln_g", "conv_ln_b", "w_pw", "b_pw",
               "lam_re", "lam_im", "log_dt", "b_re", "b_im", "c_re", "c_im", "d_skip", "w_sv", "b_sv", "w_sg", "b_sg",
               "w_out", "b_out", "ln1_g", "ln1_b", "w_router", "b_router", "w1", "w2", "b2", "ln2_g", "ln2_b"):
        shared[nm] = f(inputs[nm][0])
    shared["b1l"] = np.ascontiguousarray(f(inputs["b1"][0]).reshape(32, 16, 128).transpose(0, 2, 1).reshape(32 * 128, 16))
    shared.update(consts)
    xp, xs = f(inputs["x_prompt"]), f(inputs["x_sample"])
    cp, cs = f(inputs["c_prompt"]), f(inputs["c_sample"])
    sc, sr, si = f(inputs["state_conv"][0]), f(inputs["state_ssm_re"][0]), f(inputs["state_ssm_im"][0])
    maps = []
    for i in range(8):
        m = dict(shared)
        m["xp"] = xp[i]
        m["xs"] = np.ascontiguousarray(xs[16 * i:16 * i + 16].reshape(DS, D))
        m["cc"] = np.ascontiguousarray(np.concatenate([cp[i:i + 1], cs[16 * i:16 * i + 16]], axis=0))
        m["sconv"] = np.ascontiguousarray(sc[16 * i:16 * i + 16])
        m["sre"] = np.ascontiguousarray(sr[16 * i:16 * i + 16].reshape(16, 2048))
        m["sim"] = np.ascontiguousarray(si[16 * i:16 * i + 16].reshape(16, 2048))
        maps.append(m)
    return maps


def assemble(results):
    yp = np.stack([np.asarray(r["yp"], np.float32) for r in results], 0)
    ys = np.concatenate([np.asarray(r["ys"], np.float32).reshape(16, 4, D) for r in results], 0)
    ncp = np.stack([np.asarray(r["ncp"], np.float32) for r in results], 0)[None]
    nrp = np.stack([np.asarray(r["nrp"], np.float32).reshape(32, 64) for r in results], 0)[None]
    nip = np.stack([np.asarray(r["nip"], np.float32).reshape(32, 64) for r in results], 0)[None]
    ncs = np.concatenate([np.asarray(r["ncs"], np.float32) for r in results], 0)[None]
    nrs = np.concatenate([np.asarray(r["nrs"], np.float32).reshape(16, 32, 64) for r in results], 0)[None]
    nis = np.concatenate([np.asarray(r["nis"], np.float32).reshape(16, 32, 64) for r in results], 0)[None]
    return (yp, ys, ncp, nrp, nip, ncs, nrs, nis)


def kernel(**inputs):
    if "nc" not in _NC_CACHE:
        _NC_CACHE["nc"] = build_program()
    nc = _NC_CACHE["nc"]
    maps = make_in_maps(inputs)
    res = run_bass_kernel_spmd(nc, maps, core_ids=list(range(8)))
    return assemble(res.results)
```

```python
import contextlib
import math
import numpy as np
import concourse.bass as bass
import concourse.mybir as mybir
from concourse.bass_utils import run_bass_kernel_spmd

F32 = mybir.dt.float32
BF16 = mybir.dt.bfloat16
U8 = mybir.dt.uint8
AF = mybir.ActivationFunctionType
ALU = mybir.AluOpType
DTSIZE = {F32: 4, BF16: 2, U8: 1, mybir.dt.int32: 4, mybir.dt.uint32: 4}

ENGS = ("pe", "act", "dve", "pool", "sp")
SEM_ROLL = 30000
NDMA_SEM = 20

D = 1024
NT = 8
L = 2048
NB = 256
NBLK = L // NB
CH = NB // 4
DS = 64
NSEQ = 17
NTOK = L + DS
DN_ALPHA = 2.0 ** 0.25
LN_EPS = 1e-5
N_EXP = 32


class Buf:
    __slots__ = ("ap", "key")

    def __init__(self, ap, key):
        self.ap = ap
        self.key = key

    def __getitem__(self, idx):
        return Buf(self.ap[idx], self.key)

    def re(self, pat, **kw):
        return Buf(self.ap.rearrange(pat, **kw), self.key)

    def bc(self, shape):
        return Buf(self.ap.to_broadcast(list(shape)), self.key)

    def un(self, d):
        return Buf(self.ap.unsqueeze(d), self.key)

    def cast(self, dt):
        return Buf(self.ap.bitcast(dt), self.key)

    def k(self, suffix):
        return Buf(self.ap, f"{self.key}.{suffix}")


def A(x):
    return x.ap if isinstance(x, Buf) else x


class Prog:
    def __init__(self, nc):
        self.nc = nc
        self.ops = {e: [] for e in ENGS}
        self.last_w = {}
        self.readers = {}
        self.ndma = {e: 0 for e in ENGS}
        self.out_dmas = []
        self.open_dmas = set()
        self.pending = {e: set() for e in ENGS}
        self.rr = 0
        self._cap = None

    def begin(self):
        self._cap = []
        return self._cap

    def end(self):
        self._cap = None

    def replay(self, streams):
        pos = [0] * len(streams)
        total = sum(len(x) for x in streams)
        for _ in range(total):
            best, bi = None, None
            for i, x in enumerate(streams):
                if pos[i] < len(x):
                    frac = pos[i] / len(x)
                    if best is None or frac < best:
                        best, bi = frac, i
            kind, args, kw = streams[bi][pos[bi]]
            pos[bi] += 1
            if kind == "op":
                self.op(*args, **kw)
            else:
                self.dma(*args, **kw)

    def _deps(self, reads, writes):
        deps = set()
        for k in reads:
            if k in self.last_w:
                deps.add(self.last_w[k])
        for k in writes:
            if k in self.last_w:
                deps.add(self.last_w[k])
            for r in self.readers.get(k, ()):
                deps.add(r)
        return deps

    def _commit(self, ident, reads, writes):
        for k in reads:
            self.readers.setdefault(k, []).append(ident)
        for k in writes:
            self.last_w[k] = ident
            self.readers[k] = []

    def _keys(self, outs, ins):
        reads = [b.key for b in ins if isinstance(b, Buf)]
        writes = [b.key for b in outs if isinstance(b, Buf)]
        writes += [k for k in reads if k.startswith("ps")]
        return reads, writes

    def op(self, eng, fn, outs=(), ins=()):
        if self._cap is not None:
            self._cap.append(("op", (eng, fn, list(outs), list(ins)), {}))
            return {}
        reads, writes = self._keys(outs, ins)
        idx = len(self.ops[eng])
        deps = self._deps(reads, writes)
        deps |= self.pending[eng]
        self.pending[eng] = set()
        for d in deps:
            self.open_dmas.discard(d)
        rec = dict(kind="op", fn=fn, deps=deps, signal=False, eng=eng, idx=idx)
        self.ops[eng].append(rec)
        self._commit((eng, idx), reads, writes)
        return rec

    def dma(self, q, out, in_, is_out=False, extra_reads=(), **kw):
        if self._cap is not None:
            self._cap.append(("dma", (q, out, in_), dict(is_out=is_out, extra_reads=list(extra_reads), **kw)))
            return {}
        reads, writes = self._keys([out], [in_] + list(extra_reads))
        deps = self._deps(reads, writes)
        deps |= self.pending[q]
        self.pending[q] = set()
        for d in deps:
            self.open_dmas.discard(d)
        n = self.ndma[q]
        self.ndma[q] += 1
        did = ("dma", q, n)
        if n >= NDMA_SEM:
            deps.add(("dma", q, n - NDMA_SEM))
        o, i = A(out), A(in_)
        rec = dict(kind="dma", fn=(lambda h: h.dma_start(out=o, in_=i, **kw)), deps=deps, q=q, n=n,
                   eng=q, idx=len(self.ops[q]))
        self.ops[q].append(rec)
        self._commit(did, reads, writes)
        self.open_dmas.add(did)
        if is_out:
            self.out_dmas.append(did)
        return rec

    def barrier(self):
        deps = set(self.open_dmas)
        for e in ENGS:
            n = len(self.ops[e])
            for i in range(n - 1, -1, -1):
                if self.ops[e][i]["kind"] == "op":
                    deps.add((e, i))
                    break
        for e in ENGS:
            self.pending[e] |= deps
        self.open_dmas = set()

    def act(self, out, in_, func, bias=None, scale=None):
        kw = {}
        ins = [in_]
        if bias is not None:
            kw["bias"] = A(bias)
            ins.append(bias)
        if scale is not None:
            kw["scale"] = A(scale)
            ins.append(scale)
        o, i = A(out), A(in_)
        return self.op("act", lambda h: h.activation(out=o, in_=i, func=func, **kw), [out], ins)

    def tt(self, eng, out, in0, in1, op):
        o, a, b = A(out), A(in0), A(in1)
        return self.op(eng, lambda h: h.tensor_tensor(out=o, in0=a, in1=b, op=op), [out], [in0, in1])

    def ts(self, eng, out, in0, s1, s2=None, op0=ALU.mult, op1=None):
        o, a = A(out), A(in0)
        x1, x2 = A(s1), A(s2)
        if op1 is None:
            fn = lambda h: h.tensor_scalar(out=o, in0=a, scalar1=x1, scalar2=None, op0=op0)
        else:
            fn = lambda h: h.tensor_scalar(out=o, in0=a, scalar1=x1, scalar2=x2, op0=op0, op1=op1)
        return self.op(eng, fn, [out], [in0, s1, s2])

    def stt(self, out, in0, scalar, in1, op0, op1):
        o, a, s, b = A(out), A(in0), A(scalar), A(in1)
        return self.op("dve", lambda h: h.scalar_tensor_tensor(out=o, in0=a, scalar=s, in1=b, op0=op0, op1=op1),
                       [out], [in0, scalar, in1])

    def copy(self, eng, out, in_):
        o, i = A(out), A(in_)
        if eng == "act":
            return self.op("act", lambda h: h.copy(out=o, in_=i), [out], [in_])
        return self.op(eng, lambda h: h.tensor_copy(out=o, in_=i), [out], [in_])

    def memset(self, eng, out, val):
        o = A(out)
        return self.op(eng, lambda h: h.memset(o, val), [out], [])

    def mm(self, out, pairs, extra_ins=()):
        o = A(out)
        prs = [(A(l), A(r)) for l, r in pairs]
        n = len(prs)

        def fn(h):
            ins = None
            for i, (l, r) in enumerate(prs):
                ins = h.matmul(o, lhsT=l, rhs=r, start=(i == 0), stop=(i == n - 1))
            return ins
        ins_b = [x for pr in pairs for x in pr] + list(extra_ins)
        return self.op("pe", fn, [out], ins_b)

    def tr(self, out, in_, ident):
        o, i, idn = A(out), A(in_), A(ident)
        return self.op("pe", lambda h: h.transpose(out=o, in_=i, identity=idn), [out], [in_, ident])

    def scan(self, out, d0, d1, init):
        o, a, b, c = A(out), A(d0), A(d1), A(init)
        return self.op("dve", lambda h: h.tensor_tensor_scan(out=o, data0=a, data1=b, initial=c,
                                                             op0=ALU.mult, op1=ALU.add), [out], [d0, d1, init])

    def emit(self, stack):
        nc = self.nc
        for e in ENGS:
            for rec in self.ops[e]:
                for d in rec["deps"]:
                    if d[0] == "dma":
                        continue
                    de, di = d
                    if de == e and rec["kind"] == "op" and e == "pe":
                        continue
                    self.ops[de][di]["signal"] = True
        self.sems = {}
        for e in ENGS:
            cnt = 0
            for rec in self.ops[e]:
                if rec["kind"] == "op" and rec["signal"]:
                    rec["semi"] = cnt // SEM_ROLL
                    rec["semv"] = cnt % SEM_ROLL + 1
                    cnt += 1
            ns = max(1, (cnt + SEM_ROLL - 1) // SEM_ROLL)
            self.sems[e] = [stack.enter_context(nc.semaphore(f"s_{e}_{i}")) for i in range(ns)]
        self.dsems = {}
        for q in ENGS:
            if self.ndma[q]:
                self.dsems[q] = [stack.enter_context(nc.semaphore(f"d_{q}_{i}"))
                                 for i in range(min(NDMA_SEM, self.ndma[q]))]
        block = stack.enter_context(nc.Block())
        prog = self

        def run_engine(e, h):
            waited = {x: -1 for x in ENGS}
            waited_dma = set()
            for rec in prog.ops[e]:
                need = {}
                for d in rec["deps"]:
                    if d[0] == "dma":
                        if d in waited_dma:
                            continue
                        waited_dma.add(d)
                        _, q, n = d
                        h.wait_ge(prog.dsems[q][n % NDMA_SEM], 16 * (n // NDMA_SEM + 1))
                        continue
                    de, di = d
                    if de == e and rec["kind"] == "op" and e == "pe":
                        continue
                    if di <= waited[de]:
                        continue
                    need[de] = max(need.get(de, -1), di)
                for de, di in need.items():
                    src = prog.ops[de][di]
                    h.wait_ge(prog.sems[de][src["semi"]], src["semv"])
                    waited[de] = di
                ins = rec["fn"](h)
                if rec["kind"] == "dma":
                    ins.then_inc(prog.dsems[rec["q"]][rec["n"] % NDMA_SEM], 16)
                elif rec["signal"]:
                    ins.then_inc(prog.sems[e][rec["semi"]], 1)
            if e == "sp":
                for d in prog.out_dmas:
                    if d in waited_dma:
                        continue
                    _, q, n = d
                    h.wait_ge(prog.dsems[q][n % NDMA_SEM], 16 * (n // NDMA_SEM + 1))

        @block.tensor
        def _(h):
            run_engine("pe", h)

        @block.scalar
        def _(h):
            run_engine("act", h)

        @block.vector
        def _(h):
            run_engine("dve", h)

        @block.gpsimd
        def _(h):
            run_engine("pool", h)

        @block.sync
        def _(h):
            run_engine("sp", h)


class Arena:
    def __init__(self, nc, stack, nbytes):
        self.t = stack.enter_context(nc.sbuf_tensor("arena", [128, nbytes], U8))
        self.n = nbytes
        self.limit = nbytes
        self.off = 0
        self.stk = []
        self.peak = 0
        self.cnt = 0

    def alloc(self, key, shape, dt, parts=128):
        n = int(np.prod(shape)) * DTSIZE[dt]
        n_al = (n + 63) // 64 * 64
        assert self.off + n_al <= self.limit, (key, self.off, n_al, self.limit)
        ap = self.t[0:parts, self.off:self.off + n].bitcast(dt)
        if len(shape) > 1:
            names = [f"d{i}" for i in range(len(shape))]
            pat = "p (" + " ".join(names) + ") -> p " + " ".join(names)
            ap = ap.rearrange(pat, **{nm: s for nm, s in zip(names[1:], shape[1:])})
        self.off += n_al
        self.peak = max(self.peak, self.off)
        self.cnt += 1
        return Buf(ap, f"{key}#{self.cnt}")

    def alloc_top(self, key, shape, dt, parts=128):
        n = int(np.prod(shape)) * DTSIZE[dt]
        n_al = (n + 63) // 64 * 64
        self.limit -= n_al
        save = self.off
        self.off = self.limit
        lim = self.limit
        self.limit = self.n
        b = self.alloc(key, shape, dt, parts)
        self.limit = lim
        self.off = save
        return b

    def push(self):
        self.stk.append(self.off)

    def pop(self):
        self.off = self.stk.pop()


IN_SPECS = [
    ("xp", [L, D]), ("xs", [DS, D]), ("cc", [NSEQ, D]), ("sconv", [16, 30, 512]),
    ("sre", [16, 2048]), ("sim", [16, 2048]),
    ("w_ada", [D, 6 * D]), ("b_ada", [6 * D]), ("w_in", [D, 3584]), ("b_in", [3584]),
    ("w_dw", [31, 512]), ("b_dw", [512]), ("conv_ln_g", [512]), ("conv_ln_b", [512]),
    ("w_pw", [512, D]), ("b_pw", [D]), ("lam_re", [32, 64]), ("lam_im", [32, 64]), ("log_dt", [32]),
    ("b_re", [32, 64, 16]), ("b_im", [32, 64, 16]), ("c_re", [32, 16, 64]), ("c_im", [32, 16, 64]),
    ("d_skip", [32, 16]), ("w_sv", [512, D]), ("b_sv", [D]), ("w_sg", [512, D]), ("b_sg", [D]),
    ("w_out", [D, D]), ("b_out", [D]), ("ln1_g", [D]), ("ln1_b", [D]), ("w_router", [D, 32]),
    ("b_router", [32]), ("w1", [32, D, 2 * D]), ("b1l", [32 * 128, 16]), ("w2", [32, D, D]), ("b2", [32, D]),
    ("ln2_g", [D]), ("ln2_b", [D]),
    ("k_ident", [128, 128]), ("k_selu", [128, 16 * 128]), ("k_sely", [128, 16 * 128]),
    ("k_selp", [NSEQ, 128]), ("k_sels", [NSEQ, DS]),
    ("k_ls", [128, 128]), ("k_iota32", [128, 32]), ("k_tokid", [128, 17]), ("k_svals", [1, 48]),
    ("k_iotak", [128, 8]), ("k_iotae", [32, 1]), ("k_esel", [128, 17 * 17]), ("k_selt", [17, 17 * 128]),
]
OUT_SPECS = [
    ("yp", [L, D]), ("ys", [DS, D]), ("ncp", [30, 512]), ("nrp", [16, 128]), ("nip", [16, 128]),
    ("ncs", [16, 30, 512]), ("nrs", [16, 2048]), ("nis", [16, 2048]),
]
C_BIN, C_BDW, C_CG, C_CB, C_BPW, C_BSV, C_BSG, C_BADA, C_L1G, C_L1B = 0, 28, 32, 36, 40, 48, 56, 64, 112, 120
M_SH1, M_SC1, M_G1, M_SH2, M_SC2, M_G2 = 0, 8, 16, 24, 32, 40


def build_program(dbg=(), stop_after=None):
    nc = bass.Bass("TRN2", target_bir_lowering=False)
    P = Prog(nc)
    I = {n: nc.dram_tensor(n, list(s), F32, kind="ExternalInput").ap() for n, s in IN_SPECS}
    O = {n: nc.dram_tensor(n, list(s), F32, kind="ExternalOutput").ap() for n, s in OUT_SPECS}
    st = contextlib.ExitStack()
    st.__enter__()
    AR = Arena(nc, st, 204 * 1024)
    PS = [Buf(st.enter_context(nc.psum_tensor(f"psb{i}", [128, 512], F32))[:], f"ps{i}") for i in range(8)]
    psi = [0]
    bank_sets = {"all": list(range(8)), "conv": [0, 1, 2, 7], "s5a": [3, 4], "s5b": [5, 6]}
    bank_ctr = {k: 0 for k in bank_sets}
    cur_set = ["all"]

    def bank():
        st_ = cur_set[0]
        lst = bank_sets[st_]
        b = PS[lst[bank_ctr[st_] % len(lst)]]
        bank_ctr[st_] += 1
        return b

    def dbg_out(name, buf, shape, dt=F32):
        if name in dbg:
            t = nc.dram_tensor("dbg_" + name, list(shape), dt, kind="ExternalOutput").ap()
            P.dma("sp", t, buf, is_out=True)

    def finish():
        P.emit(st)
        st.close()
        return nc

    identf = AR.alloc("identf", [128], F32)
    identb = AR.alloc("identb", [128], BF16)
    onesf = AR.alloc("onesf", [128], F32)
    colsT = AR.alloc("colsT", [128], F32)
    modT = AR.alloc("modT", [48, NSEQ], F32)
    A_T = AR.alloc("A_T", [NT, NSEQ], F32)
    Bv_T = AR.alloc("Bv_T", [NT, NSEQ], F32)
    b1T = AR.alloc("b1T", [16, N_EXP], F32)
    merged = AR.alloc_top("merged", [NT, NTOK], BF16)
    epsc = AR.alloc("epsc", [1], F32)

    P.dma("sp", identf, I["k_ident"])
    P.copy("dve", identb, identf)
    P.memset("pool", onesf, 1.0)
    P.memset("pool", epsc, LN_EPS)

    AR.push()
    selu = AR.alloc("selu", [16, 128], BF16)
    sely = AR.alloc("sely", [16, 128], BF16)
    P.dma("pool", selu.re("p a b -> p (a b)"), I["k_selu"])
    P.dma("pool", sely.re("p a b -> p (a b)"), I["k_sely"])
    ring = [AR.alloc(f"ring{i}", [NT, 512], BF16) for i in range(3)]
    rix = [0]

    def ring_load(src_ap, view4=False):
        slot = ring[rix[0] % len(ring)]
        rix[0] += 1
        if view4:
            dst = slot.re("p a b -> p (a b)").re("p (k c) -> p k c", k=4)
        else:
            dst = slot
        P.dma("pool", dst, src_ap)
        return dst

    wdwT = AR.alloc("wdwT", [4, 31], F32)
    Kmat = AR.alloc("Kmat", [16, 128], BF16)
    CexpA = AR.alloc("CexpA", [16, 4, 2, 16], BF16)
    CexpB = AR.alloc("CexpB", [16, 4, 2, 16], BF16)
    BexpRe = AR.alloc("BexpRe", [16, 128], BF16)
    BexpIm = AR.alloc("BexpIm", [16, 128], BF16)
    Tre = AR.alloc("Tre", [16, CH], F32)
    Tim = AR.alloc("Tim", [16, CH], F32)
    dcol = AR.alloc("dcol", [16], F32)
    carry = AR.alloc("carry", [16, 2], F32)
    h0sb = AR.alloc("h0sb", [16, 2, 16], F32)
    rot0 = AR.alloc("rot0", [16, 2, 16], F32)
    Hsh_s = AR.alloc("Hsh_s", [16, 2, 16], BF16)
    hend_s = AR.alloc("hend_s", [16, 2, 16], F32)
    full_s = AR.alloc("full_s", [4, 16, 34], F32)
    P.memset("pool", carry, 0.0)

    AR.push()
    stg = AR.alloc("stg", [128], F32)
    stg_parts = []
    for nm, r0, J in (("b_in", C_BIN, 28), ("b_dw", C_BDW, 4), ("conv_ln_g", C_CG, 4), ("conv_ln_b", C_CB, 4),
                      ("b_pw", C_BPW, 8), ("b_sv", C_BSV, 8), ("b_sg", C_BSG, 8), ("b_ada", C_BADA, 48),
                      ("ln1_g", C_L1G, 8), ("ln1_b", C_L1B, 8)):
        sub = stg[r0:r0 + J, :].k(nm)
        P.dma("sp", sub, I[nm].rearrange("(j p) -> j p", p=128))
        stg_parts.append(sub)
    pb = bank()
    P.op("pe", (lambda h, o=A(pb[:, 0:128]), i=A(stg), idn=A(identf): h.transpose(out=o, in_=i, identity=idn)),
         [pb], [identf] + stg_parts)
    P.copy("dve", colsT, pb[:, 0:128])

    cc_sb = AR.alloc("cc_sb", [D], F32, parts=NSEQ)
    P.dma("sp", cc_sb, I["cc"])
    csl = AR.alloc("csl", [D], F32, parts=NSEQ)
    P.act(csl, cc_sb, AF.Silu)
    siluT = AR.alloc("siluT", [NT, NSEQ], BF16)
    pb = bank()
    for k in range(NT):
        P.tr(pb[:, k * NSEQ:(k + 1) * NSEQ], csl[:, k * 128:(k + 1) * 128], identf[0:NSEQ, 0:NSEQ])
    P.copy("dve", siluT.re("p a b -> p (a b)"), pb[:, 0:NT * NSEQ])

    for c in range(12):
        slot = ring_load(I["w_ada"][:, c * 512:(c + 1) * 512].rearrange("(kt p) c -> p kt c", p=128))
        pb = bank()
        for jj in range(4):
            P.mm(pb[:, jj * NSEQ:(jj + 1) * NSEQ],
                 [(slot[:, k, jj * 128:(jj + 1) * 128], siluT[:, k, :]) for k in range(NT)])
        P.tt("dve", modT[:, 4 * c:4 * c + 4, :], pb[:, 0:4 * NSEQ].re("p (a b) -> p a b", a=4),
             colsT[:, C_BADA + 4 * c:C_BADA + 4 * c + 4].un(2).bc([128, 4, NSEQ]), ALU.add)
    P.ts("dve", modT[:, M_SC1:M_SC1 + 8, :], modT[:, M_SC1:M_SC1 + 8, :], 1.0, None, op0=ALU.add)
    P.ts("dve", modT[:, M_SC2:M_SC2 + 8, :], modT[:, M_SC2:M_SC2 + 8, :], 1.0, None, op0=ALU.add)
    P.tt("dve", A_T, modT[:, M_SC2:M_SC2 + 8, :], colsT[:, C_L1G:C_L1G + 8].un(2).bc([128, NT, NSEQ]), ALU.mult)
    P.tt("dve", Bv_T, modT[:, M_SC2:M_SC2 + 8, :], colsT[:, C_L1B:C_L1B + 8].un(2).bc([128, NT, NSEQ]), ALU.mult)
    P.tt("dve", Bv_T, Bv_T, modT[:, M_SH2:M_SH2 + 8, :], ALU.add)
    dbg_out("modT", modT, [128, 48, NSEQ])

    stgw = AR.alloc("stgw", [512], F32, parts=31)
    P.dma("sp", stgw, I["w_dw"])
    pb = bank()
    for ct in range(4):
        P.tr(pb[:, ct * 31:(ct + 1) * 31], stgw[:, ct * 128:(ct + 1) * 128], identf[0:31, 0:31])
    P.copy("dve", wdwT.re("p a b -> p (a b)"), pb[:, 0:124])

    stgc = AR.alloc("stgc", [4, 512], F32, parts=30)
    for sq in range(4):
        P.dma("sp", stgc, I["sconv"][4 * sq:4 * sq + 4].rearrange("s r c -> r s c"))
        for ct in range(4):
            pb = bank()
            for s4 in range(4):
                P.tr(pb[:, s4 * 30:(s4 + 1) * 30], stgc[:, s4, ct * 128:(ct + 1) * 128], identf[0:30, 0:30])
            P.copy("dve", full_s[:, ct, 4 * sq:4 * sq + 4, 0:30], pb[:, 0:120].re("p (s r) -> p s r", s=4))
    P.dma("sp", O["ncs"][:, 0:26, :], I["sconv"][:, 4:30, :], is_out=True)

    stg2 = AR.alloc("stg2", [128], F32, parts=64)
    P.dma("sp", stg2[0:16, :].k("a"), I["lam_re"].rearrange("(q g) p -> q (g p)", g=2))
    P.dma("sp", stg2[16:32, :].k("b"), I["lam_im"].rearrange("(q g) p -> q (g p)", g=2))
    ldt_raw = AR.alloc("ldt_raw", [2], F32, parts=48)
    P.dma("sp", ldt_raw[32:48, :], I["log_dt"].rearrange("(q g) -> q g", g=2))
    P.copy("dve", stg2[32:48, :].k("c").re("q (g p) -> q g p", g=2), ldt_raw[32:48, :].un(2).bc([16, 2, 64]))
    P.dma("sp", stg2[48:64, :].k("d").re("q (s m) -> q s m", s=4),
          I["d_skip"].rearrange("(q g) h -> q (g h)", g=2).unsqueeze(1).to_broadcast([16, 4, 32]))
    parT = AR.alloc("parT", [64], F32)
    pb = bank()
    P.op("pe", (lambda h, o=A(pb[:, 0:64]), i=A(stg2), idn=A(identf[0:64, 0:64]): h.transpose(out=o, in_=i, identity=idn)),
         [pb], [identf, stg2.k("a"), stg2.k("b"), stg2.k("c"), stg2.k("d")])
    P.copy("dve", parT, pb[:, 0:64])
    lamre, lamim, ldt, dexp = parT[:, 0:16], parT[:, 16:32], parT[:, 32:48], parT[:, 48:64]

    Bre = AR.alloc("Bre", [16, 16], F32)
    Bim = AR.alloc("Bim", [16, 16], F32)
    P.dma("sp", Bre, I["b_re"].rearrange("(q g) p h -> (g p) q h", g=2))
    P.dma("sp", Bim, I["b_im"].rearrange("(q g) p h -> (g p) q h", g=2))
    Craw = AR.alloc("Craw", [32, 64], F32, parts=16)
    CTre = AR.alloc("CTre", [16, 16], F32)
    CTim = AR.alloc("CTim", [16, 16], F32)
    for (dst, nm) in ((CTre, "c_re"), (CTim, "c_im")):
        P.dma("sp", Craw, I[nm].rearrange("g h p -> h g p"))
        pb = bank()
        for q in range(16):
            P.tr(pb[:, q * 16:(q + 1) * 16], Craw[:, 2 * q:2 * q + 2, :].re("h g p -> h (g p)"), identf[0:16, 0:16])
        P.copy("dve", dst.re("p a b -> p (a b)"), pb[:, 0:256])

    def sm(name, n=16):
        return AR.alloc(name, [n], F32)

    dt_ = sm("dt")
    P.act(dt_, ldt, AF.Exp)
    mag = sm("mag")
    lrd = sm("lrd")
    P.tt("dve", lrd, lamre, dt_, ALU.mult)
    P.act(mag, lrd, AF.Exp)
    ang = sm("ang")
    P.tt("dve", ang, lamim, dt_, ALU.mult)
    halfpi = sm("halfpi", 1)
    P.memset("pool", halfpi, math.pi / 2)
    s_ = sm("s_")
    c_ = sm("c_")
    P.act(s_, ang, AF.Sin, scale=1.0 / 8)
    P.act(c_, ang, AF.Sin, bias=halfpi[:, 0:1], scale=-1.0 / 8)
    t1 = sm("t1")
    t2 = sm("t2")
    for _ in range(3):
        P.tt("dve", t1, c_, c_, ALU.mult)
        P.tt("dve", t2, s_, s_, ALU.mult)
        P.tt("dve", s_, s_, c_, ALU.mult)
        P.ts("dve", s_, s_, 2.0, None, op0=ALU.mult)
        P.tt("dve", c_, t1, t2, ALU.subtract)
    PWre = AR.alloc("PWre", [5, 16], F32)
    PWim = AR.alloc("PWim", [5, 16], F32)
    P.tt("dve", PWre[:, 1, :], mag, c_, ALU.mult)
    P.tt("dve", PWim[:, 1, :], mag, s_, ALU.mult)

    cmT = [AR.alloc(f"cmT{i}", [16 * 32], F32) for i in range(4)]

    def cmul(eng, ore, oim, are, aim, bre, bim, shape_tmp):
        n = int(np.prod(shape_tmp))

        def view(b):
            v = b[:, 0:n]
            if len(shape_tmp) == 2:
                v = v.re("p (a b) -> p a b", a=shape_tmp[0])
            return v
        ta, tb, tc_, td = [view(b) for b in cmT]
        P.tt(eng, ta, are, bre, ALU.mult)
        P.tt(eng, tb, aim, bim, ALU.mult)
        P.tt(eng, tc_, are, bim, ALU.mult)
        P.tt(eng, td, aim, bre, ALU.mult)
        P.tt(eng, ore, ta, tb, ALU.subtract)
        P.tt(eng, oim, tc_, td, ALU.add)

    for kk in (2, 3, 4):
        cmul("dve", PWre[:, kk, :], PWim[:, kk, :], PWre[:, kk - 1, :], PWim[:, kk - 1, :],
             PWre[:, 1, :], PWim[:, 1, :], [16])
    nr = sm("nr")
    P.ts("dve", nr, PWre[:, 1, :], -1.0, None, op0=ALU.add)
    den = sm("den")
    P.tt("dve", t1, lamre, lamre, ALU.mult)
    P.tt("dve", t2, lamim, lamim, ALU.mult)
    P.tt("dve", den, t1, t2, ALU.add)
    rden = sm("rden")
    P.op("dve", (lambda h, o=A(rden), i=A(den): h.reciprocal(out=o, in_=i)), [rden], [den])
    cre = sm("cre")
    cim = sm("cim")
    t3 = sm("t3")
    t4 = sm("t4")
    P.tt("dve", t1, nr, lamre, ALU.mult)
    P.tt("dve", t2, PWim[:, 1, :], lamim, ALU.mult)
    P.tt("dve", t3, t1, t2, ALU.add)
    P.tt("dve", cre, t3, rden, ALU.mult)
    P.tt("dve", t1, PWim[:, 1, :], lamre, ALU.mult)
    P.tt("dve", t2, nr, lamim, ALU.mult)
    P.tt("dve", t4, t1, t2, ALU.subtract)
    P.tt("dve", cim, t4, rden, ALU.mult)
    bbre = AR.alloc("bbre", [16, 16], F32)
    bbim = AR.alloc("bbim", [16, 16], F32)
    cmul("dve", bbre, bbim, cre.un(2).bc([128, 16, 16]), cim.un(2).bc([128, 16, 16]), Bre, Bim, [16, 16])
    CT0re = AR.alloc("CT0re", [16, 2, 16], F32)
    CT0imn = AR.alloc("CT0imn", [16, 2, 16], F32)
    P.memset("pool", CT0re, 0.0)
    P.memset("pool", CT0imn, 0.0)
    P.copy("pool", CT0re[0:64, :, 0, :], CTre[0:64])
    P.copy("pool", CT0re[64:128, :, 1, :], CTre[64:128])
    P.ts("pool", CT0imn[0:64, :, 0, :], CTim[0:64], -1.0, None, op0=ALU.mult)
    P.ts("pool", CT0imn[64:128, :, 1, :], CTim[64:128], -1.0, None, op0=ALU.mult)
    Are = AR.alloc("Are", [8, 7, 2, 16], F32)
    Aim = AR.alloc("Aim", [8, 7, 2, 16], F32)
    xr = AR.alloc("xr", [16, 16], F32)
    xi = AR.alloc("xi", [16, 16], F32)
    for qh in range(2):
        qo = 8 * qh
        P.memset("pool", Are, 0.0)
        P.memset("pool", Aim, 0.0)
        for a in range(4):
            kk = 3 - a
            if kk == 0:
                sr_, si_ = bbre, bbim
            else:
                cmul("dve", xr, xi, PWre[:, kk, :].un(2).bc([128, 16, 16]), PWim[:, kk, :].un(2).bc([128, 16, 16]),
                     bbre, bbim, [16, 16])
                sr_, si_ = xr, xi
            for (dstA, src) in ((Are, sr_), (Aim, si_)):
                P.copy("pool", dstA[0:64, :, a, 0, :], src[0:64, qo:qo + 8, :])
                P.copy("pool", dstA[64:128, :, a, 1, :], src[64:128, qo:qo + 8, :])
        for (dst, srcA) in ((BexpRe, Are), (BexpIm, Aim)):
            for q4 in range(2):
                pb = bank()
                for lp in range(4):
                    ql = 4 * q4 + lp
                    P.tr(pb[:, lp * 128:(lp + 1) * 128], srcA[:, ql, 0:4, :, :].re("p a g h -> p (a g h)"), identf)
                P.copy("act", dst[:, qo + 4 * q4:qo + 4 * q4 + 4, :].re("p a b -> p (a b)"), pb[:, 0:512])
        for q4 in range(2):
            pb = bank()
            for lp in range(4):
                ql = 4 * q4 + lp
                q = qo + ql
                for tau in range(4):
                    a0 = 3 - tau
                    P.mm(pb[:, lp * 128 + tau * 32: lp * 128 + tau * 32 + 32],
                         [(Are[:, ql, a0:a0 + 4, :, :].re("p a g h -> p (a g h)"), CT0re[:, q, :, :].re("p g h -> p (g h)")),
                          (Aim[:, ql, a0:a0 + 4, :, :].re("p a g h -> p (a g h)"), CT0imn[:, q, :, :].re("p g h -> p (g h)"))])
            for lp in range(4):
                q = qo + 4 * q4 + lp
                P.stt(Kmat[:, q, :], identf, dexp[:, q:q + 1], pb[:, lp * 128:(lp + 1) * 128], ALU.mult, ALU.add)
    P.memset("pool", CexpA, 0.0)
    P.memset("pool", CexpB, 0.0)
    for tau in range(4):
        cmul("dve", xr, xi, PWre[:, tau + 1, :].un(2).bc([128, 16, 16]), PWim[:, tau + 1, :].un(2).bc([128, 16, 16]),
             CTre, CTim, [16, 16])
        P.copy("pool", CexpA[0:64, :, tau, 0, :], xr[0:64])
        P.copy("pool", CexpA[64:128, :, tau, 1, :], xr[64:128])
        P.ts("pool", CexpB[0:64, :, tau, 0, :], xi[0:64], -1.0, None, op0=ALU.mult)
        P.ts("pool", CexpB[64:128, :, tau, 1, :], xi[64:128], -1.0, None, op0=ALU.mult)
    rd = sm("rd")
    P.tt("dve", t1, PWre[:, 4, :], PWre[:, 4, :], ALU.mult)
    P.tt("dve", t2, PWim[:, 4, :], PWim[:, 4, :], ALU.mult)
    P.tt("dve", t3, t1, t2, ALU.add)
    P.act(dcol, t3, AF.Sqrt)
    P.op("dve", (lambda h, o=A(rd), i=A(dcol): h.reciprocal(out=o, in_=i)), [rd], [dcol])
    P.tt("dve", Tre[:, :, 0], PWre[:, 4, :], rd, ALU.mult)
    P.tt("dve", t1, PWim[:, 4, :], rd, ALU.mult)
    P.ts("dve", Tim[:, :, 0], t1, -1.0, None, op0=ALU.mult)
    m = 1
    while m < CH:
        ar = Tre[:, :, m - 1:m].bc([128, 16, m])
        ai = Tim[:, :, m - 1:m].bc([128, 16, m])
        cmul("dve", Tre[:, :, m:2 * m], Tim[:, :, m:2 * m], Tre[:, :, 0:m], Tim[:, :, 0:m], ar, ai, [16, m])
        m *= 2
    sraw = AR.alloc("sraw", [2048], F32, parts=16)
    for ri, nm in ((0, "sre"), (1, "sim")):
        P.dma("sp", sraw, I[nm])
        pb = bank()
        for q in range(16):
            P.tr(pb[:, q * 16:(q + 1) * 16], sraw[:, q * 128:(q + 1) * 128], identf[0:16, 0:16])
        P.copy("dve", h0sb[:, :, ri, :], pb[:, 0:256].re("p (q s) -> p q s", q=16))
    P.copy("dve", Hsh_s, h0sb)
    cmul("dve", rot0[:, :, 0, :], rot0[:, :, 1, :], PWre[:, 4, :].un(2).bc([128, 16, 16]), PWim[:, 4, :].un(2).bc([128, 16, 16]),
         h0sb[:, :, 0, :], h0sb[:, :, 1, :], [16, 16])
    dbg_out("Kmat", Kmat, [128, 16, 128], BF16)
    dbg_out("Tre", Tre, [128, 16, CH])
    dbg_out("PWre", PWre, [128, 5, 16])
    dbg_out("PWim", PWim, [128, 5, 16])
    AR.pop()
    P.barrier()
    if stop_after == "setup":
        return finish()

    xt = AR.alloc("xt", [D], F32)
    hT = AR.alloc("hT", [NT, NB], BF16)
    a_sb = AR.alloc("a_sb", [4, NB], F32)
    sgt = AR.alloc("sgt", [4, NB], F32)
    fullp = AR.alloc("fullp", [4, 30 + NB], F32)
    hist = AR.alloc("hist", [4, 30], F32)
    ptap = [AR.alloc(f"ptap{i}", [NB], BF16) for i in range(4)]
    zsd = AR.alloc("zsd", [4, 4, CH], BF16)
    v_sb = AR.alloc("v_sb", [4, NB], F32)
    sq_sb = Buf(a_sb.ap, a_sb.key)
    mean_sb = AR.alloc("mean_sb", [NB], F32)
    var_sb = AR.alloc("var_sb", [NB], F32)
    rstd_sb = AR.alloc("rstd_sb", [NB], F32)
    vact = AR.alloc("vact", [4, NB], BF16)
    cvo = AR.alloc("cvo", [NT, NB], BF16)
    sso = AR.alloc("sso", [NT, NB], BF16)
    sgg2 = [AR.alloc(f"sgg{i}", [NB], BF16) for i in range(2)]
    S5B = []
    for i in range(2):
        S5B.append(dict(
            U=AR.alloc(f"U{i}", [4, CH], BF16), hl=AR.alloc(f"hl{i}", [4, 2, CH], F32),
            gb=AR.alloc(f"gb{i}", [4, 2, CH], F32), gg=AR.alloc(f"gg{i}", [4, 2, CH], F32),
            hfull=AR.alloc(f"hfull{i}", [4, 2, CH], F32), rt=[AR.alloc(f"rt{i}_{j}", [4, CH], F32) for j in range(4)],
            Hsh=AR.alloc(f"Hsh{i}", [4, 2, CH], BF16), yg=AR.alloc(f"yg{i}", [4, CH], BF16)))
    ygT = AR.alloc("ygT", [4, NB], BF16)
    sgc = [AR.alloc(f"sgc{i}", [NB], BF16) for i in range(2)]
    sgs = [AR.alloc(f"sgs{i}", [NB], BF16) for i in range(2)]
    m1 = [AR.alloc(f"m1{i}", [NB], F32) for i in range(2)]
    m2 = [AR.alloc(f"m2{i}", [NB], F32) for i in range(2)]
    htmp = AR.alloc("htmp", [NT, DS], F32)
    P.memset("pool", hist, 0.0)

    def win_chunk(c):
        return I["w_in"][:, c * 512:(c + 1) * 512].rearrange("(kt p) c -> p kt c", p=128)

    def w512(name):
        return I[name].rearrange("(kt p) c -> p kt c", p=128)

    for blk in range(NBLK + 1):
        samp = blk == NBLK
        N = DS if samp else NB
        C = N // 4
        tok0 = L if samp else blk * NB
        full = None if samp else fullp
        if not samp:
            for tt_ in range(NB // 128):
                P.dma("sp", xt, I["xp"][tok0 + tt_ * 128: tok0 + (tt_ + 1) * 128, :])
                for half in range(2):
                    pb = bank()
                    for d4 in range(4):
                        dt = half * 4 + d4
                        P.tr(pb[:, d4 * 128:(d4 + 1) * 128], xt[:, dt * 128:(dt + 1) * 128], identf)
                    for d4 in range(4):
                        dt = half * 4 + d4
                        P.act(hT[:, dt, tt_ * 128:(tt_ + 1) * 128], pb[:, d4 * 128:(d4 + 1) * 128], AF.Identity,
                              bias=modT[:, M_SH1 + dt, 0:1], scale=modT[:, M_SC1 + dt, 0:1])
        else:
            P.dma("sp", xt[0:DS, :], I["xs"])
            pb = bank()
            for dt in range(NT):
                P.tr(pb[:, dt * DS:(dt + 1) * DS], xt[0:DS, dt * 128:(dt + 1) * 128], identf[0:DS, 0:DS])
            P.tt("dve", htmp.re("p d (s t) -> p d s t", t=4), pb[:, 0:NT * DS].re("p (d s t) -> p d s t", d=NT, t=4),
                 modT[:, M_SC1:M_SC1 + 8, 1:NSEQ].un(3).bc([128, NT, 16, 4]), ALU.mult)
            P.tt("dve", hT[:, :, 0:DS].re("p d (s t) -> p d s t", t=4), htmp.re("p d (s t) -> p d s t", t=4),
                 modT[:, M_SH1:M_SH1 + 8, 1:NSEQ].un(3).bc([128, NT, 16, 4]), ALU.add)
        if blk == 0:
            dbg_out("hT0", hT, [128, NT, NB], BF16)
        for c in range(3):
            slot = ring_load(win_chunk(c))
            for half in range(2):
                pb = bank()
                for c2 in range(2):
                    ct = half * 2 + c2
                    P.mm(pb[:, c2 * NB:c2 * NB + N], [(slot[:, k, ct * 128:(ct + 1) * 128], hT[:, k, 0:N]) for k in range(NT)])
                for c2 in range(2):
                    ct = half * 2 + c2
                    src = pb[:, c2 * NB:c2 * NB + N]
                    bcol = colsT[:, C_BIN + c * 4 + ct:C_BIN + c * 4 + ct + 1]
                    if c == 0:
                        P.act(a_sb[:, ct, 0:N], src, AF.Identity, bias=bcol)
                    elif c == 1:
                        P.act(sgt[:, ct, 0:N], src, AF.Sigmoid, bias=bcol)
                    else:
                        P.act(zsd[:, ct, :, 0:C], src.re("p (c s) -> p s c", s=4), AF.Identity, bias=bcol)
        st_conv = P.begin()
        cur_set[0] = "conv"
        if not samp:
            P.copy("act", full[:, :, 0:30], hist)
            P.tt("dve", full[:, :, 30:30 + NB], a_sb, sgt, ALU.mult)
            if blk == NBLK - 1:
                pb = bank()
                for ct in range(4):
                    P.tr(pb[0:30, ct * 128:(ct + 1) * 128], full[:, ct, NB:NB + 30], identf)
                nco = AR.alloc("nco", [512], F32, parts=30)
                P.copy("dve", nco, pb[0:30, :])
                P.dma("sp", O["ncp"], nco, is_out=True)
        else:
            P.tt("dve", full_s[:, :, :, 30:34], a_sb[:, :, 0:DS].re("p c (s t) -> p c s t", t=4),
                 sgt[:, :, 0:DS].re("p c (s t) -> p c s t", t=4), ALU.mult)
            pb = bank()
            for ct in range(4):
                ucont = AR.alloc("ucont", [16, 4], F32) if ct == 0 else ucont
                P.copy("dve", ucont, full_s[:, ct, :, 30:34])
                P.tr(pb[0:DS, ct * 128:(ct + 1) * 128], ucont.re("p s t -> p (s t)"), identf)
            ncs_sb = AR.alloc("ncs_sb", [512], F32, parts=DS)
            P.copy("dve", ncs_sb, pb[0:DS, :])
            for s in range(16):
                P.dma("sp", O["ncs"][s, 26:30, :], ncs_sb[4 * s:4 * s + 4, :], is_out=True)
        KSPLIT = 12
        pbcs = []
        for ct in range(4):
            pbc = bank()
            pbcs.append(pbc)
            for k in range(KSPLIT, 31):
                pk = ptap[k % 4]
                if samp:
                    P.act(pk[:, 0:DS].re("p (s t) -> p s t", t=4), full_s[:, ct, :, k:k + 4], AF.Identity, scale=wdwT[:, ct, k:k + 1])
                else:
                    P.act(pk, full[:, ct, k:k + NB], AF.Identity, scale=wdwT[:, ct, k:k + 1])
                o_, l_, r_ = A(pbc[:, 0:N]), A(identb), A(pk[:, 0:N])
                P.op("pe", (lambda h, o_=o_, l_=l_, r_=r_, first=(k == KSPLIT), last=(k == 30):
                            h.matmul(o_, lhsT=l_, rhs=r_, start=first, stop=last)), [pbc], [identb, pk])
        for k in range(KSPLIT):
            for ct in range(4):
                if samp:
                    src = full_s[:, ct, :, k:k + 4]
                    dst = v_sb[:, ct, 0:DS].re("p (s t) -> p s t", t=4)
                else:
                    src = full[:, ct, k:k + NB]
                    dst = v_sb[:, ct, :]
                wcol = wdwT[:, ct, k:k + 1]
                if k == 0:
                    P.ts("dve", dst, src, wcol, colsT[:, C_BDW + ct:C_BDW + ct + 1], op0=ALU.mult, op1=ALU.add)
                else:
                    P.stt(dst, src, wcol, dst, ALU.mult, ALU.add)
        for ct in range(4):
            P.tt("dve", v_sb[:, ct, 0:N], v_sb[:, ct, 0:N], pbcs[ct][:, 0:N], ALU.add)
        if not samp:
            P.copy("act", hist, full[:, :, NB:NB + 30])
        P.act(sq_sb[:, :, 0:N], v_sb[:, :, 0:N], AF.Square)
        pb = bank()
        P.mm(pb[:, 0:N], [(onesf, v_sb[:, ct, 0:N]) for ct in range(4)])
        P.mm(pb[:, NB:NB + N], [(onesf, sq_sb[:, ct, 0:N]) for ct in range(4)])
        P.ts("dve", mean_sb[:, 0:N], pb[:, 0:N], 1.0 / 512, None, op0=ALU.mult)
        P.tt("dve", var_sb[:, 0:N], mean_sb[:, 0:N], mean_sb[:, 0:N], ALU.mult)
        P.stt(var_sb[:, 0:N], pb[:, NB:NB + N], 1.0 / 512, var_sb[:, 0:N], ALU.mult, ALU.subtract)
        P.act(rstd_sb[:, 0:N], var_sb[:, 0:N], AF.Sqrt, bias=epsc[:, 0:1])
        P.op("dve", (lambda h, o=A(rstd_sb[:, 0:N]), i=A(rstd_sb[:, 0:N]): h.reciprocal(out=o, in_=i)), [rstd_sb], [rstd_sb])
        P.tt("dve", v_sb[:, :, 0:N], v_sb[:, :, 0:N], mean_sb[:, 0:N].un(1).bc([128, 4, N]), ALU.subtract)
        P.tt("dve", v_sb[:, :, 0:N], v_sb[:, :, 0:N], rstd_sb[:, 0:N].un(1).bc([128, 4, N]), ALU.mult)
        for ct in range(4):
            P.act(vact[:, ct, 0:N], v_sb[:, ct, 0:N], AF.Silu, bias=colsT[:, C_CB + ct:C_CB + ct + 1],
                  scale=colsT[:, C_CG + ct:C_CG + ct + 1])
        slot = ring_load(w512("w_pw"), view4=True)
        for j2 in range(4):
            pb = bank()
            for c2 in range(2):
                j = 2 * j2 + c2
                P.mm(pb[:, c2 * NB:c2 * NB + N], [(slot[:, k, j * 128:(j + 1) * 128], vact[:, k, 0:N]) for k in range(4)])
            for c2 in range(2):
                j = 2 * j2 + c2
                P.act(cvo[:, j, 0:N], pb[:, c2 * NB:c2 * NB + N], AF.Identity, bias=colsT[:, C_BPW + j:C_BPW + j + 1])
        P.end()
        st_s5 = []
        for ct in range(4):
            st_s5.append(P.begin())
            cur_set[0] = "s5a" if ct % 2 == 0 else "s5b"
            SB = S5B[ct % 2]
            U, hl, gb, gg, hfull, rt, Hsh, yg = (SB["U"], SB["hl"], SB["gb"], SB["gg"], SB["hfull"], SB["rt"], SB["Hsh"], SB["yg"])
            cry = carry.k(ct)
            pb = bank()
            for lp in range(4):
                P.mm(pb[:, lp * CH:lp * CH + C], [(selu[:, lp * 4 + s, :], zsd[:, ct, s, 0:C]) for s in range(4)])
            P.copy("act", U[:, :, 0:C], pb[:, 0:4 * CH].re("p (a c) -> p a c", a=4)[:, :, 0:C])
            pb = bank()
            for lp in range(4):
                q = 4 * ct + lp
                P.mm(pb[:, (lp * 2) * CH:(lp * 2) * CH + C], [(BexpRe[:, q, :], U[:, lp, 0:C])])
                P.mm(pb[:, (lp * 2 + 1) * CH:(lp * 2 + 1) * CH + C], [(BexpIm[:, q, :], U[:, lp, 0:C])])
            P.copy("act", hl[:, :, :, 0:C], pb[:, 0:8 * CH].re("p (a r c) -> p a r c", a=4, r=2)[:, :, :, 0:C])
            qs = slice(4 * ct, 4 * ct + 4)
            if not samp:
                hre, him = hl[:, :, 0, :], hl[:, :, 1, :]
                tr_, ti_ = Tre[:, qs, :], Tim[:, qs, :]
                P.tt("dve", rt[0], hre, tr_, ALU.mult)
                P.tt("dve", rt[1], him, ti_, ALU.mult)
                P.tt("dve", rt[2], him, tr_, ALU.mult)
                P.tt("dve", rt[3], hre, ti_, ALU.mult)
                P.tt("dve", gb[:, :, 0, :], rt[0], rt[1], ALU.subtract)
                P.tt("dve", gb[:, :, 1, :], rt[2], rt[3], ALU.add)
                P.copy("act", Hsh[:, :, :, 0], cry[:, qs, :])
                for lp in range(4):
                    q = 4 * ct + lp
                    for ri in range(2):
                        P.scan(gg[:, lp, ri, :], dcol[:, q:q + 1].bc([128, CH]), gb[:, lp, ri, :], cry[:, q, ri:ri + 1])
                gre, gim = gg[:, :, 0, :], gg[:, :, 1, :]
                P.tt("dve", rt[0], gre, tr_, ALU.mult)
                P.tt("dve", rt[1], gim, ti_, ALU.mult)
                P.tt("dve", rt[2], gim, tr_, ALU.mult)
                P.tt("dve", rt[3], gre, ti_, ALU.mult)
                P.tt("dve", hfull[:, :, 0, :], rt[0], rt[1], ALU.add)
                P.tt("dve", hfull[:, :, 1, :], rt[2], rt[3], ALU.subtract)
                P.copy("act", Hsh[:, :, :, 1:CH], hfull[:, :, :, 0:CH - 1])
                P.copy("dve", cry[:, qs, :], hfull[:, :, :, CH - 1])
                hsrc = lambda lp, ri: Hsh[:, lp, ri, 0:C]
                if blk == 0 and ct in (0, 2):
                    dbg_out(f"hl{ct}", hl, [128, 4, 2, CH])
                    dbg_out(f"gb{ct}", gb, [128, 4, 2, CH])
                    dbg_out(f"gg{ct}", gg, [128, 4, 2, CH])
                    dbg_out(f"hfull{ct}", hfull, [128, 4, 2, CH])
                    dbg_out(f"dcol{ct}", dcol, [128, 16])
                    dbg_out(f"Tre{ct}", Tre, [128, 16, CH])
                    dbg_out(f"Tim{ct}", Tim, [128, 16, CH])
            else:
                P.tt("dve", hend_s[:, qs, :, :], rot0[:, qs, :, :], hl[:, :, :, 0:C], ALU.add)
                hsrc = lambda lp, ri: Hsh_s[:, 4 * ct + lp, ri, :]
            pb = bank()
            for lp in range(4):
                q = 4 * ct + lp
                P.mm(pb[:, lp * CH:lp * CH + C],
                     [(Kmat[:, q, :], U[:, lp, 0:C]),
                      (CexpA[:, q].re("p t g h -> p (t g h)"), hsrc(lp, 0)),
                      (CexpB[:, q].re("p t g h -> p (t g h)"), hsrc(lp, 1))])
            P.act(yg[:, :, 0:C], pb[:, 0:4 * CH].re("p (a c) -> p a c", a=4)[:, :, 0:C], AF.Gelu_apprx_tanh)
            pb = bank()
            for tau in range(4):
                P.mm(pb[:, tau * CH:tau * CH + C], [(sely[:, lp * 4 + tau, :], yg[:, lp, 0:C]) for lp in range(4)])
            P.copy("dve", ygT[:, ct, 0:N].re("p (c t) -> p t c", t=4).k(ct),
                   pb[:, 0:4 * CH].re("p (t c) -> p t c", t=4)[:, :, 0:C])
            P.end()
        def _merge(a, b_):
            out, ia, ib = [], 0, 0
            while ia < len(a) or ib < len(b_):
                if ib >= len(b_) or (ia < len(a) and ia * len(b_) <= ib * len(a)):
                    out.append(a[ia]); ia += 1
                else:
                    out.append(b_[ib]); ib += 1
            return out
        cur_set[0] = "all"
        slot_sv = ring_load(w512("w_sv"), view4=True)
        slot_sg = ring_load(w512("w_sg"), view4=True)
        P.replay([_merge(st_s5[0], st_s5[1]) + _merge(st_s5[2], st_s5[3]), st_conv])
        if blk == 0:
            dbg_out("ygT0", ygT, [128, 4, NB], BF16)
            dbg_out("cvo0", cvo, [128, NT, NB], BF16)
        for j in range(NT):
            pb = bank()
            ygk_ = [ygT.k(c4) for c4 in range(4)]
            P.mm(pb[:, 0:N], [(slot_sv[:, k, j * 128:(j + 1) * 128], ygT[:, k, 0:N]) for k in range(4)], extra_ins=ygk_)
            P.mm(pb[:, NB:NB + N], [(slot_sg[:, k, j * 128:(j + 1) * 128], ygT[:, k, 0:N]) for k in range(4)], extra_ins=ygk_)
            P.act(sgg2[j % 2][:, 0:N], pb[:, NB:NB + N], AF.Sigmoid, bias=colsT[:, C_BSG + j:C_BSG + j + 1])
            P.stt(sso[:, j, 0:N], pb[:, 0:N], colsT[:, C_BSV + j:C_BSV + j + 1], sgg2[j % 2][:, 0:N], ALU.add, ALU.mult)
        for half in range(2):
            slot_c = ring_load(win_chunk(3 + half))
            slot_s = ring_load(win_chunk(5 + half))
            for j4 in range(4):
                j = half * 4 + j4
                pb = bank()
                P.mm(pb[:, 0:N], [(slot_c[:, k, j4 * 128:(j4 + 1) * 128], hT[:, k, 0:N]) for k in range(NT)])
                P.mm(pb[:, NB:NB + N], [(slot_s[:, k, j4 * 128:(j4 + 1) * 128], hT[:, k, 0:N]) for k in range(NT)])
                b = j % 2
                P.act(sgc[b][:, 0:N], pb[:, 0:N], AF.Sigmoid, bias=colsT[:, C_BIN + 12 + j:C_BIN + 12 + j + 1])
                P.act(sgs[b][:, 0:N], pb[:, NB:NB + N], AF.Sigmoid, bias=colsT[:, C_BIN + 20 + j:C_BIN + 20 + j + 1])
                P.tt("dve", m1[b][:, 0:N], cvo[:, j, 0:N], sgc[b][:, 0:N], ALU.mult)
                P.tt("dve", m2[b][:, 0:N], sso[:, j, 0:N], sgs[b][:, 0:N], ALU.mult)
                P.tt("dve", merged[:, j, tok0:tok0 + N], m1[b][:, 0:N], m2[b][:, 0:N], ALU.add)
    pb = bank()
    for ri in range(2):
        P.op("pe", (lambda h, o=A(pb[0:16, ri * 128:(ri + 1) * 128]), i=A(carry[:, :, ri]), idn=A(identf): h.transpose(out=o, in_=i, identity=idn)),
             [pb], [identf, carry] + [carry.k(c4) for c4 in range(4)])
    fst = AR.alloc("fst", [256], F32, parts=16)
    P.copy("dve", fst, pb[0:16, 0:256])
    P.dma("sp", O["nrp"], fst[:, 0:128], is_out=True)
    P.dma("sp", O["nip"], fst[:, 128:256], is_out=True)
    for ri in range(2):
        for q4 in range(4):
            pb = bank()
            for lp in range(4):
                q = 4 * q4 + lp
                P.tr(pb[0:16, lp * 128:(lp + 1) * 128], hend_s[:, q, ri, :], identf)
            P.copy("dve", ring[ri].re("p a b -> p (a b)").cast(F32)[0:16, q4 * 512:(q4 + 1) * 512], pb[0:16, :])
    P.dma("sp", O["nrs"], ring[0].re("p a b -> p (a b)").cast(F32)[0:16, :], is_out=True)
    P.dma("sp", O["nis"], ring[1].re("p a b -> p (a b)").cast(F32)[0:16, :], is_out=True)
    dbg_out("merged", merged, [128, NT, NTOK], BF16)
    AR.pop()
    P.barrier()
    if stop_after == "A":
        return finish()

    I32 = mybir.dt.int32
    U32 = mybir.dt.uint32
    NSUB = 4
    G = 128 * NSUB
    NSLOT_T = (NTOK * 4) // G + 32
    NTILE = 17
    tiles = [(t * 128, 128) for t in range(16)] + [(L, DS)]
    h2d = Buf(nc.dram_tensor("h2d", [NTOK, D], BF16).ap(), "h2d")
    accd = Buf(nc.dram_tensor("accd", [NTOK, D], F32).ap(), "accd")
    Yd = Buf(nc.dram_tensor("Yd", [NSLOT_T * G, D], F32).ap(), "Yd")
    tokslot = Buf(nc.dram_tensor("tokslot", [NSLOT_T * G, 1], I32).ap(), "tokslot")

    def idma(out, in_, idx, gather, extra_reads=()):
        rec = P.dma("pool", out, in_, extra_reads=[idx] + list(extra_reads))
        o, i, ix = A(out), A(in_), A(idx)
        if gather:
            rec["fn"] = (lambda h: h.indirect_dma_start(out=o, out_offset=None, in_=i,
                                                        in_offset=bass.IndirectOffsetOnAxis(ap=ix, axis=0)))
        else:
            rec["fn"] = (lambda h: h.indirect_dma_start(out=o, out_offset=bass.IndirectOffsetOnAxis(ap=ix, axis=0),
                                                        in_=i, in_offset=None))
        return rec

    g2bc_p = AR.alloc("g2bc_p", [D], F32)
    g2bc_s = AR.alloc("g2bc_s", [D], F32)
    gates = AR.alloc("gates", [NTILE, 4], F32)
    slots_i = AR.alloc("slots_i", [NTILE, 4], I32)
    widx = AR.alloc("widx", [NSLOT_T, NT], I32)
    widx_g = AR.alloc("widx_g", [NSLOT_T, NT], I32)
    widx_u = AR.alloc("widx_u", [NSLOT_T, NT], I32)
    bidx = AR.alloc("bidx", [NSLOT_T], I32)
    iota32 = AR.alloc("iota32", [N_EXP], F32)
    tokid = AR.alloc("tokid", [NTILE], I32)
    P.dma("sp", iota32, I["k_iota32"])
    tokid_f = AR.alloc("tokid_f", [NTILE], F32)
    P.dma("sp", tokid_f, I["k_tokid"])
    P.copy("dve", tokid, tokid_f)

    def make_gbc(dsts, src3, m0):
        AR.push()
        modtm = AR.alloc("modtm", [D], F32, parts=NSEQ)
        selp = AR.alloc("selp", [128], F32, parts=NSEQ)
        sels = AR.alloc("sels", [DS], F32, parts=NSEQ)
        P.dma("sp", selp, I["k_selp"])
        P.dma("sp", sels, I["k_sels"])
        for h4 in range(2):
            pb = bank()
            for d4 in range(4):
                dt = h4 * 4 + d4
                P.tr(pb[0:NSEQ, d4 * 128:(d4 + 1) * 128], src3[:, m0 + dt, :], identf)
            P.copy("dve", modtm[:, h4 * 512:(h4 + 1) * 512], pb[0:NSEQ, :])
        for (dst, sel, R) in ((dsts[0], selp, 128), (dsts[1], sels, DS)):
            for hh in range(2):
                pb = bank()
                P.mm(pb[0:R, :], [(sel[:, 0:R], modtm[:, hh * 512:(hh + 1) * 512])])
                P.copy("act", dst[0:R, hh * 512:(hh + 1) * 512], pb[0:R, :])
        AR.pop()
        P.barrier()

    make_gbc((g2bc_p, g2bc_s), modT, M_G2)

    AR.push()
    oh4 = AR.alloc("oh4", [NTILE, 4, N_EXP], F32)
    pos_all = AR.alloc("pos_all", [NTILE, N_EXP], F32)
    g1bc_p = AR.alloc("g1bc_p", [D], F32)
    g1bc_s = AR.alloc("g1bc_s", [D], F32)
    Abc_p = AR.alloc("Abc_p", [D], F32)
    Abc_s = AR.alloc("Abc_s", [D], F32)
    Bvbc_p = AR.alloc("Bvbc_p", [D], F32)
    Bvbc_s = AR.alloc("Bvbc_s", [D], F32)
    make_gbc((g1bc_p, g1bc_s), modT, M_G1)
    make_gbc((Abc_p, Abc_s), A_T, 0)
    make_gbc((Bvbc_p, Bvbc_s), Bv_T, 0)
    l1g_bc = AR.alloc("l1g_bc", [D], F32)
    l1b_bc = AR.alloc("l1b_bc", [D], F32)
    for dst, nm in ((l1g_bc, "ln1_g"), (l1b_bc, "ln1_b")):
        P.dma("sp", dst, I[nm].partition_broadcast(128))
        P.ts("pool", dst, dst, float(DN_ALPHA), None, op0=ALU.mult)
    wr_bf = AR.alloc("wr_bf", [NT, N_EXP], BF16)
    br_row = AR.alloc("br_row", [N_EXP], F32, parts=1)
    b2_bf = AR.alloc("b2_bf", [D], BF16, parts=N_EXP)
    bout_row = AR.alloc("bout_row", [D], F32, parts=1)
    Ls = AR.alloc("Ls", [128], F32)
    base_row = AR.alloc("base_row", [N_EXP], F32, parts=1)
    P.dma("pool", wr_bf, I["w_router"].rearrange("(kt p) e -> p kt e", p=128))
    P.dma("sp", br_row, I["b_router"].rearrange("(o n) -> o n", o=1))
    P.dma("pool", b2_bf, I["b2"])
    P.dma("sp", bout_row, I["b_out"].rearrange("(o n) -> o n", o=1))
    P.dma("sp", Ls, I["k_ls"])
    P.memset("pool", base_row, 0.0)
    wout = AR.alloc("wout", [NT, D], BF16)
    P.dma("pool", wout, I["w_out"].rearrange("(kt p) c -> p kt c", p=128))
    xtb = [AR.alloc(f"xtb{i}", [D], F32) for i in range(2)]
    accb = [AR.alloc(f"accb{i}", [D], F32) for i in range(2)]
    h2b = [AR.alloc(f"h2b{i}", [D], BF16) for i in range(2)]
    BSETS = []
    for i in range(2):
        BSETS.append(dict(
            tsb=AR.alloc(f"tsb{i}", [D], F32), pre=AR.alloc(f"pre{i}", [D], F32), xn=AR.alloc(f"xn{i}", [D], F32),
            u1=None, h2f=None, h2Tt=AR.alloc(f"h2Tt{i}", [NT, 128], BF16),
            bst=AR.alloc(f"bst{i}", [2, 6], F32), mv=AR.alloc(f"mv{i}", [2], F32), rstd=AR.alloc(f"rstd{i}", [1], F32),
            nmr=AR.alloc(f"nmr{i}", [1], F32), lg=AR.alloc(f"lg{i}", [N_EXP], F32), mx8=AR.alloc(f"mx8{i}", [8], F32),
            ix8=AR.alloc(f"ix8{i}", [8], U32), ixf=AR.alloc(f"ixf{i}", [4], F32), nmx=AR.alloc(f"nmx{i}", [1], F32),
            ex4=AR.alloc(f"ex4{i}", [4], F32), ssum=AR.alloc(f"ssum{i}", [1], F32), cmb3=AR.alloc(f"cmb3{i}", [4, N_EXP], F32),
            comb=AR.alloc(f"comb{i}", [N_EXP], F32), Mk=AR.alloc(f"Mk{i}", [N_EXP], F32),
            combT=AR.alloc(f"combT{i}", [128], BF16, parts=N_EXP)))
    esel = AR.alloc("esel", [NTILE, NTILE], F32)
    selt = AR.alloc("selt", [NTILE, 128], F32, parts=NTILE)
    P.dma("sp", esel.re("p a b -> p (a b)"), I["k_esel"])
    P.dma("sp", selt.re("p a b -> p (a b)"), I["k_selt"])
    cnt_ps = PS[7]
    bank_sets["b0"] = [0, 1, 2]
    bank_sets["b1"] = [3, 4, 5, 6]
    bank_ctr["b0"] = 0
    bank_ctr["b1"] = 0
    b_streams = []
    for ti, (g0, R) in enumerate(tiles):
        BS = BSETS[ti % 2]
        (tsb, pre, xn, u1, h2f, h2Tt, bst, mv, rstd, nmr, lg, mx8, ix8, ixf, nmx, ex4, ssum, cmb3, comb, Mk, combT) = (
            BS["tsb"], BS["pre"], BS["xn"], BS["u1"], BS["h2f"], BS["h2Tt"], BS["bst"], BS["mv"], BS["rstd"], BS["nmr"],
            BS["lg"], BS["mx8"], BS["ix8"], BS["ixf"], BS["nmx"], BS["ex4"], BS["ssum"], BS["cmb3"], BS["comb"], BS["Mk"],
            BS["combT"])
        b_streams.append(P.begin())
        cur_set[0] = "b0" if ti % 2 == 0 else "b1"
        samp = g0 >= L
        xsrc = I["xs"] if samp else I["xp"][g0:g0 + R, :]
        xb = xtb[ti % 2]
        P.dma("sp", xb[0:R, :], xsrc)
        g1bc = g1bc_s if samp else g1bc_p
        g2bc = g2bc_s if samp else g2bc_p
        Abc = Abc_s if samp else Abc_p
        Bvbc = Bvbc_s if samp else Bvbc_p
        for hh in range(2):
            pb = bank()
            o = A(pb[0:R, :])
            prs = [(A(merged[:, k, g0:g0 + R]), A(wout[:, k, hh * 512:(hh + 1) * 512])) for k in range(NT)]
            l1, r1 = A(onesf[0:1, 0:R]), A(bout_row[0:1, hh * 512:(hh + 1) * 512])

            def fn(h, o=o, prs=prs, l1=l1, r1=r1):
                for i, (l, r) in enumerate(prs):
                    h.matmul(o, lhsT=l, rhs=r, start=(i == 0), stop=False)
                return h.matmul(o, lhsT=l1, rhs=r1, start=False, stop=True)
            P.op("pe", fn, [pb], [merged, wout, onesf, bout_row])
            P.tt("dve", tsb[0:R, hh * 512:(hh + 1) * 512], pb[0:R, :], g1bc[0:R, hh * 512:(hh + 1) * 512], ALU.mult)
        P.stt(pre[0:R, :], xb[0:R, :], float(DN_ALPHA), tsb[0:R, :], ALU.mult, ALU.add)
        for hh in range(2):
            P.op("dve", (lambda h, o=A(bst[0:R, hh, :]), i=A(pre[0:R, hh * 512:(hh + 1) * 512]): h.bn_stats(out=o, in_=i)),
                 [bst.k(hh)], [pre])
        P.op("dve", (lambda h, o=A(mv[0:R, :]), i=A(bst[0:R].re("p a b -> p (a b)")): h.bn_aggr(out=o, in_=i)),
             [mv], [bst.k(0), bst.k(1)])
        P.act(rstd[0:R, :], mv[0:R, 1:2], AF.Sqrt, bias=epsc[0:R, 0:1])
        P.op("dve", (lambda h, o=A(rstd[0:R, :]), i=A(rstd[0:R, :]): h.reciprocal(out=o, in_=i)), [rstd], [rstd])
        P.stt(nmr[0:R, :], mv[0:R, 0:1], -1.0, rstd[0:R, :], ALU.mult, ALU.mult)
        P.act(xn[0:R, :], pre[0:R, :], AF.Identity, bias=nmr[0:R, 0:1], scale=rstd[0:R, 0:1])
        ab = accb[ti % 2]
        P.tt("dve", ab[0:R, :], xn[0:R, :], l1g_bc[0:R, :], ALU.mult)
        P.tt("dve", ab[0:R, :], ab[0:R, :], l1b_bc[0:R, :], ALU.add)
        hb_ = h2b[ti % 2]
        P.tt("dve", pre[0:R, :], xn[0:R, :], Abc[0:R, :], ALU.mult)
        P.tt("dve", hb_[0:R, :], pre[0:R, :], Bvbc[0:R, :], ALU.add)
        P.dma("sp", h2d[g0:g0 + R, :].k(ti), hb_[0:R, :])
        pb = bank()
        pbb = pb.cast(BF16)
        for dt in range(NT):
            P.tr(pbb[:, dt * 128:dt * 128 + R], hb_[0:R, dt * 128:(dt + 1) * 128], identb[0:R, 0:R])
        P.copy("act", h2Tt[:, :, 0:R], pbb[:, 0:NT * 128].re("p (d t) -> p d t", d=NT)[:, :, 0:R])
        pb = bank()
        o = A(pb[0:R, 0:N_EXP])
        prs = [(A(h2Tt[:, k, 0:R]), A(wr_bf[:, k, :])) for k in range(NT)]
        l1, r1 = A(onesf[0:1, 0:R]), A(br_row[0:1, :])

        def fn(h, o=o, prs=prs, l1=l1, r1=r1):
            for i, (l, r) in enumerate(prs):
                h.matmul(o, lhsT=l, rhs=r, start=(i == 0), stop=False)
            return h.matmul(o, lhsT=l1, rhs=r1, start=False, stop=True)
        P.op("pe", fn, [pb], [h2Tt, wr_bf, onesf, br_row])
        P.copy("dve", lg[0:R, :], pb[0:R, 0:N_EXP])
        P.op("dve", (lambda h, o=A(mx8[0:R, :]), i=A(lg[0:R, :]): h.max(out=o, in_=i)), [mx8], [lg])
        P.op("dve", (lambda h, o=A(ix8[0:R, :]), m=A(mx8[0:R, :]), i=A(lg[0:R, :]): h.max_index(out=o, in_max=m, in_values=i)),
             [ix8], [mx8, lg])
        P.copy("dve", ixf[0:R, :], ix8[0:R, 0:4])
        P.ts("dve", nmx[0:R, :], mx8[0:R, 0:1], -1.0, None, op0=ALU.mult)
        P.act(ex4[0:R, :], mx8[0:R, 0:4], AF.Exp, bias=nmx[0:R, 0:1])
        P.op("dve", (lambda h, o=A(ssum[0:R, :]), i=A(ex4[0:R, :]): h.reduce_sum(out=o, in_=i, axis=mybir.AxisListType.X)),
             [ssum], [ex4])
        P.op("dve", (lambda h, o=A(ssum[0:R, :]), i=A(ssum[0:R, :]): h.reciprocal(out=o, in_=i)), [ssum], [ssum])
        P.ts("dve", gates[0:R, ti, :].k(ti), ex4[0:R, :], ssum[0:R, 0:1], None, op0=ALU.mult)
        P.tt("dve", oh4[0:R, ti, :, :].k(ti), iota32[0:R, :].un(1).bc([R, 4, N_EXP]),
             ixf[0:R, :].un(2).bc([R, 4, N_EXP]), ALU.is_equal)
        P.tt("dve", cmb3[0:R], oh4[0:R, ti, :, :].k(ti), gates[0:R, ti, :].k(ti).un(2).bc([R, 4, N_EXP]), ALU.mult)
        P.op("dve", (lambda h, o=A(comb[0:R, :]), i=A(cmb3[0:R].re("p k e -> p e k")): h.reduce_sum(out=o, in_=i, axis=mybir.AxisListType.X)),
             [comb], [cmb3])
        P.op("dve", (lambda h, o=A(Mk[0:R, :]), i=A(oh4[0:R, ti, :, :].re("p k e -> p e k")): h.reduce_sum(out=o, in_=i, axis=mybir.AxisListType.X)),
             [Mk], [oh4.k(ti)])
        pb = bank()
        P.mm(pb[0:R, 0:N_EXP], [(Ls[0:R, 0:R], Mk[0:R, :])])
        P.copy("act", pos_all[0:R, ti, :].k(ti), pb[0:R, 0:N_EXP])
        o_, l_, r_ = A(cnt_ps[0:NTILE, 0:N_EXP]), A(esel[0:R, ti, :]), A(Mk[0:R, :])
        P.op("pe", (lambda h, o_=o_, l_=l_, r_=r_, first=(ti == 0), last=(ti == NTILE - 1):
                    h.matmul(o_, lhsT=l_, rhs=r_, start=first, stop=last)), [cnt_ps], [esel, Mk])
        pb = bank()
        P.tr(pb[0:N_EXP, 0:R], comb[0:R, :], identf[0:R, 0:R])
        P.copy("act", combT[:, 0:R], pb[0:N_EXP, 0:R])
        for hh in range(2):
            pb = bank()
            P.mm(pb[0:R, :], [(combT[:, 0:R], b2_bf[:, hh * 512:(hh + 1) * 512])])
            P.tt("dve", tsb[0:R, hh * 512:(hh + 1) * 512], pb[0:R, :], g2bc[0:R, hh * 512:(hh + 1) * 512], ALU.mult)
        P.tt("dve", ab[0:R, :], ab[0:R, :], tsb[0:R, :], ALU.add)
        P.dma("sp", accd[g0:g0 + R, :].k(ti), ab[0:R, :])
        P.end()
        cur_set[0] = "all"
        if ti % 2 == 1 or ti == NTILE - 1:
            P.replay(b_streams)
            b_streams = []
    bank_sets["all"] = [0, 1, 2, 3, 4, 5, 6]
    cnt_all = AR.alloc("cnt_all", [N_EXP], F32, parts=NTILE)
    P.copy("dve", cnt_all, cnt_ps[0:NTILE, 0:N_EXP])
    pb = bank()
    P.mm(pb[0:NTILE, 0:N_EXP], [(Ls[0:NTILE, 0:NTILE], cnt_all)])
    P.mm(pb[0:1, 64:64 + N_EXP], [(onesf[0:NTILE, 0:1], cnt_all)])
    base_all = AR.alloc("base_all", [N_EXP], F32, parts=NTILE)
    P.copy("dve", base_all, pb[0:NTILE, 0:N_EXP])
    P.copy("dve", base_row, pb[0:1, 64:64 + N_EXP])
    for ti, (g0, R) in enumerate(tiles):
        pb = bank()
        P.mm(pb[0:R, 0:N_EXP], [(selt[:, ti, 0:R], base_all)])
        P.tt("dve", pos_all[0:R, ti, :].k(ti), pos_all[0:R, ti, :].k(ti), pb[0:R, 0:N_EXP], ALU.add)
    bank_sets["all"] = list(range(8))
    NE = N_EXP
    qrow = AR.alloc("qrow", [NE], F32, parts=1)
    tmpr = AR.alloc("tmpr", [NE], F32, parts=1)
    incl = AR.alloc("incl", [NE], F32, parts=1)
    strt = AR.alloc("strt", [NE], F32, parts=1)
    onesr = AR.alloc("onesr", [NE], F32, parts=1)
    P.memset("pool", onesr, 1.0)
    P.ts("dve", qrow, base_row, 0.0, None, op0=ALU.is_gt)
    for j in range(1, NTOK // G + 1):
        P.ts("dve", tmpr, base_row, float(G * j), None, op0=ALU.is_gt)
        P.tt("dve", qrow, qrow, tmpr, ALU.add)
    P.ts("dve", qrow, qrow, float(G), None, op0=ALU.mult)
    P.scan(incl, onesr, qrow, onesr[0:1, 0:1].k("z") if False else 0.0)
    P.tt("dve", strt, incl, qrow, ALU.subtract)
    svals = AR.alloc("svals", [NSLOT_T], F32, parts=1)
    P.dma("sp", svals, I["k_svals"])
    cmpb = AR.alloc("cmpb", [NSLOT_T, NE], F32, parts=1)
    P.tt("dve", cmpb, incl[0:1, :].un(1).bc([1, NSLOT_T, NE]), svals[0:1, :].un(2).bc([1, NSLOT_T, NE]), ALU.is_le)
    etile = AR.alloc("etile", [NSLOT_T], F32, parts=1)
    P.op("dve", (lambda h, o=A(etile), i=A(cmpb): h.reduce_sum(out=o, in_=i, axis=mybir.AxisListType.X)), [etile], [cmpb])
    P.ts("dve", etile, etile, float(NE - 1), None, op0=ALU.min)
    pb = bank()
    P.mm(pb[:, 0:NE], [(onesf[0:1, :], strt[0:1, :])])
    P.mm(pb[:, 64:64 + NSLOT_T], [(onesf[0:1, :], etile[0:1, :])])
    start_bc = AR.alloc("start_bc", [NE], F32)
    etile_bc = AR.alloc("etile_bc", [NSLOT_T], F32)
    P.copy("dve", start_bc, pb[:, 0:NE])
    P.copy("dve", etile_bc, pb[:, 64:64 + NSLOT_T])
    iotaK = AR.alloc("iotaK", [NT], F32)
    P.dma("sp", iotaK, I["k_iotak"])
    wf = AR.alloc("wf", [NSLOT_T, NT], F32)
    e1k = AR.alloc("e1k", [NSLOT_T], F32)
    P.ts("dve", e1k, etile_bc, 1024.0, None, op0=ALU.mult)
    P.tt("dve", wf, e1k.un(2).bc([128, NSLOT_T, NT]), iotaK.un(1).bc([128, NSLOT_T, NT]), ALU.add)
    P.copy("dve", widx, wf)
    wf2 = AR.alloc("wf2", [NSLOT_T, NT], F32)
    P.ts("dve", wf2, wf, 2.0, None, op0=ALU.mult)
    P.copy("dve", widx_g, wf2)
    P.ts("dve", wf2, wf2, 1.0, None, op0=ALU.add)
    P.copy("dve", widx_u, wf2)
    bf_ = AR.alloc("bf_", [NSLOT_T], F32)
    P.ts("dve", bf_, etile_bc, 128.0, iotaK[:, 0:1], op0=ALU.mult, op1=ALU.add)
    P.copy("dve", bidx, bf_)
    sfull = AR.alloc("sfull", [NTILE, NE], F32)
    slf = AR.alloc("slf", [NTILE, 4], F32)
    zero_i = AR.alloc("zero_i", [NSLOT_T * NSUB], I32)
    P.memset("pool", zero_i, 0)
    P.dma("sp", tokslot.re("(p j) o -> p (j o)", p=128).k("init"), zero_i)
    allk = [pos_all.k(ti) for ti in range(NTILE)] + [oh4.k(ti) for ti in range(NTILE)]
    P.memset("pool", pos_all[64:128, NTILE - 1, :].k(NTILE - 1), 0.0)
    P.memset("pool", oh4[64:128, NTILE - 1, :, :].k(NTILE - 1), 0.0)
    P.op("dve", (lambda h, o=A(sfull), a=A(pos_all), c=A(start_bc.un(1).bc([128, NTILE, NE])):
                 h.tensor_tensor(out=o, in0=a, in1=c, op=ALU.add)), [sfull], [start_bc] + allk)
    ohk = [oh4.k(ti) for ti in range(NTILE)]
    for k in range(4):
        P.op("dve", (lambda h, o=A(oh4[:, :, k, :]), a=A(oh4[:, :, k, :]), c=A(sfull):
                     h.tensor_tensor(out=o, in0=a, in1=c, op=ALU.mult)), [oh4] + ohk, [sfull] + allk)
    P.op("dve", (lambda h, o=A(slf.re("p t k -> p (t k)")), i=A(oh4.re("p t k e -> p (t k) e")):
                 h.reduce_sum(out=o, in_=i, axis=mybir.AxisListType.X)), [slf], [oh4] + ohk)
    P.op("dve", (lambda h, o=A(slots_i), i=A(slf): h.tensor_copy(out=o, in_=i)),
         [slots_i] + [slots_i.k(ti) for ti in range(NTILE)], [slf])
    sc_keys = []
    for ti, (g0, R) in enumerate(tiles):
        for k in range(4):
            kk = tokslot.k(f"s{ti}_{k}")
            idma(kk, tokid[0:R, ti:ti + 1], slots_i[0:R, ti, k:k + 1].k(ti), gather=False, extra_reads=[tokslot.k("init")])
            sc_keys.append(kk)
    dbg_out("slots", slots_i, [128, NTILE, 4], I32)
    dbg_out("gates", gates, [128, NTILE, 4])
    dbg_out("etile", etile, [1, NSLOT_T])
    AR.pop()
    P.barrier()
    if stop_after == "B":
        return finish()

    AR.limit = AR.n
    AR.push()
    NWR = 6
    wring = [AR.alloc(f"wring{i}", [NT, D], BF16) for i in range(NWR)]
    wix = [0]
    w1rows = I["w1"].rearrange("e k (h f) -> (e k h) f", h=2)
    w2rows = I["w2"].rearrange("e k f -> (e k) f")

    def wgather(rows_ap, s, wi):
        slot = wring[wix[0] % NWR]
        wix[0] += 1
        for kt in range(NT):
            idma(slot[:, kt, :].k(kt), rows_ap, wi[:, s, kt:kt + 1], gather=True)
        return slot

    XT = [AR.alloc(f"XT{i}", [NT, G], BF16) for i in range(2)]
    xg = [AR.alloc(f"xg{i}", [NSUB, D], BF16) for i in range(2)]
    actT = [AR.alloc(f"actT{i}", [NT, G], BF16) for i in range(2)]
    gc = [AR.alloc(f"gc{i}", [G], F32) for i in range(2)]
    sgm = [AR.alloc(f"sgm{i}", [G], F32) for i in range(2)]
    uc = [AR.alloc(f"uc{i}", [G], F32) for i in range(2)]
    tg = [AR.alloc(f"tg{i}", [G], F32) for i in range(2)]
    ysb = [AR.alloc(f"ysb{i}", [D], F32) for i in range(2)]
    tsl = [AR.alloc(f"tsl{i}", [NSUB], I32) for i in range(2)]
    b1c = [AR.alloc(f"b1c{i}", [16], F32) for i in range(2)]
    h2keys = [h2d.k(ti) for ti in range(NTILE)]
    ykeys = []
    loaded = {}

    def issue_load(s):
        b = s % 2
        P.dma("sp", tsl[b], tokslot[s * G:(s + 1) * G, :].re("(p sub) o -> p (sub o)", sub=NSUB), extra_reads=sc_keys)
        for sub in range(NSUB):
            idma(xg[b][:, sub, :].k(sub), h2d, tsl[b][:, sub:sub + 1], gather=True, extra_reads=h2keys)
        idma(b1c[b], I["b1l"], bidx[:, s:s + 1], gather=True)
        loaded[s] = (wgather(w1rows, s, widx_g), wgather(w1rows, s, widx_u), wgather(w2rows, s, widx))

    issue_load(0)
    for s in range(NSLOT_T):
        b = s % 2
        wg_, wu_, w2_ = loaded.pop(s)
        for dt in range(NT):
            pb = bank()
            pbb = pb.cast(BF16)
            for sub in range(NSUB):
                P.tr(pbb[:, sub * 128:(sub + 1) * 128], xg[b][:, sub, dt * 128:(dt + 1) * 128].k(sub), identb)
            P.copy("act" if dt % 2 else "dve", XT[b][:, dt, :], pbb[:, 0:G])
        if s + 1 < NSLOT_T:
            issue_load(s + 1)
        wgk = [wg_.k(kt) for kt in range(NT)]
        wuk = [wu_.k(kt) for kt in range(NT)]
        w2k = [w2_.k(kt) for kt in range(NT)]
        P.ts("dve", b1c[b][:, 8:16], b1c[b][:, 8:16], 1.0, None, op0=ALU.add)
        aT = actT[b]
        for jj in range(NT):
            bb = jj % 2
            pg = bank()
            P.mm(pg[:, 0:G], [(wg_[:, k, jj * 128:(jj + 1) * 128], XT[b][:, k, :]) for k in range(NT)], extra_ins=wgk)
            pu = bank()
            P.mm(pu[:, 0:G], [(wu_[:, k, jj * 128:(jj + 1) * 128], XT[b][:, k, :]) for k in range(NT)], extra_ins=wuk)
            P.ts("dve", gc[bb], pg[:, 0:G], b1c[b][:, jj:jj + 1], 7.0, op0=ALU.add, op1=ALU.min)
            P.act(sgm[bb], gc[bb], AF.Sigmoid, scale=1.702)
            P.ts("dve", uc[bb], pu[:, 0:G], b1c[b][:, 8 + jj:9 + jj], 8.0, op0=ALU.add, op1=ALU.min)
            P.tt("dve", tg[bb], gc[bb], sgm[bb], ALU.mult)
            P.stt(aT[:, jj, :], uc[bb], -6.0, tg[bb], ALU.max, ALU.mult)
        for sub in range(NSUB):
            yb_ = ysb[sub % 2]
            for hh in range(2):
                pb = bank()
                P.mm(pb[:, :], [(aT[:, k, sub * 128:(sub + 1) * 128], w2_[:, k, hh * 512:(hh + 1) * 512]) for k in range(NT)],
                     extra_ins=w2k)
                P.copy("act", yb_[:, hh * 512:(hh + 1) * 512], pb[:, :])
            yk_ = Yd.k(f"{s}_{sub}")
            P.dma("sp", Buf(A(Yd)[s * G:(s + 1) * G, :].rearrange("(p sub) d -> p sub d", sub=NSUB)[:, sub, :], yk_.key), yb_)
            ykeys.append(yk_)
    AR.pop()
    P.barrier()

    AR.push()
    l2g_bc = AR.alloc("l2g_bc", [D], F32)
    l2b_bc = AR.alloc("l2b_bc", [D], F32)
    P.dma("sp", l2g_bc, I["ln2_g"].partition_broadcast(128))
    P.dma("sp", l2b_bc, I["ln2_b"].partition_broadcast(128))
    bst = AR.alloc("bst2", [2, 6], F32)
    mv = AR.alloc("mv2", [2], F32)
    rstd = AR.alloc("rstd2", [1], F32)
    nmr = AR.alloc("nmr2", [1], F32)
    accs = [AR.alloc(f"accs{i}", [D], F32) for i in range(2)]
    ygk2 = [[AR.alloc(f"ygk{j}_{i}", [D], F32) for i in range(4)] for j in range(2)]
    tq = [AR.alloc(f"tq{i}", [D], F32) for i in range(2)]
    yb = [AR.alloc(f"yb{i}", [D], F32) for i in range(2)]
    y2 = [AR.alloc(f"y2{i}", [D], F32) for i in range(2)]
    for ti, (g0, R) in enumerate(tiles):
        g2bc = g2bc_s if g0 >= L else g2bc_p
        av = accs[ti % 2]
        ygk = ygk2[ti % 2]
        P.dma("sp", av[0:R, :], accd[g0:g0 + R, :].k(ti))
        for k in range(4):
            idma(ygk[k][0:R, :], Yd, slots_i[0:R, ti, k:k + 1].k(ti), gather=True, extra_reads=ykeys)
        t_ = tq[ti % 2]
        P.ts("dve", t_[0:R, :], ygk[0][0:R, :], gates[0:R, ti, 0:1].k(ti), None, op0=ALU.mult)
        for k in range(1, 4):
            P.stt(t_[0:R, :], ygk[k][0:R, :], gates[0:R, ti, k:k + 1].k(ti), t_[0:R, :], ALU.mult, ALU.add)
        P.tt("dve", t_[0:R, :], t_[0:R, :], g2bc[0:R, :], ALU.mult)
        P.tt("dve", av[0:R, :], av[0:R, :], t_[0:R, :], ALU.add)
        for hh in range(2):
            P.op("dve", (lambda h, o=A(bst[0:R, hh, :]), i=A(av[0:R, hh * 512:(hh + 1) * 512]): h.bn_stats(out=o, in_=i)),
                 [bst.k(hh)], [av])
        P.op("dve", (lambda h, o=A(mv[0:R, :]), i=A(bst[0:R].re("p a b -> p (a b)")): h.bn_aggr(out=o, in_=i)),
             [mv], [bst.k(0), bst.k(1)])
        P.act(rstd[0:R, :], mv[0:R, 1:2], AF.Sqrt, bias=epsc[0:R, 0:1])
        P.op("dve", (lambda h, o=A(rstd[0:R, :]), i=A(rstd[0:R, :]): h.reciprocal(out=o, in_=i)), [rstd], [rstd])
        P.stt(nmr[0:R, :], mv[0:R, 0:1], -1.0, rstd[0:R, :], ALU.mult, ALU.mult)
        y_ = yb[ti % 2]
        z_ = y2[ti % 2]
        P.act(y_[0:R, :], av[0:R, :], AF.Identity, bias=nmr[0:R, 0:1], scale=rstd[0:R, 0:1])
        P.tt("dve", z_[0:R, :], y_[0:R, :], l2g_bc[0:R, :], ALU.mult)
        P.tt("dve", z_[0:R, :], z_[0:R, :], l2b_bc[0:R, :], ALU.add)
        dst = O["ys"] if g0 >= L else O["yp"][g0:g0 + R, :]
        P.dma("sp", dst, z_[0:R, :], is_out=True)
    AR.pop()
    print("arena peak bytes", AR.peak, "ops", {e: len(P.ops[e]) for e in ENGS})
    return finish()


def _consts():
    ident = np.eye(128, dtype=np.float32)
    selu = np.zeros((128, 16, 128), np.float32)
    sely = np.zeros((128, 16, 128), np.float32)
    for lp in range(4):
        for s in range(4):
            for r in range(32):
                selu[lp * 32 + r, lp * 4 + s, s * 32 + r] = 1.0
                sely[s * 32 + r, lp * 4 + s, lp * 32 + r] = 1.0
    selp = np.zeros((NSEQ, 128), np.float32)
    selp[0, :] = 1.0
    sels = np.zeros((NSEQ, DS), np.float32)
    for s in range(16):
        sels[1 + s, 4 * s:4 * s + 4] = 1.0
    ls = np.triu(np.ones((128, 128), np.float32), 1)
    iota32 = np.tile(np.arange(32, dtype=np.float32)[None, :], (128, 1))
    tokid = np.zeros((128, 17), np.float32)
    for ti in range(16):
        tokid[:, ti] = ti * 128 + np.arange(128)
    tokid[:, 16] = 2048 + np.arange(128)
    svals = (512.0 * np.arange(48, dtype=np.float32))[None, :]
    iotak = (128.0 * np.arange(8, dtype=np.float32))[None, :] + np.arange(128, dtype=np.float32)[:, None]
    iotae = np.arange(32, dtype=np.float32)[:, None]
    esel = np.zeros((128, 17, 17), np.float32)
    selt = np.zeros((17, 17, 128), np.float32)
    for ti in range(17):
        esel[:, ti, ti] = 1.0
        selt[ti, ti, :] = 1.0
    return dict(k_ident=ident, k_selu=selu.reshape(128, -1), k_sely=sely.reshape(128, -1), k_selp=selp, k_sels=sels,
                k_ls=ls, k_iota32=iota32, k_tokid=tokid, k_svals=svals, k_iotak=iotak, k_iotae=iotae,
                k_esel=esel.reshape(128, -1), k_selt=selt.reshape(17, -1))


_NC_CACHE = {}


def make_in_maps(inputs):
    f = lambda a: np.ascontiguousarray(np.asarray(a, dtype=np.float32))
    consts = _consts()
    shared = {}
    for nm in ("w_ada", "b_ada", "w_in", "b_in", "w_dw", "b_dw", "conv_ln_g", "conv_ln_b", "w_pw", "b_pw",
               "lam_re", "lam_im", "log_dt", "b_re", "b_im", "c_re", "c_im", "d_skip", "w_sv", "b_sv", "w_sg", "b_sg",
               "w_out", "b_out", "ln1_g", "ln1_b", "w_router", "b_router", "w1", "w2", "b2", "ln2_g", "ln2_b"):
        shared[nm] = f(inputs[nm][0])
    shared["b1l"] = np.ascontiguousarray(f(inputs["b1"][0]).reshape(32, 16, 128).transpose(0, 2, 1).reshape(32 * 128, 16))
    shared.update(consts)
    xp, xs = f(inputs["x_prompt"]), f(inputs["x_sample"])
    cp, cs = f(inputs["c_prompt"]), f(inputs["c_sample"])
    sc, sr, si = f(inputs["state_conv"][0]), f(inputs["state_ssm_re"][0]), f(inputs["state_ssm_im"][0])
    maps = []
    for i in range(8):
        m = dict(shared)
        m["xp"] = xp[i]
        m["xs"] = np.ascontiguousarray(xs[16 * i:16 * i + 16].reshape(DS, D))
        m["cc"] = np.ascontiguousarray(np.concatenate([cp[i:i + 1], cs[16 * i:16 * i + 16]], axis=0))
        m["sconv"] = np.ascontiguousarray(sc[16 * i:16 * i + 16])
        m["sre"] = np.ascontiguousarray(sr[16 * i:16 * i + 16].reshape(16, 2048))
        m["sim"] = np.ascontiguousarray(si[16 * i:16 * i + 16].reshape(16, 2048))
        maps.append(m)
    return maps


def assemble(results):
    yp = np.stack([np.asarray(r["yp"], np.float32) for r in results], 0)
    ys = np.concatenate([np.asarray(r["ys"], np.float32).reshape(16, 4, D) for r in results], 0)
    ncp = np.stack([np.asarray(r["ncp"], np.float32) for r in results], 0)[None]
    nrp = np.stack([np.asarray(r["nrp"], np.float32).reshape(32, 64) for r in results], 0)[None]
    nip = np.stack([np.asarray(r["nip"], np.float32).reshape(32, 64) for r in results], 0)[None]
    ncs = np.concatenate([np.asarray(r["ncs"], np.float32) for r in results], 0)[None]
    nrs = np.concatenate([np.asarray(r["nrs"], np.float32).reshape(16, 32, 64) for r in results], 0)[None]
    nis = np.concatenate([np.asarray(r["nis"], np.float32).reshape(16, 32, 64) for r in results], 0)[None]
    return (yp, ys, ncp, nrp, nip, ncs, nrs, nis)


def kernel(**inputs):
    if "nc" not in _NC_CACHE:
        _NC_CACHE["nc"] = build_program()
    nc = _NC_CACHE["nc"]
    maps = make_in_maps(inputs)
    res = run_bass_kernel_spmd(nc, maps, core_ids=list(range(8)))
    return assemble(res.results)
```
